# Optimizing a Trainium2 kernel written in Bass

```python
import jax, jax.numpy as jnp
from jax import lax
import numpy as np

D_MODEL = 1024
BATCH = 2
SEQ = 8192
DEPTH = 2

GRID_W = 64
CTX_LEN = 256
N_MIXERS = 2
HG_HEADS = 8
HG_HEAD_DIM = D_MODEL // HG_HEADS
HG_CHUNK = 64
SC_WIDTH = 3
N_EXPERTS = 16
EC_CAPACITY_FACTOR = 2
D_EXPERT = 2 * D_MODEL
N_ADA = 6
N_HGRN_LAYERS = (DEPTH + 1) // 2
N_CONV_LAYERS = DEPTH // 2
LAST_CTX_READER = N_MIXERS * ((DEPTH - 1) // N_MIXERS)
EPS = 1e-6
POS_TEMP = 10000.0

kernel_name = "hybrid_hgrn2_shortconv_ecmoe_dit"


def _rmsnorm(x, g):
    xf = x.astype(jnp.float32)
    y = xf * lax.rsqrt(jnp.mean(xf * xf, axis=-1, keepdims=True) + EPS)
    return (y * g.astype(jnp.float32)).astype(x.dtype)


def _modulate(x, g, shift, scale):
    return _rmsnorm(x, g) * (1 + scale) + shift


def _grid_sincos(n_tokens, dtype):
    rows = n_tokens // GRID_W
    row = jnp.repeat(jnp.arange(rows), GRID_W).astype(jnp.float32)
    col = jnp.tile(jnp.arange(GRID_W), rows).astype(jnp.float32)
    n_freq = D_MODEL // 4
    omega = POS_TEMP ** (-jnp.arange(n_freq, dtype=jnp.float32) / n_freq)
    def emb(p):
        a = p[:, None] * omega[None, :]
        return jnp.concatenate([jnp.sin(a), jnp.cos(a)], axis=-1)
    return jnp.concatenate([emb(row), emb(col)], axis=-1).astype(dtype)


def _gla_chunks(q, k, v, logf, s0):
    b_, h_, t_, _ = q.shape
    n = t_ // HG_CHUNK
    def to_chunks(a):
        return jnp.moveaxis(a.reshape(b_, h_, n, HG_CHUNK, a.shape[-1]), 2, 0)
    incl = jnp.tril(jnp.ones((HG_CHUNK, HG_CHUNK), dtype=bool))[:, :, None]
    def step(s, inp):
        qc, kc, vc, lf = inp
        cum = jnp.cumsum(lf.astype(jnp.float32), axis=2)
        diff = cum[:, :, :, None, :] - cum[:, :, None, :, :]
        decay = jnp.exp(jnp.where(incl, diff, -jnp.inf))
        scores = jnp.einsum('bhtk,bhsk,bhtsk->bhts', qc, kc, decay)
        o = (jnp.einsum('bhts,bhsv->bhtv', scores, vc)
             + jnp.einsum('bhtk,bhkv->bhtv', qc * jnp.exp(cum), s))
        last = cum[:, :, -1, :]
        s = (jnp.exp(last)[..., None] * s
             + jnp.einsum('bhsk,bhsv->bhkv', kc * jnp.exp(last[:, :, None, :] - cum), vc))
        return s, o
    s_fin, o = lax.scan(step, s0, (to_chunks(q), to_chunks(k), to_chunks(v), to_chunks(logf)))
    o = jnp.moveaxis(o, 0, 2).reshape(b_, h_, t_, -1)
    return o, s_fin


def _hgrn_heads(h, w_in, lb, s0_fwd, s0_bwd):
    b_, t_, _ = h.shape
    q, i_in, z_fwd, z_bwd, g = jnp.split(h @ w_in, 5, axis=-1)
    def heads(a):
        return a.reshape(b_, t_, HG_HEADS, HG_HEAD_DIM).transpose(0, 2, 1, 3)
    q = heads(q) * HG_HEAD_DIM ** -0.5
    v = heads(i_in)
    lbh = lb.reshape(HG_HEADS, 1, HG_HEAD_DIM)
    def forget(z):
        f = lbh + (1 - lbh) * jax.nn.sigmoid(heads(z).astype(jnp.float32))
        return jnp.log(f), 1.0 - f
    logf_f, k_f = forget(z_fwd)
    logf_b, k_b = forget(z_bwd)
    o_f, s_f = _gla_chunks(q, k_f, v, logf_f, s0_fwd)
    flip = lambda a: jnp.flip(a, axis=2)
    o_b, s_b = _gla_chunks(flip(q), flip(k_b), flip(v), flip(logf_b), s0_bwd)
    return o_f + flip(o_b), g, s_f, s_b


def _hgrn_out(o, g, norm_g, w_out):
    b_, h_, t_, dv = o.shape
    o = _rmsnorm(o, norm_g)
    o = o.transpose(0, 2, 1, 3).reshape(b_, t_, h_ * dv).astype(g.dtype) * jax.nn.silu(g)
    return o @ w_out


def _short_conv(h, w_in, w_conv, w_out):
    b_gate, c_gate, u = jnp.split(h @ w_in, 3, axis=-1)
    u = c_gate * u
    y = lax.conv_general_dilated(
        u, w_conv.astype(u.dtype)[:, None, :], window_strides=(1,),
        padding=((SC_WIDTH // 2, SC_WIDTH // 2),),
        dimension_numbers=('NWC', 'WIO', 'NWC'), feature_group_count=D_MODEL)
    return (b_gate * y) @ w_out


def _ec_moe(h, w_router, w_gate, w_up, w_down):
    b_, t_, _ = h.shape
    cap = EC_CAPACITY_FACTOR * t_ // N_EXPERTS
    affinity = jax.nn.softmax((h @ w_router).astype(jnp.float32), axis=-1)
    gate, idx = lax.top_k(jnp.swapaxes(affinity, 1, 2), cap)
    bidx = jnp.arange(b_)[:, None, None]
    xg = h[bidx, idx]
    a = jnp.einsum('becd,edf->becf', xg, w_gate)
    u = jnp.einsum('becd,edf->becf', xg, w_up)
    y = jnp.einsum('becf,efd->becd', jax.nn.silu(a) * u, w_down)
    y = y * gate[..., None].astype(y.dtype)
    return jnp.zeros_like(h).at[bidx, idx].add(y)


def setup_inputs(seed: int = 0) -> dict:
    key = jax.random.key(seed)
    ks = jax.random.split(key, 22)
    f32 = jnp.float32
    def nrm(k, shape, fan_in):
        return jax.random.normal(k, shape, f32) * fan_in ** -0.5
    def gain(k, shape):
        return 1.0 + 0.02 * jax.random.normal(k, shape, f32)
    return {
        "x": jax.random.normal(ks[0], (BATCH, SEQ, D_MODEL), f32),
        "c": jax.random.normal(ks[1], (BATCH, D_MODEL), f32),
        "ctx": jax.random.normal(ks[2], (BATCH, CTX_LEN, D_MODEL), f32),
        "c_ctx": jax.random.normal(ks[3], (D_MODEL,), f32),
        "ada_w": 0.5 * nrm(ks[4], (DEPTH, D_MODEL, N_ADA * D_MODEL), D_MODEL),
        "ada_b": 0.02 * jax.random.normal(ks[5], (DEPTH, N_ADA * D_MODEL), f32),
        "norm_mix": gain(ks[6], (DEPTH, D_MODEL)),
        "norm_ffn": gain(ks[7], (DEPTH, D_MODEL)),
        "norm_final": gain(ks[8], (D_MODEL,)),
        "hg_w_in": nrm(ks[9], (N_HGRN_LAYERS, D_MODEL, 5 * D_MODEL), D_MODEL),
        "hg_lb_logits": 0.1 * jax.random.normal(ks[10], (DEPTH + 1, D_MODEL), f32),
        "hg_norm": gain(ks[11], (N_HGRN_LAYERS, HG_HEAD_DIM)),
        "hg_w_out": nrm(ks[12], (N_HGRN_LAYERS, D_MODEL, D_MODEL), D_MODEL),
        "sc_w_in": nrm(ks[13], (N_CONV_LAYERS, D_MODEL, 3 * D_MODEL), D_MODEL),
        "sc_conv": nrm(ks[14], (N_CONV_LAYERS, SC_WIDTH, D_MODEL), SC_WIDTH),
        "sc_w_out": nrm(ks[15], (N_CONV_LAYERS, D_MODEL, D_MODEL), D_MODEL),
        "moe_router": nrm(ks[16], (DEPTH, D_MODEL, N_EXPERTS), D_MODEL),
        "moe_w_gate": nrm(ks[17], (DEPTH, N_EXPERTS, D_MODEL, D_EXPERT), D_MODEL),
        "moe_w_up": nrm(ks[18], (DEPTH, N_EXPERTS, D_MODEL, D_EXPERT), D_MODEL),
        "moe_w_down": nrm(ks[19], (DEPTH, N_EXPERTS, D_EXPERT, D_MODEL), D_EXPERT),
    }


def reference(x, c, ctx, c_ctx, ada_w, ada_b, norm_mix, norm_ffn, norm_final,
              hg_w_in, hg_lb_logits, hg_norm, hg_w_out,
              sc_w_in, sc_conv, sc_w_out,
              moe_router, moe_w_gate, moe_w_up, moe_w_down):
    b_, t_, _ = x.shape
    x = x + _grid_sincos(t_, x.dtype)[None]
    xc = ctx
    lower_bounds = jnp.cumsum(jax.nn.softmax(hg_lb_logits.astype(jnp.float32), axis=0), axis=0)
    silu_c = jax.nn.silu(c)
    silu_cc = jax.nn.silu(c_ctx)
    for i in range(DEPTH):
        j = i // N_MIXERS
        ctx_live = i < LAST_CTX_READER
        sh1, sc1, g1, sh2, sc2, g2 = jnp.split((silu_c @ ada_w[i] + ada_b[i])[:, None, :], N_ADA, axis=-1)
        csh1, csc1, cg1, csh2, csc2, cg2 = jnp.split(silu_cc @ ada_w[i] + ada_b[i], N_ADA, axis=-1)
        h = _modulate(x, norm_mix[i], sh1, sc1)
        if i % N_MIXERS == 0:
            hc = _modulate(xc, norm_mix[i], csh1, csc1)
            zeros = jnp.zeros((b_, HG_HEADS, HG_HEAD_DIM, HG_HEAD_DIM), jnp.float32)
            oc, gc, s_f, s_b = _hgrn_heads(hc, hg_w_in[j], lower_bounds[i], zeros, zeros)
            o, g, _, _ = _hgrn_heads(h, hg_w_in[j], lower_bounds[i], s_f, s_b)
            x = x + g1 * _hgrn_out(o, g, hg_norm[j], hg_w_out[j])
            if ctx_live:
                xc = xc + cg1 * _hgrn_out(oc, gc, hg_norm[j], hg_w_out[j])
        else:
            x = x + g1 * _short_conv(h, sc_w_in[j], sc_conv[j], sc_w_out[j])
            if ctx_live:
                hc = _modulate(xc, norm_mix[i], csh1, csc1)
                xc = xc + cg1 * _short_conv(hc, sc_w_in[j], sc_conv[j], sc_w_out[j])
        h = _modulate(x, norm_ffn[i], sh2, sc2)
        x = x + g2 * _ec_moe(h, moe_router[i], moe_w_gate[i], moe_w_up[i], moe_w_down[i])
        if ctx_live:
            hc = _modulate(xc, norm_ffn[i], csh2, csc2)
            xc = xc + cg2 * _ec_moe(hc, moe_router[i], moe_w_gate[i], moe_w_up[i], moe_w_down[i])
    return _rmsnorm(x, norm_final)
```

```python
import numpy as np
from contextlib import ExitStack
import concourse.bass as bass
import concourse.mybir as mybir
from concourse.bass_utils import run_bass_kernel_spmd

F32 = mybir.dt.float32
BF16 = mybir.dt.bfloat16
I32 = mybir.dt.int32
AF = mybir.ActivationFunctionType
ALU = mybir.AluOpType
AX = mybir.AxisListType

D = 1024
NH = 8
NE = 16
CTXL = 256
EPS = 1e-6
BIG = 16384.0
ENGS = ["sync", "scalar", "vector", "gpsimd", "tensor"]
NDMA = 32
SAME_ENGINE_SYNC = True


class Cfg:
    def __init__(self, NT=2048, DEXP=2048, stop=99, debug=False):
        self.NT = NT
        self.DEXP = DEXP
        self.stop = stop
        self.debug = debug
        self.NB = NT // 128
        self.NCH = NT // 64
        self.CAP = NT // 2
        self.CAPP = self.CAP + 128
        self.NSB = self.CAP // 128
        self.NFC = DEXP // 128


class Sched:
    def __init__(self, nc, sems, n_dma):
        self.nc = nc
        self.ops = {e: [] for e in ENGS}
        self.cnt = {e: 0 for e in ENGS}
        self.waited = {e: {} for e in ENGS}
        self.last_w = {}
        self.readers = {}
        self.esem = {"scalar": sems[0], "vector": sems[1], "gpsimd": sems[2], "tensor": sems[3]}
        self.dsem = list(sems[4:4 + n_dma])
        self.csem = sems[4 + n_dma]
        self.cval = 0
        self.dval = [0] * n_dma
        self.drr = 0
        self.grr = 0
        self.n_g = 12

    def _semh(self, key):
        if isinstance(key, str):
            return self.esem[key]
        return self.csem if key[0] == "c" else self.dsem[key[1]]

    def _need(self, eng, ev, waits):
        if ev is None:
            return
        key, val = ev
        if key == eng and (eng == "tensor" or not SAME_ENGINE_SYNC):
            return
        if self.waited[eng].get(key, 0) >= val:
            return
        self.waited[eng][key] = val
        waits.append((self._semh(key), val))

    def _hazards(self, eng, reads, writes, waits):
        for r in reads:
            self._need(eng, self.last_w.get(r), waits)
        for w in writes:
            self._need(eng, self.last_w.get(w), waits)
            for ev in self.readers.get(w, ()):
                self._need(eng, ev, waits)

    def _commit(self, ev, reads, writes):
        for r in reads:
            self.readers.setdefault(r, []).append(ev)
        for w in writes:
            self.last_w[w] = ev
            self.readers[w] = []

    def op(self, eng, fn, reads=(), writes=()):
        waits = []
        self._hazards(eng, reads, writes, waits)
        self.cnt[eng] += 1
        ev = (eng, self.cnt[eng])
        self.ops[eng].append((waits, fn, self.esem[eng], 1))
        self._commit(ev, reads, writes)
        return ev

    def dma(self, eng, fn, reads=(), writes=(), inc=16):
        waits = []
        self._hazards(eng, reads, writes, waits)
        if eng == "gpsimd":
            i = self.grr
            self.grr = (self.grr + 1) % self.n_g
        else:
            i = self.n_g + self.drr
            self.drr = (self.drr + 1) % (len(self.dsem) - self.n_g)
        key = ("d", i)
        if self.dval[i] > 0:
            self._need(eng, (key, self.dval[i]), waits)
        self.dval[i] += inc
        ev = (key, self.dval[i])
        self.ops[eng].append((waits, fn, self.dsem[i], inc))
        self._commit(ev, reads, writes)
        return ev

    def coll(self, fn, reads=(), writes=()):
        waits = []
        self._hazards("gpsimd", reads, writes, waits)
        self.cval += 1
        ev = (("c", 0), self.cval)
        self.ops["gpsimd"].append((waits, fn, self.csem, 1))
        self._commit(ev, reads, writes)
        return ev

    def barrier(self):
        for eng in ENGS:
            waits = []
            for k in self.esem:
                if self.cnt[k] > 0:
                    self._need(eng, (k, self.cnt[k]), waits)
            for i in range(len(self.dsem)):
                if self.dval[i] > 0:
                    self._need(eng, (("d", i), self.dval[i]), waits)
            if self.cval > 0:
                self._need(eng, (("c", 0), self.cval), waits)
            self.ops[eng].append((waits, None, None, 0))
        self.last_w = {}
        self.readers = {}

    def emit(self):
        nc = self.nc
        ops = self.ops
        self.ops = {e: [] for e in ENGS}
        with nc.Block() as block:
            def run(e, lst):
                for waits, fn, sem, inc in lst:
                    for s, v in waits:
                        e.wait_ge(s, v)
                    if fn is not None:
                        fn(e).then_inc(sem, inc)

            @block.sync
            def _(e):
                run(e, ops["sync"])

            @block.scalar
            def _(e):
                run(e, ops["scalar"])

            @block.vector
            def _(e):
                run(e, ops["vector"])

            @block.gpsimd
            def _(e):
                run(e, ops["gpsimd"])

            @block.tensor
            def _(e):
                run(e, ops["tensor"])


CONST_SPEC = [("ident", 128), ("maskf", 128), ("maskb", 128), ("Bm", 128), ("Tm", 128), ("hgn", 128),
              ("sel", 32), ("retbase", 32), ("gsel", 32), ("bsel", 2), ("mf", 8), ("mb", 8),
              ("selL", 8), ("selR", 8), ("nmix", 16), ("nffn", 16), ("nfin", 8), ("lbl", 24),
              ("scw", 24), ("adab", 12), ("cvT", 32), ("ones", 128)]
CO = {}
_o = 0
for _n, _w in CONST_SPEC:
    CO[_n] = (_o, _w)
    _o += _w
NCONST = _o


def k_dma(out, in_):
    return lambda e: e.dma_start(out=out, in_=in_)


def k_mm(out, lhsT, rhs, start, stop):
    return lambda e: e.matmul(out, lhsT=lhsT, rhs=rhs, start=start, stop=stop)


def k_tr(out, in_, ident):
    return lambda e: e.transpose(out, in_, ident)


def k_act(out, in_, func, scale=1.0, bias=None, accum=None):
    def f(e):
        kw = {}
        if bias is not None:
            kw["bias"] = bias
        if accum is not None:
            kw["accum_out"] = accum
        return e.activation(out=out, in_=in_, func=func, scale=scale, **kw)
    return f


def k_ts(out, in0, s1, s2, op0, op1=None, accum=None):
    def f(e):
        kw = {}
        if op1 is not None:
            kw["op1"] = op1
        if accum is not None:
            kw["accum_out"] = accum
        return e.tensor_scalar(out=out, in0=in0, scalar1=s1, scalar2=s2, op0=op0, **kw)
    return f


def k_tt(out, in0, in1, op):
    return lambda e: e.tensor_tensor(out=out, in0=in0, in1=in1, op=op)


def k_stt(out, in0, scalar, in1, op0, op1):
    return lambda e: e.scalar_tensor_tensor(out=out, in0=in0, scalar=scalar, in1=in1, op0=op0, op1=op1)


def k_copy(out, in_):
    return lambda e: e.tensor_copy(out=out, in_=in_)


def k_memset(ap, v):
    return lambda e: e.memset(ap, v)


def k_scan(out, d0, d1, init, op0, op1):
    return lambda e: e.tensor_tensor_scan(out=out, data0=d0, data1=d1, initial=init, op0=op0, op1=op1)


def k_recip(out, in_):
    return lambda e: e.reciprocal(out=out, in_=in_)


def k_ag(in_, out):
    return lambda e: e.collective_compute("AllGather", ALU.bypass, replica_groups=[list(range(8))],
                                          ins=[in_], outs=[out])


BREG = {}


def _breg(e, bound):
    if bound not in BREG:
        BREG[bound] = e.to_reg(bound)
    return BREG[bound]


def k_scatter(out, idx, in_, bound):
    return lambda e: e.indirect_dma_start(out=out, out_offset=bass.IndirectOffsetOnAxis(ap=idx, axis=0),
                                          in_=in_, in_offset=None, bounds_check=_breg(e, bound), oob_is_err=False)


def k_gather(out, in_, idx, bound):
    return lambda e: e.indirect_dma_start(out=out, out_offset=None, in_=in_,
                                          in_offset=bass.IndirectOffsetOnAxis(ap=idx, axis=0),
                                          bounds_check=_breg(e, bound), oob_is_err=False)


def build(cfg):
    NT, NB, NCH, CAP, CAPP, NSB, DEXP, NFC = cfg.NT, cfg.NB, cfg.NCH, cfg.CAP, cfg.CAPP, cfg.NSB, cfg.DEXP, cfg.NFC
    nc = bass.Bass("TRN2", target_bir_lowering=False)
    BREG.clear()

    def din(name, shape, dt=F32):
        return nc.dram_tensor(name, list(shape), dt, kind="ExternalInput").ap()

    def dscr(name, shape, dt=F32):
        return nc.dram_tensor(name, list(shape), dt).ap()

    x_in = din("x", [NT, D])
    pos_in = din("pos", [NT, D])
    ctx_in = din("ctx", [CTXL, D])
    consts_in = din("consts", [128, NCONST])
    adaw_in = din("adaw", [2, D, 768])
    hgwin_in = din("hg_w_in", [D, 5 * D])
    hgwout_in = din("hg_w_out", [D, D])
    scwin_in = din("sc_w_in", [D, 3 * D])
    scwout_in = din("sc_w_out", [D, D])
    router_in = din("router", [2, D, NE])
    wg_in = din("wg", [2, 2, D, DEXP])
    wu_in = din("wu", [2, 2, D, DEXP])
    wd_in = din("wd", [2, 2, DEXP, D])
    out_ap = nc.dram_tensor("out", [NT, D], F32, kind="ExternalOutput").ap()
    dbg = {}
    if cfg.debug:
        for nm, shp in (("d_xmix0", [NT, D]), ("d_xmoe0", [NT, D]), ("d_xmix1", [NT, D]), ("d_xmoe1", [NT, D]), ("d_h1", [NT, D]),
                        ("d_o", [NT, D]), ("d_aff", [NE, NT]), ("d_dest", [128, NB * 32])):
            dbg[nm] = nc.dram_tensor(nm, shp, F32, kind="ExternalOutput").ap()

    xres = dscr("xres", [NT, D])
    ada_loc = dscr("ada_loc", [128, 48])
    ada_all = dscr("ada_all", [8 * 128, 48])
    qseg_scr = dscr("qseg_scr", [NH, 2, 128, NT], BF16)
    o_scr = dscr("o_scr", [NH, 128, NB * 128])
    st_loc = dscr("st_loc", [128, NH * 258])
    st_all = dscr("st_all", [8 * 128, NH * 258])
    aff_loc = [dscr(f"aff_loc{l}", [NE, NT]) for l in range(2)]
    aff_all = [dscr(f"aff_all{l}", [8 * NE, NT]) for l in range(2)]
    ROWW = D + 32
    h2_loc = [dscr(f"h2_loc{l}", [NT, ROWW], BF16) for l in range(2)]
    h2_all = [dscr(f"h2_all{l}", [8 * NT, ROWW], BF16) for l in range(2)]
    xg = [[dscr(f"xg{l}_{i}", [CAPP, ROWW], BF16) for i in range(4)] for l in range(2)]
    y_loc = [dscr(f"y_loc{l}", [4 * CAPP, D], BF16) for l in range(2)]
    y_all = [dscr(f"y_all{l}", [8 * 4 * CAPP, D], BF16) for l in range(2)]
    halo_loc = dscr("halo_loc", [128, 16])
    halo_all = dscr("halo_all", [8 * 128, 16])

    with ExitStack() as top:
        uid = [0]

        def sb(es, name, shape, dt):
            uid[0] += 1
            return es.enter_context(nc.sbuf_tensor(f"{name}_u{uid[0]}", list(shape), dt))

        def ps(es, name, shape, dt):
            uid[0] += 1
            return es.enter_context(nc.psum_tensor(f"{name}_u{uid[0]}", list(shape), dt))

        sems = [top.enter_context(nc.semaphore(f"s{i}")) for i in range(5 + NDMA)]
        S = Sched(nc, sems, NDMA)
        for s_ in sems:
            nc.gpsimd.sem_clear(s_)
        nc.all_engine_barrier()

        def finish():
            nc.all_engine_barrier()
            for s_ in sems:
                nc.gpsimd.sem_clear(s_)
            nc.all_engine_barrier()
        cst = sb(top, "cst", [128, NCONST], F32)

        def C(name, lo=0, n=None):
            o, w = CO[name]
            n = w - lo if n is None else n
            return cst[:, o + lo:o + lo + n]

        identb = sb(top, "identb", [128, 128], BF16)
        ADAo = sb(top, "ADAo", [128, 2 * 48], F32)
        ADAx = sb(top, "ADAx", [128, 48], F32)
        lbt = sb(top, "lbt", [128, 16], F32)
        S.dma("sync", k_dma(cst[:], consts_in), writes=["cst"])
        S.op("vector", k_copy(identb[:], C("ident")), reads=["cst"], writes=["identb"])
        ident = C("ident")

        def bcast_rows(BC, es_ps, col, slot, tag):
            pb = es_ps
            for dc in range(8):
                cb = colbc[dc % 2]
                S.op("vector", k_ts(cb[:], C("ones"), col[:, dc:dc + 1], None, ALU.mult),
                     reads=["cst", tag], writes=[f"colbc{dc % 2}"])
                S.op("tensor", k_mm(pb[dc // 4][:, (dc % 4) * 128:(dc % 4 + 1) * 128], cb[:], ident, True, True),
                     reads=[f"colbc{dc % 2}", "cst"], writes=[f"pb{dc // 4}"])
            for hlf in range(2):
                S.op("scalar", k_act(BC[:, slot, hlf * 512:(hlf + 1) * 512], pb[hlf][:], AF.Copy),
                     reads=[f"pb{hlf}"], writes=[f"BC{slot}"])

        def prenorm(BC, xt, xkey, gslot, sslot, hb, hbkey, ss, rs, sqj):
            S.op("scalar", k_act(sqj[:], xt, AF.Square, accum=ss[:, 0:1]), reads=[xkey], writes=["sqj", "ss"])
            S.op("scalar", k_act(rs[:, 0:1], ss[:, 0:1], AF.Sqrt, scale=1.0 / D, bias=epsb[:, 0:1]),
                 reads=["ss", "epsb"], writes=["rs"])
            S.op("vector", k_recip(rs[:, 1:2], rs[:, 0:1]), reads=["rs"], writes=["rs2"])
            S.op("vector", k_stt(sqj[:], xt, rs[:, 1:2], BC[:, gslot, :], ALU.mult, ALU.mult),
                 reads=[xkey, "rs2", f"BC{gslot}"], writes=["sqj"])
            S.op("vector", k_tt(hb, sqj[:], BC[:, sslot, :], ALU.add), reads=["sqj", f"BC{sslot}"], writes=[hbkey])

        def transpose_to(hb, hbkey, dstT, dkey, col0, ptile, pkey, eng="scalar"):
            for kc in range(8):
                S.op("tensor", k_tr(ptile[:, kc * 128:(kc + 1) * 128], hb[:, kc * 128:(kc + 1) * 128], identb[:]),
                     reads=[hbkey, "identb"], writes=[pkey])
            src = ptile[:].rearrange("p (k n) -> p k n", k=8)
            if eng == "scalar":
                S.op("scalar", k_act(dstT[:, :, col0:col0 + 128], src, AF.Copy), reads=[pkey], writes=[dkey])
            else:
                S.op("vector", k_copy(dstT[:, :, col0:col0 + 128], src), reads=[pkey], writes=[dkey])

        colbc = [sb(top, f"colbc{i}", [128, 128], F32) for i in range(2)]
        epsb = sb(top, "epsb", [128, 1], F32)
        S.op("gpsimd", k_memset(epsb[:], EPS), writes=["epsb"])

        with ExitStack() as ph:
            W = sb(ph, "adaW", [128, 8, 768], F32)
            scv = sb(ph, "scv", [128, 32], F32)
            adaloc = sb(ph, "adaloc", [128, 48], F32)
            adaA = sb(ph, "adaA", [128, 8, 48], F32)
            ADAc = sb(ph, "ADAc", [128, 6 * 48], F32)
            lbe = sb(ph, "lbe", [128, 32], F32)
            pa = ps(ph, "pa", [128, 8], F32)
            S.op("scalar", k_act(scv[:], C("cvT"), AF.Silu), reads=["cst"], writes=["scv"])
            for l in range(2):
                S.dma("sync", k_dma(W[:], adaw_in[l].rearrange("(kc p) n -> p kc n", p=128)), writes=["adaW"])
                for fc in range(6):
                    for kc in range(8):
                        S.op("tensor", k_mm(pa[:, 0:4], W[:, kc, fc * 128:(fc + 1) * 128], scv[:, kc * 4:(kc + 1) * 4],
                                            kc == 0, kc == 7), reads=["adaW", "scv"], writes=["pa"])
                    c0 = (l * 6 + fc) * 4
                    S.op("vector", k_ts(adaloc[:, c0:c0 + 4], pa[:, 0:4], C("adab", l * 6 + fc, 1), None, ALU.add),
                         reads=["pa", "cst"], writes=["adaloc"])
            S.dma("sync", k_dma(ada_loc, adaloc[:]), reads=["adaloc"], writes=["ada_loc"])
            S.coll(k_ag(ada_loc, ada_all), reads=["ada_loc"], writes=["ada_all"])
            S.dma("sync", k_dma(adaA[:], ada_all.rearrange("(r p) n -> p r n", p=128)), reads=["ada_all"], writes=["adaA"])
            for l in range(2):
                for v in range(3):
                    i6 = l * 3 + v
                    S.op("vector", k_copy(ADAc[:, i6 * 48:(i6 + 1) * 48].rearrange("p (r f) -> p r f", f=6),
                                          adaA[:, :, l * 24 + v:l * 24 + 24:4]), reads=["adaA"], writes=["ADAc"])
            for l in range(2):
                S.op("vector", k_ts(ADAo[:, l * 48:(l + 1) * 48], ADAc[:, (l * 3) * 48:(l * 3 + 1) * 48],
                                    C("bsel", 0, 1), None, ALU.mult), reads=["ADAc", "cst"], writes=["ADAo"])
                S.op("vector", k_stt(ADAo[:, l * 48:(l + 1) * 48], ADAc[:, (l * 3 + 1) * 48:(l * 3 + 2) * 48],
                                     C("bsel", 1, 1), ADAo[:, l * 48:(l + 1) * 48], ALU.mult, ALU.add),
                     reads=["ADAc", "cst", "ADAo"], writes=["ADAo"])
            S.op("vector", k_copy(ADAx[:], ADAc[:, 2 * 48:3 * 48]), reads=["ADAc"], writes=["ADAx"])
            S.op("scalar", k_act(lbe[:, 0:24], C("lbl"), AF.Exp), reads=["cst"], writes=["lbe"])
            S.op("vector", k_tt(lbe[:, 24:32], lbe[:, 0:8], lbe[:, 8:16], ALU.add), reads=["lbe"], writes=["lbe"])
            S.op("vector", k_tt(lbe[:, 24:32], lbe[:, 24:32], lbe[:, 16:24], ALU.add), reads=["lbe"], writes=["lbe"])
            S.op("vector", k_recip(lbe[:, 24:32], lbe[:, 24:32]), reads=["lbe"], writes=["lbe"])
            S.op("vector", k_tt(lbt[:, 0:8], lbe[:, 0:8], lbe[:, 24:32], ALU.mult), reads=["lbe"], writes=["lbt"])
            S.op("vector", k_ts(lbt[:, 8:16], lbt[:, 0:8], -1.0, 1.0, ALU.mult, ALU.add), reads=["lbt"], writes=["lbt"])
            S.barrier()
            S.emit()
            if cfg.stop == 0:
                finish()
                return nc

        def ada(l, j):
            return ADAo[:, l * 48 + j * 8:l * 48 + (j + 1) * 8]

        def make_gain(es, name, scale_col, norm_col, tagr):
            g = sb(es, name, [128, 8], F32)
            S.op("vector", k_stt(g[:], scale_col, 1.0, norm_col, ALU.add, ALU.mult), reads=tagr, writes=[name])
            return g

        with ExitStack() as ph:
            hT = sb(ph, "hT", [128, 8, NT], BF16)
            hcT = sb(ph, "hcT", [128, 8, CTXL], BF16)
            cstate = [sb(ph, f"cstate{d}", [128, NH, 128], F32) for d in range(2)]
            pb = [ps(ph, f"pb{i}", [128, 512], F32) for i in range(2)]
            with ExitStack() as ph1:
                BC = sb(ph1, "BC", [128, 4, D], F32)
                ptr = ps(ph1, "ptr", [128, 1024], BF16)
                xt = [sb(ph1, f"xt{i}", [128, D], F32) for i in range(2)]
                pt = [sb(ph1, f"pt{i}", [128, D], F32) for i in range(2)]
                hb = [sb(ph1, f"hb{i}", [128, D], BF16) for i in range(2)]
                sqj = sb(ph1, "sqj", [128, D], F32)
                ss = sb(ph1, "ss", [128, 1], F32)
                rs = sb(ph1, "rs", [128, 2], F32)
                G1 = make_gain(ph1, "G1c", ada(0, 1), C("nmix", 0, 8), ["ADAo", "cst"])
                GC = make_gain(ph1, "GCc", ADAx[:, 8:16], C("nmix", 0, 8), ["ADAx", "cst"])
                bcast_rows(BC, pb, G1, 0, "G1c")
                bcast_rows(BC, pb, ada(0, 0), 1, "ADAo")
                bcast_rows(BC, pb, GC, 2, "GCc")
                bcast_rows(BC, pb, ADAx[:, 0:8], 3, "ADAx")
                for j in range(NB):
                    i = j % 2
                    S.dma("sync", k_dma(xt[i][:], x_in[j * 128:(j + 1) * 128, :]), writes=[f"xt{i}"])
                    S.dma("sync", k_dma(pt[i][:], pos_in[j * 128:(j + 1) * 128, :]), writes=[f"pt{i}"])
                    S.op("gpsimd", k_tt(xt[i][:], xt[i][:], pt[i][:], ALU.add), reads=[f"xt{i}", f"pt{i}"], writes=[f"xt{i}"])
                    S.dma("sync", k_dma(xres[j * 128:(j + 1) * 128, :], xt[i][:]), reads=[f"xt{i}"], writes=["xres"])
                    prenorm(BC, xt[i][:], f"xt{i}", 0, 1, hb[i][:], f"hb{i}", ss, rs, sqj)
                    if cfg.debug:
                        S.op("vector", k_copy(sqj[:], hb[i][:]), reads=[f"hb{i}"], writes=["sqj"])
                        S.dma("sync", k_dma(dbg["d_h1"][j * 128:(j + 1) * 128, :], sqj[:]), reads=["sqj"], writes=["d_h1"])
                    transpose_to(hb[i], f"hb{i}", hT, "hT", j * 128, ptr, "ptr")
                for j in range(CTXL // 128):
                    i = j % 2
                    S.dma("sync", k_dma(xt[i][:], ctx_in[j * 128:(j + 1) * 128, :]), writes=[f"xt{i}"])
                    prenorm(BC, xt[i][:], f"xt{i}", 2, 3, hb[i][:], f"hb{i}", ss, rs, sqj)
                    transpose_to(hb[i], f"hb{i}", hcT, "hcT", j * 128, ptr, "ptr")
                S.barrier()
                S.emit()
                if cfg.stop == 1:
                    finish()
                    return nc

            NTC = NT + CTXL
            NCHC = NTC // 64
            NCB = CTXL // 128
            with ExitStack() as phA:
                wts = [[sb(phA, f"w{n}{i}", [128, 8, 128], BF16) for n in range(4)] for i in range(2)]
                qs = sb(phA, "qs", [128, NT], F32)
                A1 = sb(phA, "A1", [128, NTC], F32)
                A2 = sb(phA, "A2", [128, NTC], F32)
                A3 = sb(phA, "A3", [128, NTC], F32)
                A4 = sb(phA, "A4", [128, NTC], F32)
                A5 = sb(phA, "A5", [128, NTC], F32)
                qd = [sb(phA, f"qd{d}", [128, NT], BF16) for d in range(2)]
                kd = [sb(phA, f"kd{d}", [128, NT], BF16) for d in range(2)]
                kd2 = [sb(phA, f"kd2{d}", [128, NTC], BF16) for d in range(2)]
                qsg = [sb(phA, f"qsg{d}", [128, NT], BF16) for d in range(2)]
                bse = sb(phA, "bse", [128, NCHC], F32)
                lastc = [sb(phA, f"lastc{d}", [128, NCHC], F32) for d in range(2)]
                elast = [sb(phA, f"elast{d}", [128, NCHC], F32) for d in range(2)]
                vh = sb(phA, "vh", [128, NB + NCB, 128], BF16)
                oloc = sb(phA, "oloc", [128, NB, 128], F32)
                S32 = [sb(phA, f"S32{d}", [128, 128], F32) for d in range(2)]
                Sbf = [sb(phA, f"Sbf{d}", [128, 128], BF16) for d in range(2)]
                ATm = [sb(phA, f"ATm{d}", [128, 128], BF16) for d in range(2)]
                k2T = [sb(phA, f"k2T{d}", [128, 128], BF16) for d in range(2)]
                stt_t = sb(phA, "stt_t", [128, NH * 258], F32)
                pA = [ps(phA, f"pA{d}", [128, 512], F32) for d in range(2)]
                pO = [ps(phA, f"pO{d}", [128, 512], F32) for d in range(2)]
                pS = [ps(phA, f"pS{d}", [128, 512], F32) for d in range(2)]
                pAt = [pA[d][:, 256:384].bitcast(BF16) for d in range(2)]
                ones_b = C("ones", 0, 1)

                def featproj(w, wkey, dst_fn):
                    tiles = [(hT, "hT", t0, min(512, NT - t0), t0) for t0 in range(0, NT, 512)]
                    tiles += [(hcT, "hcT", 0, CTXL, NT)]
                    for ti, (src, skey, t0, n, o0) in enumerate(tiles):
                        pp = pb[ti % 2]
                        for kc in range(8):
                            S.op("tensor", k_mm(pp[:, 0:n], w[:, kc, :], src[:, kc, t0:t0 + n], kc == 0, kc == 7),
                                 reads=[wkey, skey], writes=[f"pb{ti % 2}"])
                        dst_fn(pp[:, 0:n], f"pb{ti % 2}", o0, n)

                for h in range(NH):
                    wi = h % 2
                    w5 = wts[wi]
                    for n in range(4):
                        S.dma("gpsimd", k_dma(w5[n][:], hgwin_in[:, n * D + h * 128:n * D + (h + 1) * 128]
                                              .rearrange("(kc p) n -> p kc n", p=128)), writes=[f"w{n}{wi}"])
                    lb_h = lbt[:, h:h + 1]
                    oml_h = lbt[:, 8 + h:9 + h]

                    def q_dst(pp, pkey, o0, n):
                        if o0 < NT:
                            S.op("scalar", k_act(qs[:, o0:o0 + n], pp, AF.Copy, scale=128.0 ** -0.5), reads=[pkey], writes=["qs"])
                    featproj(w5[0], f"w0{wi}", q_dst)
                    for jb in range(NB + NCB):
                        src, skey, c0 = (hT, "hT", jb * 128) if jb < NB else (hcT, "hcT", (jb - NB) * 128)
                        pp = pb[jb % 2]
                        for kc in range(8):
                            S.op("tensor", k_mm(pp[:, 0:128], src[:, kc, c0:c0 + 128], w5[1][:, kc, :], kc == 0, kc == 7),
                                 reads=[skey, f"w1{wi}"], writes=[f"pb{jb % 2}"])
                        S.op("vector", k_copy(vh[:, jb, :], pp[:, 0:128]), reads=[f"pb{jb % 2}"], writes=["vh"])
                    P3 = A4[:].rearrange("p (c j) -> p c j", j=64)
                    C3 = A5[:].rearrange("p (c j) -> p c j", j=64)
                    E3 = A1[:].rearrange("p (c j) -> p c j", j=64)
                    for d in range(2):
                        def z_dst(pp, pkey, o0, n):
                            S.op("scalar", k_act(A1[:, o0:o0 + n], pp, AF.Sigmoid), reads=[pkey], writes=["A1"])
                        featproj(w5[2 + d], f"w{2 + d}{wi}", z_dst)
                        S.op("vector", k_ts(A1[:], A1[:], oml_h, lb_h, ALU.mult, ALU.add), reads=["A1", "lbt"], writes=["A1"])
                        S.op("scalar", k_act(A2[:], A1[:], AF.Ln), reads=["A1"], writes=["A2"])
                        S.op("gpsimd", k_ts(A3[:], A1[:], -1.0, 1.0, ALU.mult, ALU.add), reads=["A1"], writes=["A3"])
                        S.op("vector", k_scan(A4[:, 0:NT], ones_b.to_broadcast([128, NT]), A2[:, 0:NT], 0.0, ALU.mult, ALU.add),
                             reads=["A2", "cst"], writes=["A4"])
                        S.op("vector", k_scan(A4[:, NT:NTC], ones_b.to_broadcast([128, CTXL]), A2[:, NT:NTC], 0.0, ALU.mult, ALU.add),
                             reads=["A2", "cst"], writes=["A4"])
                        if d == 0:
                            S.op("vector", k_copy(bse[:, 1:NCHC], A4[:, 63:NTC - 1:64]), reads=["A4"], writes=["bse"])
                            S.op("vector", k_memset(bse[:, 0:1], 0.0), writes=["bse"])
                            S.op("vector", k_memset(bse[:, NCH:NCH + 1], 0.0), writes=["bse"])
                            S.op("vector", k_tt(C3, P3, bse[:].unsqueeze(2).to_broadcast([128, NCHC, 64]), ALU.subtract),
                                 reads=["A4", "bse"], writes=["A5"])
                            S.op("vector", k_copy(lastc[d][:], A5[:, 63:NTC:64]), reads=["A5"], writes=[f"lastc{d}"])
                            S.op("scalar", k_act(A1[:, 0:NT], A4[:, 0:NT], AF.Exp), reads=["A4", "A3", "A2"], writes=["A1"])
                        else:
                            S.op("vector", k_copy(bse[:], A4[:, 63:NTC:64]), reads=["A4"], writes=["bse"])
                            S.op("vector", k_tt(A1[:], A2[:], A4[:], ALU.subtract), reads=["A2", "A4", "A3"], writes=["A1"])
                            S.op("vector", k_tt(C3, E3, bse[:].unsqueeze(2).to_broadcast([128, NCHC, 64]), ALU.add),
                                 reads=["A1", "bse"], writes=["A5"])
                            S.op("vector", k_copy(lastc[d][:], A5[:, 0:NTC:64]), reads=["A5"], writes=[f"lastc{d}"])
                            S.op("scalar", k_act(A1[:, 0:NT], A1[:, 0:NT], AF.Exp, bias=A4[:, NT - 1:NT]), reads=["A1", "A4"], writes=["A1"])
                        S.op("scalar", k_act(elast[d][:], lastc[d][:], AF.Exp), reads=[f"lastc{d}"], writes=[f"elast{d}"])
                        dcol = h * 258 + 256 + d
                        S.op("vector", k_tt(qsg[d][:], qs[:], A1[:, 0:NT], ALU.mult), reads=["qs", "A1"], writes=[f"qsg{d}"])
                        S.dma("sync", k_dma(qseg_scr[h, d], qsg[d][:]), reads=[f"qsg{d}"], writes=["qseg_scr"])
                        S.op("scalar", k_act(stt_t[:, dcol:dcol + 1], A4[:, NT - 1:NT], AF.Exp), reads=["A4"], writes=["stt_t"])
                        S.op("scalar", k_act(A1[:, 0:NT], A5[:, 0:NT], AF.Exp), reads=["A5", f"qsg{d}"], writes=["A1"])
                        S.op("vector", k_tt(qd[d][:], qs[:], A1[:, 0:NT], ALU.mult), reads=["qs", "A1"], writes=[f"qd{d}"])
                        S.op("scalar", k_act(A1[:, 0:NT], A5[:, 0:NT], AF.Exp, scale=-1.0), reads=["A5", f"qd{d}"], writes=["A1"])
                        S.op("gpsimd", k_tt(kd[d][:], A3[:, 0:NT], A1[:, 0:NT], ALU.mult), reads=["A3", "A1"], writes=[f"kd{d}"])
                        S.op("vector", k_tt(E3, lastc[d][:].unsqueeze(2).to_broadcast([128, NCHC, 64]), C3, ALU.subtract),
                             reads=[f"lastc{d}", "A5", f"kd{d}"], writes=["A1"])
                        S.op("scalar", k_act(A1[:], A1[:], AF.Exp), reads=["A1"], writes=["A1"])
                        S.op("vector", k_tt(kd2[d][:], A3[:], A1[:], ALU.mult), reads=["A3", "A1"], writes=[f"kd2{d}"])

                    def state_step(d, vblk, c, chunk_idx):
                        S.op("tensor", k_mm(pS[d][:, 0:128], k2T[d][c * 64:(c + 1) * 64, :], vh[c * 64:(c + 1) * 64, vblk, :], True, True),
                             reads=[f"k2T{d}", "vh"], writes=[f"pS{d}"])
                        S.op("vector", k_stt(Sbf[d][:], S32[d][:], elast[d][:, chunk_idx:chunk_idx + 1], pS[d][:, 0:128], ALU.mult, ALU.add),
                             reads=[f"S32{d}", f"elast{d}", f"pS{d}"], writes=[f"Sbf{d}"])
                        S.op("vector", k_stt(S32[d][:], S32[d][:], elast[d][:, chunk_idx:chunk_idx + 1], pS[d][:, 0:128], ALU.mult, ALU.add),
                             reads=[f"S32{d}", f"elast{d}", f"pS{d}"], writes=[f"S32{d}"])

                    def k2_transpose(d, col0):
                        S.op("tensor", k_tr(pAt[d][:, 0:128], kd2[d][:, col0:col0 + 128], identb[:]),
                             reads=[f"kd2{d}", "identb"], writes=[f"pAt{d}"])
                        S.op("scalar", k_act(k2T[d][:], pAt[d][:, 0:128], AF.Copy), reads=[f"pAt{d}"], writes=[f"k2T{d}"])

                    for d in range(2):
                        S.op("gpsimd", k_memset(S32[d][:], 0.0), writes=[f"S32{d}"])
                        S.op("gpsimd", k_memset(Sbf[d][:], 0.0), writes=[f"Sbf{d}"])
                    S.op("gpsimd", k_memset(oloc[:], 0.0), writes=["oloc"])
                    for step in range(NCB):
                        for d in range(2):
                            cb = step if d == 0 else NCB - 1 - step
                            k2_transpose(d, NT + cb * 128)
                            for c in ([0, 1] if d == 0 else [1, 0]):
                                state_step(d, NB + cb, c, NCH + cb * 2 + c)
                    for d in range(2):
                        S.op("vector", k_copy(cstate[d][:, h, :], S32[d][:]), reads=[f"S32{d}"], writes=[f"cstate{d}"])
                        S.op("gpsimd", k_memset(S32[d][:], 0.0), reads=[f"cstate{d}"], writes=[f"S32{d}"])
                        S.op("gpsimd", k_memset(Sbf[d][:], 0.0), writes=[f"Sbf{d}"])
                    for step in range(NB):
                        for d in range(2):
                            jb = step if d == 0 else NB - 1 - step
                            corder = [0, 1] if d == 0 else [1, 0]
                            mask = C("maskf") if d == 0 else C("maskb")
                            col0 = jb * 128
                            S.op("tensor", k_mm(pA[d][:, 0:128], kd[d][:, col0:col0 + 128], qd[d][:, col0:col0 + 128], True, True),
                                 reads=[f"kd{d}", f"qd{d}"], writes=[f"pA{d}"])
                            S.op("vector", k_tt(ATm[d][:], pA[d][:, 0:128], mask, ALU.mult), reads=[f"pA{d}", "cst"], writes=[f"ATm{d}"])
                            k2_transpose(d, col0)
                            S.op("tensor", k_mm(pO[d][:, 0:128], ATm[d][:], vh[:, jb, :], True, False),
                                 reads=[f"ATm{d}", "vh"], writes=[f"pO{d}"])
                            for ci, c in enumerate(corder):
                                S.op("tensor", k_mm(pO[d][c * 64:(c + 1) * 64, 0:128], qd[d][:, col0 + c * 64:col0 + (c + 1) * 64],
                                                    Sbf[d][:], False, ci == 1),
                                     reads=[f"qd{d}", f"Sbf{d}"], writes=[f"pO{d}"])
                                state_step(d, jb, c, jb * 2 + c)
                            S.op("vector", k_tt(oloc[:, jb, :], oloc[:, jb, :], pO[d][:, 0:128], ALU.add),
                                 reads=[f"pO{d}", "oloc"], writes=["oloc"])
                    for d in range(2):
                        S.op("vector", k_copy(stt_t[:, h * 258 + d * 128:h * 258 + (d + 1) * 128], S32[d][:]),
                             reads=[f"S32{d}"], writes=["stt_t"])
                    S.dma("sync", k_dma(o_scr[h], oloc[:].rearrange("p b v -> p (b v)")), reads=["oloc"], writes=["o_scr"])
                S.dma("sync", k_dma(st_loc, stt_t[:]), reads=["stt_t"], writes=["st_loc"])
                S.coll(k_ag(st_loc, st_all), reads=["st_loc"], writes=["st_all"])
                S.barrier()
                S.emit()
                if cfg.stop == 2:
                    finish()
                    return nc

            with ExitStack() as phB:
                BC = sb(phB, "BC", [128, 1, D], F32)
                ptr = ps(phB, "ptr", [128, 1024], BF16)
                pO = ps(phB, "pOb", [128, 512], F32)
                pG = ps(phB, "pGb", [128, 512], F32)
                go = sb(phB, "go", [128, NB, D], BF16)
                stA = [sb(phB, f"stA{i}", [128, 8, 258], F32) for i in range(2)]
                Sin = [sb(phB, f"Sin{d}", [128, 128], F32) for d in range(2)]
                Sinb = [sb(phB, f"Sinb{d}", [128, 128], BF16) for d in range(2)]
                acol = sb(phB, "acol", [128, 1], F32)
                qsgL = [[sb(phB, f"qsgL{i}{d}", [128, NT], BF16) for d in range(2)] for i in range(2)]
                olocL = [sb(phB, f"olocL{i}", [128, NB * 128], F32) for i in range(2)]
                wgh = [sb(phB, f"wgh{i}", [128, 8, 128], BF16) for i in range(2)]
                ot = sb(phB, "ot", [128, 128], F32)
                oj = sb(phB, "oj", [128, 128], F32)
                sg = sb(phB, "sg", [128, 128], F32)
                ss = sb(phB, "ssb", [128, 1], F32)
                rs = sb(phB, "rsb", [128, 2], F32)
                wo = sb(phB, "wo", [128, 8, D], BF16)
                xt = [sb(phB, f"xtb{i}", [128, D], F32) for i in range(2)]
                tmpy = sb(phB, "tmpy", [128, 512], F32)
                bcast_rows(BC, pb, ada(0, 2), 0, "ADAo")
                S.dma("gpsimd", k_dma(wo[:], hgwout_in.rearrange("(kc p) n -> p kc n", p=128)), writes=["wo"])
                for h in range(NH):
                    i = h % 2
                    S.dma("sync", k_dma(stA[i][:], st_all[:, h * 258:(h + 1) * 258].rearrange("(r p) n -> p r n", p=128)),
                          reads=["st_all"], writes=[f"stA{i}"])
                    S.dma("gpsimd", k_dma(wgh[i][:], hgwin_in[:, 4 * D + h * 128:4 * D + (h + 1) * 128]
                                          .rearrange("(kc p) n -> p kc n", p=128)), writes=[f"wgh{i}"])
                    for d in range(2):
                        S.dma("sync", k_dma(qsgL[i][d][:], qseg_scr[h, d]), reads=["qseg_scr"], writes=[f"qsgL{i}{d}"])
                    S.dma("sync", k_dma(olocL[i][:], o_scr[h]), reads=["o_scr"], writes=[f"olocL{i}"])
                    for d in range(2):
                        mname = "mf" if d == 0 else "mb"
                        S.op("vector", k_copy(Sin[d][:], cstate[d][:, h, :]), reads=[f"cstate{d}"], writes=[f"Sin{d}"])
                        for r in (range(8) if d == 0 else range(7, -1, -1)):
                            mcol = C(mname, r, 1)
                            S.op("vector", k_ts(acol[:], stA[i][:, r, 256 + d:257 + d], -1.0, mcol, ALU.add, ALU.mult),
                                 reads=[f"stA{i}", "cst"], writes=["acol"])
                            S.op("vector", k_ts(acol[:], acol[:], 1.0, None, ALU.add), reads=["acol"], writes=["acol"])
                            S.op("vector", k_ts(Sin[d][:], Sin[d][:], acol[:, 0:1], None, ALU.mult), reads=["acol", f"Sin{d}"], writes=[f"Sin{d}"])
                            S.op("vector", k_stt(Sin[d][:], stA[i][:, r, d * 128:(d + 1) * 128], mcol, Sin[d][:], ALU.mult, ALU.add),
                                 reads=[f"stA{i}", "cst", f"Sin{d}"], writes=[f"Sin{d}"])
                        S.op("scalar", k_act(Sinb[d][:], Sin[d][:], AF.Copy), reads=[f"Sin{d}"], writes=[f"Sinb{d}"])
                    for jb in range(NB):
                        blk = slice(jb * 128, (jb + 1) * 128)
                        S.op("tensor", k_mm(pO[:, 0:128], qsgL[i][0][:, blk], Sinb[0][:], True, False),
                             reads=[f"qsgL{i}0", "Sinb0"], writes=["pOb"])
                        S.op("tensor", k_mm(pO[:, 0:128], qsgL[i][1][:, blk], Sinb[1][:], False, True),
                             reads=[f"qsgL{i}1", "Sinb1"], writes=["pOb"])
                        S.op("vector", k_tt(ot[:], olocL[i][:, blk], pO[:, 0:128], ALU.add), reads=[f"olocL{i}", "pOb"], writes=["ot"])
                        if cfg.debug:
                            S.dma("sync", k_dma(dbg["d_o"][blk, h * 128:(h + 1) * 128], ot[:]), reads=["ot"], writes=["d_o"])
                        S.op("scalar", k_act(oj[:], ot[:], AF.Square, accum=ss[:, 0:1]), reads=["ot"], writes=["oj", "ssb"])
                        S.op("scalar", k_act(rs[:, 0:1], ss[:, 0:1], AF.Sqrt, scale=1.0 / 128, bias=epsb[:, 0:1]),
                             reads=["ssb", "epsb"], writes=["rsb"])
                        S.op("vector", k_recip(rs[:, 1:2], rs[:, 0:1]), reads=["rsb"], writes=["rsb2"])
                        S.op("vector", k_stt(oj[:], ot[:], rs[:, 1:2], C("hgn"), ALU.mult, ALU.mult), reads=["ot", "rsb2", "cst"], writes=["oj"])
                        for kc in range(8):
                            S.op("tensor", k_mm(pG[:, 0:128], hT[:, kc, blk], wgh[i][:, kc, :], kc == 0, kc == 7),
                                 reads=["hT", f"wgh{i}"], writes=["pGb"])
                        S.op("scalar", k_act(sg[:], pG[:, 0:128], AF.Silu), reads=["pGb"], writes=["sg"])
                        S.op("vector", k_tt(go[:, jb, h * 128:(h + 1) * 128], oj[:], sg[:], ALU.mult), reads=["oj", "sg"], writes=["go"])
                for jb in range(NB):
                    transpose_to(go[:, jb, :], "go", hT, "hT", jb * 128, ptr, "ptr")
                for jb in range(NB):
                    i = jb % 2
                    blk = slice(jb * 128, (jb + 1) * 128)
                    S.dma("sync", k_dma(xt[i][:], xres[blk, :]), reads=["xres"], writes=[f"xtb{i}"])
                    for hf in range(2):
                        for kc in range(8):
                            S.op("tensor", k_mm(pb[hf][:], hT[:, kc, blk], wo[:, kc, hf * 512:(hf + 1) * 512], kc == 0, kc == 7),
                                 reads=["hT", "wo"], writes=[f"pb{hf}"])
                        S.op("vector", k_tt(tmpy[:], pb[hf][:], BC[:, 0, hf * 512:(hf + 1) * 512], ALU.mult),
                             reads=[f"pb{hf}", "BC0"], writes=["tmpy"])
                        S.op("vector", k_tt(xt[i][:, hf * 512:(hf + 1) * 512], xt[i][:, hf * 512:(hf + 1) * 512], tmpy[:], ALU.add),
                             reads=["tmpy", f"xtb{i}"], writes=[f"xtb{i}"])
                    S.dma("sync", k_dma(xres[blk, :], xt[i][:]), reads=[f"xtb{i}"], writes=["xres"])
                    if cfg.debug:
                        S.dma("sync", k_dma(dbg["d_xmix0"][blk, :], xt[i][:]), reads=[f"xtb{i}"], writes=["d_xmix0"])
                S.barrier()
                S.emit()
                if cfg.stop == 3:
                    finish()
                    return nc
        def moe_layer(l, stop_base, dbg_name):
            with ExitStack() as ph:
                BC = sb(ph, "BCm", [128, 2, D], F32)
                pb = [ps(ph, f"pbm{i}", [128, 512], F32) for i in range(2)]
                pL = ps(ph, "pL", [128, 512], F32)
                xt = [sb(ph, f"xtm{i}", [128, D], F32) for i in range(2)]
                h2f = sb(ph, "h2f", [128, D], F32)
                rowt = [sb(ph, f"rowt{i}", [128, ROWW], BF16) for i in range(2)]
                h2T = sb(ph, "h2T", [128, 8, 128], F32)
                wr = sb(ph, "wr", [128, 8, NE], F32)
                sqj = sb(ph, "sqjm", [128, D], F32)
                ss = sb(ph, "ssm", [128, 1], F32)
                rs = sb(ph, "rsm", [128, 2], F32)
                mx = sb(ph, "mx", [128, 2], F32)
                sm = sb(ph, "sm", [128, 2], F32)
                ex = sb(ph, "ex", [128, NE], F32)
                aff = sb(ph, "aff", [128, NE], F32)
                affT = sb(ph, "affT", [NE, NT], F32)
                G2 = make_gain(ph, f"G2c{l}", ada(l, 4), C("nffn", l * 8, 8), ["ADAo", "cst"])
                bcast_rows(BC, pb, G2, 0, f"G2c{l}")
                bcast_rows(BC, pb, ada(l, 3), 1, "ADAo")
                S.dma("sync", k_dma(wr[:], router_in[l].rearrange("(kc p) e -> p kc e", p=128)), writes=["wr"])
                for j in range(NB):
                    i = j % 2
                    blk = slice(j * 128, (j + 1) * 128)
                    S.dma("sync", k_dma(xt[i][:], xres[blk, :]), reads=["xres"], writes=[f"xtm{i}"])
                    prenorm(BC, xt[i][:], f"xtm{i}", 0, 1, h2f[:], "h2f", ss, rs, sqj)
                    S.op("scalar", k_act(rowt[i][:, 0:D], h2f[:], AF.Copy), reads=["h2f"], writes=[f"rowt{i}"])
                    for kc in range(8):
                        S.op("tensor", k_tr(pb[kc // 4][:, (kc % 4) * 128:(kc % 4 + 1) * 128], h2f[:, kc * 128:(kc + 1) * 128], ident),
                             reads=["h2f", "cst"], writes=[f"pbm{kc // 4}"])
                    for hf in range(2):
                        S.op("scalar", k_act(h2T[:, hf * 4:(hf + 1) * 4, :], pb[hf][:].rearrange("p (k n) -> p k n", k=4), AF.Copy),
                             reads=[f"pbm{hf}"], writes=["h2T"])
                    for kc in range(8):
                        S.op("tensor", k_mm(pL[:, 0:NE], h2T[:, kc, :], wr[:, kc, :], kc == 0, kc == 7),
                             reads=["h2T", "wr"], writes=["pL"])
                    S.op("vector", lambda e: e.reduce_max(out=mx[:, 0:1], in_=pL[:, 0:NE], axis=AX.X), reads=["pL"], writes=["mx"])
                    S.op("vector", k_ts(mx[:, 1:2], mx[:, 0:1], -1.0, None, ALU.mult), reads=["mx"], writes=["mx2"])
                    S.op("scalar", k_act(ex[:], pL[:, 0:NE], AF.Exp, bias=mx[:, 1:2], accum=sm[:, 0:1]), reads=["pL", "mx2"], writes=["ex", "sm"])
                    S.op("vector", k_recip(sm[:, 1:2], sm[:, 0:1]), reads=["sm"], writes=["sm2"])
                    S.op("vector", k_ts(aff[:], ex[:], sm[:, 1:2], None, ALU.mult), reads=["ex", "sm2"], writes=["aff"])
                    S.op("vector", k_copy(rowt[i][:, D:ROWW].bitcast(F32), aff[:]), reads=["aff"], writes=[f"rowt{i}"])
                    S.dma("sync", k_dma(h2_loc[l][blk, :], rowt[i][:]), reads=[f"rowt{i}"], writes=["h2_loc"])
                    S.op("tensor", k_tr(pL[0:NE, 256:384], aff[:], ident), reads=["aff", "cst"], writes=["pLt"])
                    S.op("scalar", k_act(affT[:, blk], pL[0:NE, 256:384], AF.Copy), reads=["pLt"], writes=["affT"])
                S.dma("sync", k_dma(aff_loc[l], affT[:]), reads=["affT"], writes=["aff_loc"])
                if cfg.debug and l == 0:
                    S.dma("sync", k_dma(dbg["d_aff"], affT[:]), reads=["affT"], writes=["d_aff"])
                S.coll(k_ag(aff_loc[l], aff_all[l]), reads=["aff_loc"], writes=["aff_all"])
                S.barrier()
                S.emit()
                if cfg.stop == stop_base:
                    finish()
                    return True

            with ExitStack() as phm:
                destSel = sb(phm, "destSel", [128, NB, 36], I32)
                selm = sb(phm, "selm", [128, NB, 16], F32)
                with ExitStack() as ph:
                    A = sb(ph, "A", [128, NT], F32)
                    junk = sb(ph, "junk", [128, NT], BF16)
                    mk = sb(ph, "mk", [128, NT], F32)
                    inc = sb(ph, "inc", [128, NT], F32)
                    bs_ = sb(ph, "bs_", [128, 8], F32)
                    cnt = sb(ph, "cnt", [128, 2], F32)
                    dsf = sb(ph, "dsf", [128, 32], F32)
                    pt_ = ps(ph, "pt_", [128, 512], F32)
                    pD = ps(ph, "pD", [128, 512], F32)
                    lo, hi, mid, ge, d1, offm = (bs_[:, k:k + 1] for k in range(6))
                    S.dma("sync", k_dma(A[:], aff_all[l]), reads=["aff_all"], writes=["A"])
                    S.coll(k_ag(h2_loc[l], h2_all[l]), reads=[], writes=["h2_all"])
                    S.op("vector", k_memset(cnt[:], 0.0), writes=["cnt"])
                    S.op("vector", k_memset(bs_[:, 0:1], 0.0), writes=["bs"])
                    S.op("vector", k_memset(bs_[:, 1:2], 1.0), writes=["bs"])
                    S.op("vector", k_memset(bs_[:, 2:3], 0.5), writes=["bs"])
                    for it in range(32):
                        S.op("vector", k_ts(junk[:], A[:], mid, None, ALU.is_ge, ALU.add, accum=cnt[:, 0:1]), reads=["A", "bs"], writes=["junk", "cnt"])
                        S.op("tensor", k_mm(pt_[:, 0:2], C("Bm"), cnt[:, 0:2], True, True), reads=["cnt", "cst"], writes=["pt_"])
                        S.op("vector", k_ts(ge, pt_[:, 0:1], float(CAP), None, ALU.is_ge), reads=["pt_"], writes=["bs"])
                        S.op("vector", k_tt(d1, mid, lo, ALU.subtract), reads=["bs"], writes=["bs"])
                        S.op("vector", k_stt(lo, d1, ge, lo, ALU.mult, ALU.add), reads=["bs"], writes=["bs"])
                        S.op("vector", k_tt(d1, hi, mid, ALU.subtract), reads=["bs"], writes=["bs"])
                        S.op("vector", k_stt(hi, d1, ge, mid, ALU.mult, ALU.add), reads=["bs"], writes=["bs"])
                        S.op("vector", k_tt(mid, lo, hi, ALU.add), reads=["bs"], writes=["bs"])
                        S.op("vector", k_ts(mid, mid, 0.5, None, ALU.mult), reads=["bs"], writes=["bs"])
                    S.op("vector", k_ts(mk[:], A[:], lo, None, ALU.is_ge), reads=["A", "bs"], writes=["mk"])
                    S.op("vector", k_scan(inc[:], C("ones", 0, 1).to_broadcast([128, NT]), mk[:], 0.0, ALU.mult, ALU.add),
                         reads=["mk", "cst"], writes=["inc"])
                    S.op("vector", k_copy(cnt[:, 0:1], inc[:, NT - 1:NT]), reads=["inc"], writes=["cnt"])
                    S.op("tensor", k_mm(pt_[:, 0:2], C("Tm"), cnt[:, 0:2], True, True), reads=["cnt", "cst"], writes=["pt_"])
                    S.op("vector", k_ts(offm, pt_[:, 0:1], -(1.0 + BIG), None, ALU.add), reads=["pt_"], writes=["bs"])
                    S.op("vector", k_ts(inc[:], inc[:], offm, None, ALU.add), reads=["inc", "bs"], writes=["inc"])
                    S.op("vector", k_tt(inc[:], inc[:], mk[:], ALU.mult), reads=["inc", "mk"], writes=["inc"])
                    S.op("vector", k_ts(inc[:], inc[:], BIG, None, ALU.add), reads=["inc"], writes=["inc"])
                    for j in range(NB):
                        S.op("tensor", k_mm(pD[:, 0:32], inc[:, j * 128:(j + 1) * 128], C("sel"), True, True),
                             reads=["inc", "cst"], writes=["pD"])
                        S.op("vector", k_ts(selm[:, j, :], pD[:, 16:32], BIG - 0.5, None, ALU.is_lt), reads=["pD"], writes=["selm"])
                        S.op("vector", k_tt(dsf[:], pD[:, 0:32], C("retbase"), ALU.add), reads=["pD", "cst"], writes=["dsf"])
                        S.op("vector", k_copy(destSel[:, j, 0:32], dsf[:]), reads=["dsf"], writes=["destSel"])
                        if cfg.debug and l == 0:
                            S.dma("sync", k_dma(dbg["d_dest"][:, j * 32:(j + 1) * 32], dsf[:]), reads=["dsf"], writes=["d_dest"])
                    S.barrier()
                    S.emit()
                    if cfg.stop == stop_base + 1:
                        finish()
                        return True

                with ExitStack() as ph:
                    ptr = ps(ph, "ptre", [128, 1024], BF16)
                    pa = [ps(ph, f"pa{i}", [128, 512], F32) for i in range(2)]
                    pu = [ps(ph, f"pu{i}", [128, 512], F32) for i in range(2)]
                    py = [ps(ph, f"py{i}", [128, 512], F32) for i in range(2)]
                    rw = [sb(ph, f"rw{i}", [128, ROWW], BF16) for i in range(6)]
                    xr = [sb(ph, f"xr{i}", [128, ROWW], BF16) for i in range(2)]
                    xgT = [sb(ph, f"xgT{bb}", [128, 8, CAP], BF16) for bb in range(2)]
                    actT = [sb(ph, f"actT{bb}", [128, NFC, CAP], BF16) for bb in range(2)]
                    wdt = sb(ph, "wdt", [128, NFC, D], BF16)
                    wgf = [sb(ph, f"wgf{i}", [128, 8, 128], BF16) for i in range(3)]
                    wuf = [sb(ph, f"wuf{i}", [128, 8, 128], BF16) for i in range(3)]
                    gates = sb(ph, "gates", [128, 2, NSB], F32)
                    t16 = sb(ph, "t16", [128, NE], F32)
                    sa = [sb(ph, f"sa{i}", [128, 512], F32) for i in range(2)]
                    yrow = [sb(ph, f"yrow{i}", [128, D], BF16) for i in range(2)]
                    n_rw = 0
                    for bb in range(2):
                        for g in range(4):
                            for j in range(NB):
                                i = n_rw % 6
                                n_rw += 1
                                r0 = (bb * 4 + g) * NT + j * 128
                                S.dma("sync", k_dma(rw[i][:], h2_all[l][r0:r0 + 128, :]), reads=["h2_all"], writes=[f"rw{i}"])
                                for el in range(2):
                                    col = bb * 8 + g * 2 + el
                                    S.dma("gpsimd", k_scatter(xg[l][bb * 2 + el], destSel[:, j, col:col + 1], rw[i][:], CAPP - 1),
                                          reads=[f"rw{i}", "destSel"], writes=[f"xg{bb * 2 + el}_{g}_{j}"])
                    NST = max(1, CAP // 512)
                    NN = min(512, CAP)
                    for el in range(2):
                        S.dma("gpsimd", k_dma(wdt[:], wd_in[l, el].rearrange("(fc p) d -> p fc d", p=128)), writes=["wdt"])
                        for bb in range(2):
                            for sbk in range(NSB):
                                i = sbk % 2
                                S.dma("sync", k_dma(xr[i][:], xg[l][bb * 2 + el][sbk * 128:(sbk + 1) * 128, :]),
                                      reads=[f"xg{bb * 2 + el}_{g_}_{j_}" for g_ in range(4) for j_ in range(NB)], writes=[f"xr{i}"])
                                S.op("vector", k_tt(t16[:], xr[i][:, D:ROWW].bitcast(F32), C("gsel", el * 16, 16), ALU.mult),
                                     reads=[f"xr{i}", "cst"], writes=["t16"])
                                S.op("vector", lambda e, o=gates[:, bb, sbk:sbk + 1], a=t16[:]: e.reduce_sum(out=o, in_=a, axis=AX.X),
                                     reads=["t16"], writes=["gates"])
                                transpose_to(xr[i], f"xr{i}", xgT[bb], f"xgT{bb}", sbk * 128, ptr, "ptre", eng="vector")
                        for fc in range(NFC):
                            wi = fc % 3
                            S.dma("gpsimd", k_dma(wgf[wi][:], wg_in[l, el][:, fc * 128:(fc + 1) * 128].rearrange("(kc p) f -> p kc f", p=128)),
                                  writes=[f"wgf{wi}"])
                            S.dma("gpsimd", k_dma(wuf[wi][:], wu_in[l, el][:, fc * 128:(fc + 1) * 128].rearrange("(kc p) f -> p kc f", p=128)),
                                  writes=[f"wuf{wi}"])
                            for bb in range(2):
                                for st in range(NST):
                                    pi = (bb * NST + st) % 2
                                    cols = slice(st * NN, (st + 1) * NN)
                                    for kc in range(8):
                                        S.op("tensor", k_mm(pa[pi][:, 0:NN], wgf[wi][:, kc, :], xgT[bb][:, kc, cols], kc == 0, kc == 7),
                                             reads=[f"wgf{wi}", f"xgT{bb}"], writes=[f"pa{pi}"])
                                    for kc in range(8):
                                        S.op("tensor", k_mm(pu[pi][:, 0:NN], wuf[wi][:, kc, :], xgT[bb][:, kc, cols], kc == 0, kc == 7),
                                             reads=[f"wuf{wi}", f"xgT{bb}"], writes=[f"pu{pi}"])
                                    S.op("scalar", k_act(sa[pi][:, 0:NN], pa[pi][:, 0:NN], AF.Silu), reads=[f"pa{pi}"], writes=[f"sa{pi}"])
                                    S.op("vector", k_tt(actT[bb][:, fc, cols], sa[pi][:, 0:NN], pu[pi][:, 0:NN], ALU.mult),
                                         reads=[f"sa{pi}", f"pu{pi}"], writes=[f"actT{bb}"])
                        for bb in range(2):
                            for sbk in range(NSB):
                                yi = sbk % 2
                                for hf in range(2):
                                    for fc in range(NFC):
                                        S.op("tensor", k_mm(py[hf][:], actT[bb][:, fc, sbk * 128:(sbk + 1) * 128], wdt[:, fc, hf * 512:(hf + 1) * 512],
                                                            fc == 0, fc == NFC - 1), reads=[f"actT{bb}", "wdt"], writes=[f"py{hf}"])
                                    S.op("scalar", k_act(yrow[yi][:, hf * 512:(hf + 1) * 512], py[hf][:], AF.Copy, scale=gates[:, bb, sbk:sbk + 1]),
                                         reads=[f"py{hf}", "gates"], writes=[f"yrow{yi}"])
                                r0 = (bb * 2 + el) * CAPP + sbk * 128
                                S.dma("sync", k_dma(y_loc[l][r0:r0 + 128, :], yrow[yi][:]), reads=[f"yrow{yi}"], writes=["y_loc"])
                    S.barrier()
                    S.emit()
                    if cfg.stop == stop_base + 2:
                        finish()
                        return True

                with ExitStack() as ph:
                    BC = sb(ph, "BCr", [128, 1, D], F32)
                    pb = [ps(ph, f"pbr{i}", [128, 512], F32) for i in range(2)]
                    Yt = [sb(ph, f"Yt{e}", [128, D], BF16) for e in range(NE)]
                    dg = [sb(ph, f"dg{i}", [128, 128], BF16) for i in range(4)]
                    xt = [sb(ph, f"xtr{i}", [128, D], F32) for i in range(2)]
                    tmpy = sb(ph, "tmpyr", [128, 512], F32)
                    S.coll(k_ag(y_loc[l], y_all[l]), reads=[], writes=["y_all"])
                    bcast_rows(BC, pb, ada(l, 5), 0, "ADAo")
                    for e in range(NE):
                        S.op("gpsimd", k_memset(Yt[e][:], 0.0), writes=[f"Yt{e}"])
                    nd = 0
                    for j in range(NB):
                        i = j % 2
                        blk = slice(j * 128, (j + 1) * 128)
                        S.dma("sync", k_dma(xt[i][:], xres[blk, :]), reads=["xres"], writes=[f"xtr{i}"])
                        for e in range(NE):
                            S.dma("gpsimd", k_gather(Yt[e][:], y_all[l], destSel[:, j, 16 + e:17 + e], 8 * 4 * CAPP - 1),
                                  reads=["destSel", "y_all"], writes=[f"Yt{e}"])
                            di = nd % 4
                            nd += 1
                            S.op("vector", k_ts(dg[di][:], identb[:], selm[:, j, e:e + 1], None, ALU.mult),
                                 reads=["identb", "selm"], writes=[f"dg{di}"])
                            for hf in range(2):
                                S.op("tensor", k_mm(pb[hf][:], dg[di][:], Yt[e][:, hf * 512:(hf + 1) * 512], e == 0, e == NE - 1),
                                     reads=[f"dg{di}", f"Yt{e}"], writes=[f"pbr{hf}"])
                        for hf in range(2):
                            S.op("vector", k_tt(tmpy[:], pb[hf][:], BC[:, 0, hf * 512:(hf + 1) * 512], ALU.mult),
                                 reads=[f"pbr{hf}", "BC0"], writes=["tmpyr"])
                            S.op("vector", k_tt(xt[i][:, hf * 512:(hf + 1) * 512], xt[i][:, hf * 512:(hf + 1) * 512], tmpy[:], ALU.add),
                                 reads=["tmpyr", f"xtr{i}"], writes=[f"xtr{i}"])
                        S.dma("sync", k_dma(xres[blk, :], xt[i][:]), reads=[f"xtr{i}"], writes=["xres"])
                        if cfg.debug:
                            S.dma("sync", k_dma(dbg[dbg_name][blk, :], xt[i][:]), reads=[f"xtr{i}"], writes=[dbg_name])
                    S.barrier()
                    S.emit()
                    if cfg.stop == stop_base + 3:
                        finish()
                        return True
            return False

        if moe_layer(0, 4, "d_xmoe0"):
            return nc
        with ExitStack() as ph:
            hT = sb(ph, "hTc", [128, 8, NT], BF16)
            zT = sb(ph, "zT", [128, 8, NT], BF16)
            BC = sb(ph, "BCc", [128, 3, D], F32)
            pb = [ps(ph, f"pbc{i}", [128, 512], F32) for i in range(2)]
            ptr = ps(ph, "ptrc", [128, 1024], BF16)
            pc_ = [ps(ph, f"pcc{i}", [128, 512], F32) for i in range(2)]
            pu_ = [ps(ph, f"puc{i}", [128, 512], F32) for i in range(2)]
            xt = [sb(ph, f"xtc{i}", [128, D], F32) for i in range(2)]
            hb = [sb(ph, f"hbc{i}", [128, D], BF16) for i in range(2)]
            sqj = sb(ph, "sqjc", [128, D], F32)
            ss = sb(ph, "ssc", [128, 1], F32)
            rs = sb(ph, "rsc", [128, 2], F32)
            w3 = [[sb(ph, f"w3{n}{i}", [128, 8, 128], BF16) for n in range(3)] for i in range(2)]
            wo = sb(ph, "woc", [128, 8, D], BF16)
            cu2 = sb(ph, "cu2", [128, 16], F32)
            c2s = sb(ph, "c2s", [128, 2], F32)
            hl = sb(ph, "hl", [128, 8, 16], F32)
            t88 = sb(ph, "t88", [128, 8, 8], F32)
            halo = sb(ph, "halo", [128, 2, 8], F32)
            cup = sb(ph, "cup", [128, NT + 2], F32)
            ycv = sb(ph, "ycv", [128, NT], F32)
            cs_ = sb(ph, "cs_", [128, 512], F32)
            tmpy = sb(ph, "tmpyc", [128, 512], F32)
            G1 = make_gain(ph, "G1c1", ada(1, 1), C("nmix", 8, 8), ["ADAo", "cst"])
            bcast_rows(BC, pb, G1, 0, "G1c1")
            bcast_rows(BC, pb, ada(1, 0), 1, "ADAo")
            bcast_rows(BC, pb, ada(1, 2), 2, "ADAo")
            S.dma("gpsimd", k_dma(wo[:], scwout_in.rearrange("(kc p) n -> p kc n", p=128)), writes=["woc"])
            for j in range(NB):
                i = j % 2
                blk = slice(j * 128, (j + 1) * 128)
                S.dma("sync", k_dma(xt[i][:], xres[blk, :]), reads=["xres"], writes=[f"xtc{i}"])
                prenorm(BC, xt[i][:], f"xtc{i}", 0, 1, hb[i][:], f"hbc{i}", ss, rs, sqj)
                transpose_to(hb[i], f"hbc{i}", hT, "hTc", j * 128, ptr, "ptrc")

            def load_w3(dc, i):
                for n in range(3):
                    S.dma("gpsimd", k_dma(w3[i][n][:], scwin_in[:, n * D + dc * 128:n * D + (dc + 1) * 128]
                                          .rearrange("(kc p) n -> p kc n", p=128)), writes=[f"w3{n}{i}"])

            for dc in range(8):
                i = dc % 2
                load_w3(dc, i)
                for kc in range(8):
                    S.op("tensor", k_mm(pc_[0][:, 0:2], w3[i][1][:, kc, :], hT[:, kc, 0:NT:NT - 1], kc == 0, kc == 7),
                         reads=[f"w31{i}", "hTc"], writes=["pcc0"])
                for kc in range(8):
                    S.op("tensor", k_mm(pu_[0][:, 0:2], w3[i][2][:, kc, :], hT[:, kc, 0:NT:NT - 1], kc == 0, kc == 7),
                         reads=[f"w32{i}", "hTc"], writes=["puc0"])
                S.op("scalar", k_act(c2s[:], pc_[0][:, 0:2], AF.Copy), reads=["pcc0"], writes=["c2s"])
                S.op("vector", k_tt(cu2[:, dc * 2:dc * 2 + 2], c2s[:], pu_[0][:, 0:2], ALU.mult), reads=["c2s", "puc0"], writes=["cu2"])
            S.dma("sync", k_dma(halo_loc, cu2[:]), reads=["cu2"], writes=["halo_loc"])
            S.coll(k_ag(halo_loc, halo_all), reads=["halo_loc"], writes=["halo_all"])
            S.dma("sync", k_dma(hl[:], halo_all.rearrange("(r p) n -> p r n", p=128)), reads=["halo_all"], writes=["hl"])
            for side, (sname, off) in enumerate((("selL", 1), ("selR", 0))):
                S.op("vector", k_tt(t88[:], hl[:, :, off:16:2].rearrange("p r c -> p c r"),
                                    C(sname).unsqueeze(1).to_broadcast([128, 8, 8]), ALU.mult), reads=["hl", "cst"], writes=["t88"])
                S.op("vector", lambda e, o=halo[:, side, :], a=t88[:]: e.reduce_sum(out=o, in_=a, axis=AX.X),
                     reads=["t88"], writes=["halo"])
            NN = min(512, NT)
            NST = NT // NN
            for dc in range(8):
                i = dc % 2
                load_w3(dc, i)
                for st in range(NST):
                    cols = slice(st * NN, (st + 1) * NN)
                    pi = st % 2
                    for kc in range(8):
                        S.op("tensor", k_mm(pc_[pi][:, 0:NN], w3[i][1][:, kc, :], hT[:, kc, cols], kc == 0, kc == 7),
                             reads=[f"w31{i}", "hTc"], writes=[f"pcc{pi}"])
                    for kc in range(8):
                        S.op("tensor", k_mm(pu_[pi][:, 0:NN], w3[i][2][:, kc, :], hT[:, kc, cols], kc == 0, kc == 7),
                             reads=[f"w32{i}", "hTc"], writes=[f"puc{pi}"])
                    S.op("scalar", k_act(cs_[:, 0:NN], pc_[pi][:, 0:NN], AF.Copy), reads=[f"pcc{pi}"], writes=["cs_"])
                    S.op("vector", k_tt(cup[:, 1 + st * NN:1 + (st + 1) * NN], cs_[:, 0:NN], pu_[pi][:, 0:NN], ALU.mult),
                         reads=["cs_", f"puc{pi}"], writes=["cup"])
                S.op("vector", k_copy(cup[:, 0:1], halo[:, 0, dc:dc + 1]), reads=["halo"], writes=["cup"])
                S.op("vector", k_copy(cup[:, NT + 1:NT + 2], halo[:, 1, dc:dc + 1]), reads=["halo"], writes=["cup"])
                S.op("vector", k_ts(ycv[:], cup[:, 0:NT], C("scw", 0 * 8 + dc, 1), None, ALU.mult), reads=["cup", "cst"], writes=["ycv"])
                S.op("vector", k_stt(ycv[:], cup[:, 1:NT + 1], C("scw", 1 * 8 + dc, 1), ycv[:], ALU.mult, ALU.add),
                     reads=["cup", "cst", "ycv"], writes=["ycv"])
                S.op("vector", k_stt(ycv[:], cup[:, 2:NT + 2], C("scw", 2 * 8 + dc, 1), ycv[:], ALU.mult, ALU.add),
                     reads=["cup", "cst", "ycv"], writes=["ycv"])
                for st in range(NST):
                    cols = slice(st * NN, (st + 1) * NN)
                    pi = st % 2
                    for kc in range(8):
                        S.op("tensor", k_mm(pb[pi][:, 0:NN], w3[i][0][:, kc, :], hT[:, kc, cols], kc == 0, kc == 7),
                             reads=[f"w30{i}", "hTc"], writes=[f"pbc{pi}"])
                    S.op("vector", k_tt(zT[:, dc, cols], ycv[:, cols], pb[pi][:, 0:NN], ALU.mult), reads=["ycv", f"pbc{pi}"], writes=["zT"])
            for jb in range(NB):
                i = jb % 2
                blk = slice(jb * 128, (jb + 1) * 128)
                S.dma("sync", k_dma(xt[i][:], xres[blk, :]), reads=["xres"], writes=[f"xtc{i}"])
                for hf in range(2):
                    for kc in range(8):
                        S.op("tensor", k_mm(pb[hf][:], zT[:, kc, blk], wo[:, kc, hf * 512:(hf + 1) * 512], kc == 0, kc == 7),
                             reads=["zT", "woc"], writes=[f"pbc{hf}"])
                    S.op("vector", k_tt(tmpy[:], pb[hf][:], BC[:, 2, hf * 512:(hf + 1) * 512], ALU.mult),
                         reads=[f"pbc{hf}", "BC2"], writes=["tmpyc"])
                    S.op("vector", k_tt(xt[i][:, hf * 512:(hf + 1) * 512], xt[i][:, hf * 512:(hf + 1) * 512], tmpy[:], ALU.add),
                         reads=["tmpyc", f"xtc{i}"], writes=[f"xtc{i}"])
                S.dma("sync", k_dma(xres[blk, :], xt[i][:]), reads=[f"xtc{i}"], writes=["xres"])
                if cfg.debug:
                    S.dma("sync", k_dma(dbg["d_xmix1"][blk, :], xt[i][:]), reads=[f"xtc{i}"], writes=["d_xmix1"])
            S.barrier()
            S.emit()
            if cfg.stop == 8:
                finish()
                return nc

        if moe_layer(1, 9, "d_xmoe1"):
            return nc

        with ExitStack() as ph:
            BC = sb(ph, "BCf", [128, 1, D], F32)
            pb = [ps(ph, f"pbf{i}", [128, 512], F32) for i in range(2)]
            xt = [sb(ph, f"xtf{i}", [128, D], F32) for i in range(2)]
            ot = [sb(ph, f"otf{i}", [128, D], F32) for i in range(2)]
            sqj = sb(ph, "sqjf", [128, D], F32)
            ss = sb(ph, "ssf", [128, 1], F32)
            rs = sb(ph, "rsf", [128, 2], F32)
            bcast_rows(BC, pb, C("nfin"), 0, "cst")
            for j in range(NB):
                i = j % 2
                blk = slice(j * 128, (j + 1) * 128)
                S.dma("sync", k_dma(xt[i][:], xres[blk, :]), reads=["xres"], writes=[f"xtf{i}"])
                S.op("scalar", k_act(sqj[:], xt[i][:], AF.Square, accum=ss[:, 0:1]), reads=[f"xtf{i}"], writes=["sqjf", "ssf"])
                S.op("scalar", k_act(rs[:, 0:1], ss[:, 0:1], AF.Sqrt, scale=1.0 / D, bias=epsb[:, 0:1]), reads=["ssf", "epsb"], writes=["rsf"])
                S.op("vector", k_recip(rs[:, 1:2], rs[:, 0:1]), reads=["rsf"], writes=["rsf2"])
                S.op("vector", k_stt(ot[i][:], xt[i][:], rs[:, 1:2], BC[:, 0, :], ALU.mult, ALU.mult),
                     reads=[f"xtf{i}", "rsf2", "BC0"], writes=[f"otf{i}"])
                S.dma("sync", k_dma(out_ap[blk, :], ot[i][:]), reads=[f"otf{i}"], writes=["out"])
        S.barrier()
        S.emit()
        finish()
    return nc


def _pos_table(T):
    rows = T // 64
    row = np.repeat(np.arange(rows), 64).astype(np.float32)
    col = np.tile(np.arange(64), rows).astype(np.float32)
    n_freq = D // 4
    omega = (np.float32(10000.0) ** (-np.arange(n_freq, dtype=np.float32) / np.float32(n_freq))).astype(np.float32)

    def emb(p):
        a = (p[:, None] * omega[None, :]).astype(np.float32)
        return np.concatenate([np.sin(a), np.cos(a)], axis=-1)
    return np.concatenate([emb(row), emb(col)], axis=-1).astype(np.float32)


def _consts(core, cfg, inp):
    b, q = core // 4, core % 4
    CAPP = cfg.CAPP
    cs = {}
    cs["ident"] = np.eye(128, dtype=np.float32)
    s = np.arange(128)[:, None]
    t = np.arange(128)[None, :]
    same = (s // 64) == (t // 64)
    cs["maskf"] = (same & (s <= t)).astype(np.float32)
    cs["maskb"] = (same & (s >= t)).astype(np.float32)
    pb_, pg_, pe_ = s // 64, (s % 64) // 16, s % 16
    qb_, qg_, qe_ = t // 64, (t % 64) // 16, t % 16
    samepair = (pb_ == qb_) & (pe_ == qe_)
    cs["Bm"] = samepair.astype(np.float32)
    cs["Tm"] = (samepair & (pg_ < qg_)).astype(np.float32)
    cs["hgn"] = np.broadcast_to(np.asarray(inp["hg_norm"][0], np.float32)[None, :], (128, 128)).copy()
    sel = np.zeros((128, 32), np.float32)
    for bb in range(2):
        for g in range(4):
            for el in range(2):
                sel[bb * 64 + g * 16 + 2 * core + el, bb * 8 + g * 2 + el] = 1.0
    rb = np.zeros((128, 32), np.float32)
    for e in range(16):
        sel[b * 64 + q * 16 + e, 16 + e] = 1.0
        rb[:, 16 + e] = (e // 2) * 4 * CAPP + (b * 2 + e % 2) * CAPP
    cs["sel"] = sel
    cs["retbase"] = rb
    gs = np.zeros((128, 32), np.float32)
    for el in range(2):
        gs[:, el * 16 + 2 * core + el] = 1.0
    cs["gsel"] = gs
    bs = np.zeros((128, 2), np.float32)
    bs[:, b] = 1.0
    cs["bsel"] = bs
    mf = np.zeros((128, 8), np.float32)
    mb = np.zeros((128, 8), np.float32)
    sl = np.zeros((128, 8), np.float32)
    sr = np.zeros((128, 8), np.float32)
    for r in range(8):
        if r // 4 == b and r % 4 < q:
            mf[:, r] = 1.0
        if r // 4 == b and r % 4 > q:
            mb[:, r] = 1.0
    if q > 0:
        sl[:, core - 1] = 1.0
    if q < 3:
        sr[:, core + 1] = 1.0
    cs["mf"], cs["mb"], cs["selL"], cs["selR"] = mf, mb, sl, sr

    def fm(v):
        return np.asarray(v, np.float32).reshape(8, 128).T

    cs["nmix"] = np.concatenate([fm(inp["norm_mix"][l]) for l in range(2)], axis=1)
    cs["nffn"] = np.concatenate([fm(inp["norm_ffn"][l]) for l in range(2)], axis=1)
    cs["nfin"] = fm(inp["norm_final"])
    cs["lbl"] = np.concatenate([fm(inp["hg_lb_logits"][i]) for i in range(3)], axis=1)
    cs["scw"] = np.concatenate([fm(inp["sc_conv"][0][j]) for j in range(3)], axis=1)
    ab = np.zeros((128, 12), np.float32)
    for l in range(2):
        ab[:, l * 6:(l + 1) * 6] = np.asarray(inp["ada_b"][l][core * 768:(core + 1) * 768], np.float32).reshape(6, 128).T
    cs["adab"] = ab
    cv = np.zeros((128, 8, 4), np.float32)
    cv[:, :, 0] = fm(inp["c"][0])
    cv[:, :, 1] = fm(inp["c"][1])
    cv[:, :, 2] = fm(inp["c_ctx"])
    cs["cvT"] = cv.reshape(128, 32)
    cs["ones"] = np.ones((128, 128), np.float32)
    out = np.zeros((128, NCONST), np.float32)
    for n, (o, w) in CO.items():
        out[:, o:o + w] = cs[n]
    return out


def make_in_maps(inp, cfg):
    NT = cfg.NT
    T = 4 * NT
    pos = _pos_table(T)
    f32 = lambda a: np.ascontiguousarray(np.asarray(a, np.float32))
    shared = {
        "hg_w_in": f32(inp["hg_w_in"][0]), "hg_w_out": f32(inp["hg_w_out"][0]),
        "sc_w_in": f32(inp["sc_w_in"][0]), "sc_w_out": f32(inp["sc_w_out"][0]),
        "router": f32(inp["moe_router"]),
    }
    maps = []
    for core in range(8):
        b, q = core // 4, core % 4
        m = dict(shared)
        m["x"] = f32(inp["x"][b, q * NT:(q + 1) * NT])
        m["pos"] = f32(pos[q * NT:(q + 1) * NT])
        m["ctx"] = f32(inp["ctx"][b])
        m["consts"] = _consts(core, cfg, inp)
        m["adaw"] = f32(np.asarray(inp["ada_w"])[:, :, core * 768:(core + 1) * 768])
        m["wg"] = f32(np.asarray(inp["moe_w_gate"])[:, 2 * core:2 * core + 2])
        m["wu"] = f32(np.asarray(inp["moe_w_up"])[:, 2 * core:2 * core + 2])
        m["wd"] = f32(np.asarray(inp["moe_w_down"])[:, 2 * core:2 * core + 2])
        maps.append(m)
    return maps


_NC_CACHE = {}


def run(inp, cfg, trace=False):
    key = (cfg.NT, cfg.DEXP, cfg.stop, cfg.debug)
    if key not in _NC_CACHE:
        _NC_CACHE[key] = build(cfg)
    nc = _NC_CACHE[key]
    maps = make_in_maps(inp, cfg)
    res = run_bass_kernel_spmd(nc, maps, core_ids=list(range(8)))
    return res


def kernel(**inputs):
    cfg = Cfg(NT=np.asarray(inputs["x"]).shape[1] // 4, DEXP=np.asarray(inputs["moe_w_gate"]).shape[-1])
    res = run(inputs, cfg)
    NT = cfg.NT
    out = np.zeros((2, 4 * NT, D), np.float32)
    for core in range(8):
        b, q = core // 4, core % 4
        out[b, q * NT:(q + 1) * NT] = res.results[core]["out"]
    return out
```

```python
import numpy as np
from contextlib import ExitStack
import concourse.bass as bass
import concourse.mybir as mybir
from concourse.bass_utils import run_bass_kernel_spmd

F32 = mybir.dt.float32
BF16 = mybir.dt.bfloat16
I32 = mybir.dt.int32
AF = mybir.ActivationFunctionType
ALU = mybir.AluOpType
AX = mybir.AxisListType

D = 1024
NH = 8
NE = 16
CTXL = 256
EPS = 1e-6
BIG = 16384.0
ENGS = ["sync", "scalar", "vector", "gpsimd", "tensor"]
NDMA = 32
SAME_ENGINE_SYNC = True


class Cfg:
    def __init__(self, NT=2048, DEXP=2048, stop=99, debug=False):
        self.NT = NT
        self.DEXP = DEXP
        self.stop = stop
        self.debug = debug
        self.NB = NT // 128
        self.NCH = NT // 64
        self.CAP = NT // 2
        self.CAPP = self.CAP + 128
        self.NSB = self.CAP // 128
        self.NFC = DEXP // 128


class Sched:
    def __init__(self, nc, sems, n_dma):
        self.nc = nc
        self.ops = {e: [] for e in ENGS}
        self.cnt = {e: 0 for e in ENGS}
        self.waited = {e: {} for e in ENGS}
        self.last_w = {}
        self.readers = {}
        self.esem = {"scalar": sems[0], "vector": sems[1], "gpsimd": sems[2], "tensor": sems[3]}
        self.dsem = list(sems[4:4 + n_dma])
        self.csem = sems[4 + n_dma]
        self.cval = 0
        self.dval = [0] * n_dma
        self.drr = 0
        self.grr = 0
        self.n_g = 12

    def _semh(self, key):
        if isinstance(key, str):
            return self.esem[key]
        return self.csem if key[0] == "c" else self.dsem[key[1]]

    def _need(self, eng, ev, waits):
        if ev is None:
            return
        key, val = ev
        if key == eng and (eng == "tensor" or not SAME_ENGINE_SYNC):
            return
        if self.waited[eng].get(key, 0) >= val:
            return
        self.waited[eng][key] = val
        waits.append((self._semh(key), val))

    def _hazards(self, eng, reads, writes, waits):
        for r in reads:
            self._need(eng, self.last_w.get(r), waits)
        for w in writes:
            self._need(eng, self.last_w.get(w), waits)
            for ev in self.readers.get(w, ()):
                self._need(eng, ev, waits)

    def _commit(self, ev, reads, writes):
        for r in reads:
            self.readers.setdefault(r, []).append(ev)
        for w in writes:
            self.last_w[w] = ev
            self.readers[w] = []

    def op(self, eng, fn, reads=(), writes=()):
        waits = []
        self._hazards(eng, reads, writes, waits)
        self.cnt[eng] += 1
        ev = (eng, self.cnt[eng])
        self.ops[eng].append((waits, fn, self.esem[eng], 1))
        self._commit(ev, reads, writes)
        return ev

    def dma(self, eng, fn, reads=(), writes=(), inc=16):
        waits = []
        self._hazards(eng, reads, writes, waits)
        if eng == "gpsimd":
            i = self.grr
            self.grr = (self.grr + 1) % self.n_g
        else:
            i = self.n_g + self.drr
            self.drr = (self.drr + 1) % (len(self.dsem) - self.n_g)
        key = ("d", i)
        if self.dval[i] > 0:
            self._need(eng, (key, self.dval[i]), waits)
        self.dval[i] += inc
        ev = (key, self.dval[i])
        self.ops[eng].append((waits, fn, self.dsem[i], inc))
        self._commit(ev, reads, writes)
        return ev

    def coll(self, fn, reads=(), writes=()):
        waits = []
        self._hazards("gpsimd", reads, writes, waits)
        self.cval += 1
        ev = (("c", 0), self.cval)
        self.ops["gpsimd"].append((waits, fn, self.csem, 1))
        self._commit(ev, reads, writes)
        return ev

    def barrier(self):
        for eng in ENGS:
            waits = []
            for k in self.esem:
                if self.cnt[k] > 0:
                    self._need(eng, (k, self.cnt[k]), waits)
            for i in range(len(self.dsem)):
                if self.dval[i] > 0:
                    self._need(eng, (("d", i), self.dval[i]), waits)
            if self.cval > 0:
                self._need(eng, (("c", 0), self.cval), waits)
            self.ops[eng].append((waits, None, None, 0))
        self.last_w = {}
        self.readers = {}

    def emit(self):
        nc = self.nc
        ops = self.ops
        self.ops = {e: [] for e in ENGS}
        with nc.Block() as block:
            def run(e, lst):
                for waits, fn, sem, inc in lst:
                    for s, v in waits:
                        e.wait_ge(s, v)
                    if fn is not None:
                        fn(e).then_inc(sem, inc)

            @block.sync
            def _(e):
                run(e, ops["sync"])

            @block.scalar
            def _(e):
                run(e, ops["scalar"])

            @block.vector
            def _(e):
                run(e, ops["vector"])

            @block.gpsimd
            def _(e):
                run(e, ops["gpsimd"])

            @block.tensor
            def _(e):
                run(e, ops["tensor"])


CONST_SPEC = [("ident", 128), ("maskf", 128), ("maskb", 128), ("Bm", 128), ("Tm", 128), ("hgn", 128),
              ("sel", 32), ("retbase", 32), ("gsel", 32), ("bsel", 2), ("mf", 8), ("mb", 8),
              ("selL", 8), ("selR", 8), ("nmix", 16), ("nffn", 16), ("nfin", 8), ("lbl", 24),
              ("scw", 24), ("adab", 12), ("cvT", 32), ("ones", 128)]
CO = {}
_o = 0
for _n, _w in CONST_SPEC:
    CO[_n] = (_o, _w)
    _o += _w
NCONST = _o


def k_dma(out, in_):
    return lambda e: e.dma_start(out=out, in_=in_)


def k_mm(out, lhsT, rhs, start, stop):
    return lambda e: e.matmul(out, lhsT=lhsT, rhs=rhs, start=start, stop=stop)


def k_tr(out, in_, ident):
    return lambda e: e.transpose(out, in_, ident)


def k_act(out, in_, func, scale=1.0, bias=None, accum=None):
    def f(e):
        kw = {}
        if bias is not None:
            kw["bias"] = bias
        if accum is not None:
            kw["accum_out"] = accum
        return e.activation(out=out, in_=in_, func=func, scale=scale, **kw)
    return f


def k_ts(out, in0, s1, s2, op0, op1=None, accum=None):
    def f(e):
        kw = {}
        if op1 is not None:
            kw["op1"] = op1
        if accum is not None:
            kw["accum_out"] = accum
        return e.tensor_scalar(out=out, in0=in0, scalar1=s1, scalar2=s2, op0=op0, **kw)
    return f


def k_tt(out, in0, in1, op):
    return lambda e: e.tensor_tensor(out=out, in0=in0, in1=in1, op=op)


def k_stt(out, in0, scalar, in1, op0, op1):
    return lambda e: e.scalar_tensor_tensor(out=out, in0=in0, scalar=scalar, in1=in1, op0=op0, op1=op1)


def k_copy(out, in_):
    return lambda e: e.tensor_copy(out=out, in_=in_)


def k_memset(ap, v):
    return lambda e: e.memset(ap, v)


def k_scan(out, d0, d1, init, op0, op1):
    return lambda e: e.tensor_tensor_scan(out=out, data0=d0, data1=d1, initial=init, op0=op0, op1=op1)


def k_recip(out, in_):
    return lambda e: e.reciprocal(out=out, in_=in_)


def k_ag(in_, out):
    return lambda e: e.collective_compute("AllGather", ALU.bypass, replica_groups=[list(range(8))],
                                          ins=[in_], outs=[out])


BREG = {}


def _breg(e, bound):
    if bound not in BREG:
        BREG[bound] = e.to_reg(bound)
    return BREG[bound]


def k_scatter(out, idx, in_, bound):
    return lambda e: e.indirect_dma_start(out=out, out_offset=bass.IndirectOffsetOnAxis(ap=idx, axis=0),
                                          in_=in_, in_offset=None, bounds_check=_breg(e, bound), oob_is_err=False)


def k_gather(out, in_, idx, bound):
    return lambda e: e.indirect_dma_start(out=out, out_offset=None, in_=in_,
                                          in_offset=bass.IndirectOffsetOnAxis(ap=idx, axis=0),
                                          bounds_check=_breg(e, bound), oob_is_err=False)


def build(cfg):
    NT, NB, NCH, CAP, CAPP, NSB, DEXP, NFC = cfg.NT, cfg.NB, cfg.NCH, cfg.CAP, cfg.CAPP, cfg.NSB, cfg.DEXP, cfg.NFC
    nc = bass.Bass("TRN2", target_bir_lowering=False)
    BREG.clear()

    def din(name, shape, dt=F32):
        return nc.dram_tensor(name, list(shape), dt, kind="ExternalInput").ap()

    def dscr(name, shape, dt=F32):
        return nc.dram_tensor(name, list(shape), dt).ap()

    x_in = din("x", [NT, D])
    pos_in = din("pos", [NT, D])
    ctx_in = din("ctx", [CTXL, D])
    consts_in = din("consts", [128, NCONST])
    adaw_in = din("adaw", [2, D, 768])
    hgwin_in = din("hg_w_in", [D, 5 * D])
    hgwout_in = din("hg_w_out", [D, D])
    scwin_in = din("sc_w_in", [D, 3 * D])
    scwout_in = din("sc_w_out", [D, D])
    router_in = din("router", [2, D, NE])
    wg_in = din("wg", [2, 2, D, DEXP])
    wu_in = din("wu", [2, 2, D, DEXP])
    wd_in = din("wd", [2, 2, DEXP, D])
    out_ap = nc.dram_tensor("out", [NT, D], F32, kind="ExternalOutput").ap()
    dbg = {}
    if cfg.debug:
        for nm, shp in (("d_xmix0", [NT, D]), ("d_xmoe0", [NT, D]), ("d_xmix1", [NT, D]), ("d_xmoe1", [NT, D]), ("d_h1", [NT, D]),
                        ("d_o", [NT, D]), ("d_aff", [NE, NT]), ("d_dest", [128, NB * 32])):
            dbg[nm] = nc.dram_tensor(nm, shp, F32, kind="ExternalOutput").ap()

    xres = dscr("xres", [NT, D])
    ada_loc = dscr("ada_loc", [128, 48])
    ada_all = dscr("ada_all", [8 * 128, 48])
    qseg_scr = dscr("qseg_scr", [NH, 2, 128, NT], BF16)
    o_scr = dscr("o_scr", [NH, 128, NB * 128])
    st_loc = dscr("st_loc", [128, NH * 258])
    st_all = dscr("st_all", [8 * 128, NH * 258])
    aff_loc = [dscr(f"aff_loc{l}", [NE, NT]) for l in range(2)]
    aff_all = [dscr(f"aff_all{l}", [8 * NE, NT]) for l in range(2)]
    ROWW = D + 32
    h2_loc = [dscr(f"h2_loc{l}", [NT, ROWW], BF16) for l in range(2)]
    h2_all = [dscr(f"h2_all{l}", [8 * NT, ROWW], BF16) for l in range(2)]
    xg = [[dscr(f"xg{l}_{i}", [CAPP, ROWW], BF16) for i in range(4)] for l in range(2)]
    y_loc = [dscr(f"y_loc{l}", [4 * CAPP, D], BF16) for l in range(2)]
    y_all = [dscr(f"y_all{l}", [8 * 4 * CAPP, D], BF16) for l in range(2)]
    halo_loc = dscr("halo_loc", [128, 16])
    halo_all = dscr("halo_all", [8 * 128, 16])

    with ExitStack() as top:
        uid = [0]

        def sb(es, name, shape, dt):
            uid[0] += 1
            return es.enter_context(nc.sbuf_tensor(f"{name}_u{uid[0]}", list(shape), dt))

        def ps(es, name, shape, dt):
            uid[0] += 1
            return es.enter_context(nc.psum_tensor(f"{name}_u{uid[0]}", list(shape), dt))

        sems = [top.enter_context(nc.semaphore(f"s{i}")) for i in range(5 + NDMA)]
        S = Sched(nc, sems, NDMA)
        for s_ in sems:
            nc.gpsimd.sem_clear(s_)
        nc.all_engine_barrier()

        def finish():
            nc.all_engine_barrier()
            for s_ in sems:
                nc.gpsimd.sem_clear(s_)
            nc.all_engine_barrier()
        cst = sb(top, "cst", [128, NCONST], F32)

        def C(name, lo=0, n=None):
            o, w = CO[name]
            n = w - lo if n is None else n
            return cst[:, o + lo:o + lo + n]

        identb = sb(top, "identb", [128, 128], BF16)
        ADAo = sb(top, "ADAo", [128, 2 * 48], F32)
        ADAx = sb(top, "ADAx", [128, 48], F32)
        lbt = sb(top, "lbt", [128, 16], F32)
        S.dma("sync", k_dma(cst[:], consts_in), writes=["cst"])
        S.op("vector", k_copy(identb[:], C("ident")), reads=["cst"], writes=["identb"])
        ident = C("ident")

        def bcast_rows(BC, es_ps, col, slot, tag, pk=("pb0", "pb1")):
            pb = es_ps
            for dc in range(8):
                cb = colbc[dc % 2]
                S.op("vector", k_ts(cb[:], C("ones"), col[:, dc:dc + 1], None, ALU.mult),
                     reads=["cst", tag], writes=[f"colbc{dc % 2}"])
                S.op("tensor", k_mm(pb[dc // 4][:, (dc % 4) * 128:(dc % 4 + 1) * 128], cb[:], ident, True, True),
                     reads=[f"colbc{dc % 2}", "cst"], writes=[pk[dc // 4]])
            for hlf in range(2):
                S.op("scalar", k_act(BC[:, slot, hlf * 512:(hlf + 1) * 512], pb[hlf][:], AF.Copy),
                     reads=[pk[hlf]], writes=[f"BC{slot}"])

        def prenorm(BC, xt, xkey, gslot, sslot, hb, hbkey, ss, rs, sqj, sfx=""):
            S.op("scalar", k_act(sqj[:], xt, AF.Square, accum=ss[:, 0:1]), reads=[xkey], writes=["sqj" + sfx, "ss" + sfx])
            S.op("scalar", k_act(rs[:, 0:1], ss[:, 0:1], AF.Sqrt, scale=1.0 / D, bias=epsb[:, 0:1]),
                 reads=["ss" + sfx, "epsb"], writes=["rs" + sfx])
            S.op("vector", k_recip(rs[:, 1:2], rs[:, 0:1]), reads=["rs" + sfx], writes=["rs2" + sfx])
            S.op("vector", k_stt(sqj[:], xt, rs[:, 1:2], BC[:, gslot, :], ALU.mult, ALU.mult),
                 reads=[xkey, "rs2" + sfx, f"BC{gslot}"], writes=["sqj" + sfx])
            S.op("vector", k_tt(hb, sqj[:], BC[:, sslot, :], ALU.add), reads=["sqj" + sfx, f"BC{sslot}"], writes=[hbkey])

        def transpose_to(hb, hbkey, dstT, dkey, col0, ptile, pkey, eng="scalar"):
            for kc in range(8):
                S.op("tensor", k_tr(ptile[:, kc * 128:(kc + 1) * 128], hb[:, kc * 128:(kc + 1) * 128], identb[:]),
                     reads=[hbkey, "identb"], writes=[pkey])
            src = ptile[:].rearrange("p (k n) -> p k n", k=8)
            if eng == "scalar":
                S.op("scalar", k_act(dstT[:, :, col0:col0 + 128], src, AF.Copy), reads=[pkey], writes=[dkey])
            else:
                S.op("vector", k_copy(dstT[:, :, col0:col0 + 128], src), reads=[pkey], writes=[dkey])

        colbc = [sb(top, f"colbc{i}", [128, 128], F32) for i in range(2)]
        epsb = sb(top, "epsb", [128, 1], F32)
        S.op("gpsimd", k_memset(epsb[:], EPS), writes=["epsb"])

        with ExitStack() as ph:
            W = sb(ph, "adaW", [128, 8, 768], F32)
            scv = sb(ph, "scv", [128, 32], F32)
            adaloc = sb(ph, "adaloc", [128, 48], F32)
            adaA = sb(ph, "adaA", [128, 8, 48], F32)
            ADAc = sb(ph, "ADAc", [128, 6 * 48], F32)
            lbe = sb(ph, "lbe", [128, 32], F32)
            pa = ps(ph, "pa", [128, 8], F32)
            S.op("scalar", k_act(scv[:], C("cvT"), AF.Silu), reads=["cst"], writes=["scv"])
            for l in range(2):
                S.dma("sync", k_dma(W[:], adaw_in[l].rearrange("(kc p) n -> p kc n", p=128)), writes=["adaW"])
                for fc in range(6):
                    for kc in range(8):
                        S.op("tensor", k_mm(pa[:, 0:4], W[:, kc, fc * 128:(fc + 1) * 128], scv[:, kc * 4:(kc + 1) * 4],
                                            kc == 0, kc == 7), reads=["adaW", "scv"], writes=["pa"])
                    c0 = (l * 6 + fc) * 4
                    S.op("vector", k_ts(adaloc[:, c0:c0 + 4], pa[:, 0:4], C("adab", l * 6 + fc, 1), None, ALU.add),
                         reads=["pa", "cst"], writes=["adaloc"])
            S.dma("sync", k_dma(ada_loc, adaloc[:]), reads=["adaloc"], writes=["ada_loc"])
            S.coll(k_ag(ada_loc, ada_all), reads=["ada_loc"], writes=["ada_all"])
            S.dma("sync", k_dma(adaA[:], ada_all.rearrange("(r p) n -> p r n", p=128)), reads=["ada_all"], writes=["adaA"])
            for l in range(2):
                for v in range(3):
                    i6 = l * 3 + v
                    S.op("vector", k_copy(ADAc[:, i6 * 48:(i6 + 1) * 48].rearrange("p (r f) -> p r f", f=6),
                                          adaA[:, :, l * 24 + v:l * 24 + 24:4]), reads=["adaA"], writes=["ADAc"])
            for l in range(2):
                S.op("vector", k_ts(ADAo[:, l * 48:(l + 1) * 48], ADAc[:, (l * 3) * 48:(l * 3 + 1) * 48],
                                    C("bsel", 0, 1), None, ALU.mult), reads=["ADAc", "cst"], writes=["ADAo"])
                S.op("vector", k_stt(ADAo[:, l * 48:(l + 1) * 48], ADAc[:, (l * 3 + 1) * 48:(l * 3 + 2) * 48],
                                     C("bsel", 1, 1), ADAo[:, l * 48:(l + 1) * 48], ALU.mult, ALU.add),
                     reads=["ADAc", "cst", "ADAo"], writes=["ADAo"])
            S.op("vector", k_copy(ADAx[:], ADAc[:, 2 * 48:3 * 48]), reads=["ADAc"], writes=["ADAx"])
            S.op("scalar", k_act(lbe[:, 0:24], C("lbl"), AF.Exp), reads=["cst"], writes=["lbe"])
            S.op("vector", k_tt(lbe[:, 24:32], lbe[:, 0:8], lbe[:, 8:16], ALU.add), reads=["lbe"], writes=["lbe"])
            S.op("vector", k_tt(lbe[:, 24:32], lbe[:, 24:32], lbe[:, 16:24], ALU.add), reads=["lbe"], writes=["lbe"])
            S.op("vector", k_recip(lbe[:, 24:32], lbe[:, 24:32]), reads=["lbe"], writes=["lbe"])
            S.op("vector", k_tt(lbt[:, 0:8], lbe[:, 0:8], lbe[:, 24:32], ALU.mult), reads=["lbe"], writes=["lbt"])
            S.op("vector", k_ts(lbt[:, 8:16], lbt[:, 0:8], -1.0, 1.0, ALU.mult, ALU.add), reads=["lbt"], writes=["lbt"])
            S.barrier()
            S.emit()
            if cfg.stop == 0:
                finish()
                return nc

        def ada(l, j):
            return ADAo[:, l * 48 + j * 8:l * 48 + (j + 1) * 8]

        def make_gain(es, name, scale_col, norm_col, tagr):
            g = sb(es, name, [128, 8], F32)
            S.op("vector", k_stt(g[:], scale_col, 1.0, norm_col, ALU.add, ALU.mult), reads=tagr, writes=[name])
            return g

        with ExitStack() as ph:
            hT = sb(ph, "hT", [128, 8, NT], BF16)
            hcT = sb(ph, "hcT", [128, 8, CTXL], BF16)
            cstate = [sb(ph, f"cstate{d}", [128, NH, 128], F32) for d in range(2)]
            pb = [ps(ph, f"pb{i}", [128, 512], F32) for i in range(2)]
            with ExitStack() as ph1:
                BC = sb(ph1, "BC", [128, 4, D], F32)
                ptr = ps(ph1, "ptr", [128, 1024], BF16)
                xt = [sb(ph1, f"xt{i}", [128, D], F32) for i in range(2)]
                pt = [sb(ph1, f"pt{i}", [128, D], F32) for i in range(2)]
                hb = [sb(ph1, f"hb{i}", [128, D], BF16) for i in range(2)]
                sqj = [sb(ph1, f"sqj{i}", [128, D], F32) for i in range(2)]
                ss = [sb(ph1, f"ss{i}", [128, 1], F32) for i in range(2)]
                rs = [sb(ph1, f"rs{i}", [128, 2], F32) for i in range(2)]
                G1 = make_gain(ph1, "G1c", ada(0, 1), C("nmix", 0, 8), ["ADAo", "cst"])
                GC = make_gain(ph1, "GCc", ADAx[:, 8:16], C("nmix", 0, 8), ["ADAx", "cst"])
                bcast_rows(BC, pb, G1, 0, "G1c")
                bcast_rows(BC, pb, ada(0, 0), 1, "ADAo")
                bcast_rows(BC, pb, GC, 2, "GCc")
                bcast_rows(BC, pb, ADAx[:, 0:8], 3, "ADAx")
                for j in range(NB):
                    i = j % 2
                    S.dma("sync", k_dma(xt[i][:], x_in[j * 128:(j + 1) * 128, :]), writes=[f"xt{i}"])
                    S.dma("sync", k_dma(pt[i][:], pos_in[j * 128:(j + 1) * 128, :]), writes=[f"pt{i}"])
                    S.op("gpsimd", k_tt(xt[i][:], xt[i][:], pt[i][:], ALU.add), reads=[f"xt{i}", f"pt{i}"], writes=[f"xt{i}"])
                    S.dma("sync", k_dma(xres[j * 128:(j + 1) * 128, :], xt[i][:]), reads=[f"xt{i}"], writes=[f"xres{j}"])
                    prenorm(BC, xt[i][:], f"xt{i}", 0, 1, hb[i][:], f"hb{i}", ss[i], rs[i], sqj[i], sfx=str(i))
                    if cfg.debug:
                        S.op("vector", k_copy(sqj[i][:], hb[i][:]), reads=[f"hb{i}"], writes=[f"sqj{i}"])
                        S.dma("sync", k_dma(dbg["d_h1"][j * 128:(j + 1) * 128, :], sqj[i][:]), reads=[f"sqj{i}"], writes=["d_h1"])
                    transpose_to(hb[i], f"hb{i}", hT, "hT", j * 128, ptr, "ptr")
                for j in range(CTXL // 128):
                    i = j % 2
                    S.dma("sync", k_dma(xt[i][:], ctx_in[j * 128:(j + 1) * 128, :]), writes=[f"xt{i}"])
                    prenorm(BC, xt[i][:], f"xt{i}", 2, 3, hb[i][:], f"hb{i}", ss[i], rs[i], sqj[i], sfx=str(i))
                    transpose_to(hb[i], f"hb{i}", hcT, "hcT", j * 128, ptr, "ptr")
                S.barrier()
                S.emit()
                if cfg.stop == 1:
                    finish()
                    return nc

            NTC = NT + CTXL
            NCHC = NTC // 64
            NCB = CTXL // 128
            with ExitStack() as phA:
                wts = [[sb(phA, f"w{n}{i}", [128, 8, 128], BF16) for n in range(4)] for i in range(2)]
                qs = sb(phA, "qs", [128, NT], F32)
                A1 = sb(phA, "A1", [128, NTC], F32)
                A2 = sb(phA, "A2", [128, NTC], F32)
                A3 = sb(phA, "A3", [128, NTC], F32)
                A4 = sb(phA, "A4", [128, NTC], F32)
                A5 = sb(phA, "A5", [128, NTC], F32)
                qd = [sb(phA, f"qd{d}", [128, NT], BF16) for d in range(2)]
                kd = [sb(phA, f"kd{d}", [128, NT], BF16) for d in range(2)]
                kd2 = [sb(phA, f"kd2{d}", [128, NTC], BF16) for d in range(2)]
                qsg = [sb(phA, f"qsg{d}", [128, NT], BF16) for d in range(2)]
                bse = sb(phA, "bse", [128, NCHC], F32)
                lastc = [sb(phA, f"lastc{d}", [128, NCHC], F32) for d in range(2)]
                elast = [sb(phA, f"elast{d}", [128, NCHC], F32) for d in range(2)]
                vh = sb(phA, "vh", [128, NB + NCB, 128], BF16)
                oloc = sb(phA, "oloc", [128, NB, 128], F32)
                S32 = [sb(phA, f"S32{d}", [128, 128], F32) for d in range(2)]
                Sbf = [sb(phA, f"Sbf{d}", [128, 128], BF16) for d in range(2)]
                ATm = [sb(phA, f"ATm{d}", [128, 128], BF16) for d in range(2)]
                k2T = [sb(phA, f"k2T{d}", [128, 128], BF16) for d in range(2)]
                stt_t = sb(phA, "stt_t", [128, NH * 258], F32)
                pA = [ps(phA, f"pA{d}", [128, 512], F32) for d in range(2)]
                pO = [ps(phA, f"pO{d}", [128, 512], F32) for d in range(2)]
                pS = [ps(phA, f"pS{d}", [128, 512], F32) for d in range(2)]
                pAt = [pA[d][:, 256:384].bitcast(BF16) for d in range(2)]
                ones_b = C("ones", 0, 1)

                def featproj(w, wkey, dst_fn):
                    tiles = [(hT, "hT", t0, min(512, NT - t0), t0) for t0 in range(0, NT, 512)]
                    tiles += [(hcT, "hcT", 0, CTXL, NT)]
                    for ti, (src, skey, t0, n, o0) in enumerate(tiles):
                        pp = pb[ti % 2]
                        for kc in range(8):
                            S.op("tensor", k_mm(pp[:, 0:n], w[:, kc, :], src[:, kc, t0:t0 + n], kc == 0, kc == 7),
                                 reads=[wkey, skey], writes=[f"pb{ti % 2}"])
                        dst_fn(pp[:, 0:n], f"pb{ti % 2}", o0, n)

                for h in range(NH):
                    wi = h % 2
                    w5 = wts[wi]
                    for n in range(4):
                        S.dma("gpsimd", k_dma(w5[n][:], hgwin_in[:, n * D + h * 128:n * D + (h + 1) * 128]
                                              .rearrange("(kc p) n -> p kc n", p=128)), writes=[f"w{n}{wi}"])
                    lb_h = lbt[:, h:h + 1]
                    oml_h = lbt[:, 8 + h:9 + h]

                    def q_dst(pp, pkey, o0, n):
                        if o0 < NT:
                            S.op("scalar", k_act(qs[:, o0:o0 + n], pp, AF.Copy, scale=128.0 ** -0.5), reads=[pkey], writes=["qs"])
                    featproj(w5[0], f"w0{wi}", q_dst)
                    for jb in range(NB + NCB):
                        src, skey, c0 = (hT, "hT", jb * 128) if jb < NB else (hcT, "hcT", (jb - NB) * 128)
                        pp = pb[jb % 2]
                        for kc in range(8):
                            S.op("tensor", k_mm(pp[:, 0:128], src[:, kc, c0:c0 + 128], w5[1][:, kc, :], kc == 0, kc == 7),
                                 reads=[skey, f"w1{wi}"], writes=[f"pb{jb % 2}"])
                        S.op("vector", k_copy(vh[:, jb, :], pp[:, 0:128]), reads=[f"pb{jb % 2}"], writes=["vh"])
                    P3 = A4[:].rearrange("p (c j) -> p c j", j=64)
                    C3 = A5[:].rearrange("p (c j) -> p c j", j=64)
                    E3 = A1[:].rearrange("p (c j) -> p c j", j=64)
                    for d in range(2):
                        def z_dst(pp, pkey, o0, n):
                            S.op("scalar", k_act(A1[:, o0:o0 + n], pp, AF.Sigmoid), reads=[pkey], writes=["A1"])
                        featproj(w5[2 + d], f"w{2 + d}{wi}", z_dst)
                        S.op("vector", k_ts(A1[:], A1[:], oml_h, lb_h, ALU.mult, ALU.add), reads=["A1", "lbt"], writes=["A1"])
                        S.op("scalar", k_act(A2[:], A1[:], AF.Ln), reads=["A1"], writes=["A2"])
                        S.op("gpsimd", k_ts(A3[:], A1[:], -1.0, 1.0, ALU.mult, ALU.add), reads=["A1"], writes=["A3"])
                        S.op("vector", k_scan(A4[:, 0:NT], ones_b.to_broadcast([128, NT]), A2[:, 0:NT], 0.0, ALU.mult, ALU.add),
                             reads=["A2", "cst"], writes=["A4"])
                        S.op("vector", k_scan(A4[:, NT:NTC], ones_b.to_broadcast([128, CTXL]), A2[:, NT:NTC], 0.0, ALU.mult, ALU.add),
                             reads=["A2", "cst"], writes=["A4"])
                        if d == 0:
                            S.op("vector", k_copy(bse[:, 1:NCHC], A4[:, 63:NTC - 1:64]), reads=["A4"], writes=["bse"])
                            S.op("vector", k_memset(bse[:, 0:1], 0.0), writes=["bse"])
                            S.op("vector", k_memset(bse[:, NCH:NCH + 1], 0.0), writes=["bse"])
                            S.op("vector", k_tt(C3, P3, bse[:].unsqueeze(2).to_broadcast([128, NCHC, 64]), ALU.subtract),
                                 reads=["A4", "bse"], writes=["A5"])
                            S.op("vector", k_copy(lastc[d][:], A5[:, 63:NTC:64]), reads=["A5"], writes=[f"lastc{d}"])
                            S.op("scalar", k_act(A1[:, 0:NT], A4[:, 0:NT], AF.Exp), reads=["A4", "A3", "A2"], writes=["A1"])
                        else:
                            S.op("vector", k_copy(bse[:], A4[:, 63:NTC:64]), reads=["A4"], writes=["bse"])
                            S.op("vector", k_tt(A1[:], A2[:], A4[:], ALU.subtract), reads=["A2", "A4", "A3"], writes=["A1"])
                            S.op("vector", k_tt(C3, E3, bse[:].unsqueeze(2).to_broadcast([128, NCHC, 64]), ALU.add),
                                 reads=["A1", "bse"], writes=["A5"])
                            S.op("vector", k_copy(lastc[d][:], A5[:, 0:NTC:64]), reads=["A5"], writes=[f"lastc{d}"])
                            S.op("scalar", k_act(A1[:, 0:NT], A1[:, 0:NT], AF.Exp, bias=A4[:, NT - 1:NT]), reads=["A1", "A4"], writes=["A1"])
                        S.op("scalar", k_act(elast[d][:], lastc[d][:], AF.Exp), reads=[f"lastc{d}"], writes=[f"elast{d}"])
                        dcol = h * 258 + 256 + d
                        S.op("vector", k_tt(qsg[d][:], qs[:], A1[:, 0:NT], ALU.mult), reads=["qs", "A1"], writes=[f"qsg{d}"])
                        S.dma("sync", k_dma(qseg_scr[h, d], qsg[d][:]), reads=[f"qsg{d}"], writes=[f"qseg_scr{h}_{d}"])
                        S.op("scalar", k_act(stt_t[:, dcol:dcol + 1], A4[:, NT - 1:NT], AF.Exp), reads=["A4"], writes=["stt_t"])
                        S.op("scalar", k_act(A1[:, 0:NT], A5[:, 0:NT], AF.Exp), reads=["A5", f"qsg{d}"], writes=["A1"])
                        S.op("vector", k_tt(qd[d][:], qs[:], A1[:, 0:NT], ALU.mult), reads=["qs", "A1"], writes=[f"qd{d}"])
                        S.op("scalar", k_act(A1[:, 0:NT], A5[:, 0:NT], AF.Exp, scale=-1.0), reads=["A5", f"qd{d}"], writes=["A1"])
                        S.op("gpsimd", k_tt(kd[d][:], A3[:, 0:NT], A1[:, 0:NT], ALU.mult), reads=["A3", "A1"], writes=[f"kd{d}"])
                        S.op("vector", k_tt(E3, lastc[d][:].unsqueeze(2).to_broadcast([128, NCHC, 64]), C3, ALU.subtract),
                             reads=[f"lastc{d}", "A5", f"kd{d}"], writes=["A1"])
                        S.op("scalar", k_act(A1[:], A1[:], AF.Exp), reads=["A1"], writes=["A1"])
                        S.op("vector", k_tt(kd2[d][:], A3[:], A1[:], ALU.mult), reads=["A3", "A1"], writes=[f"kd2{d}"])

                    def state_step(d, vblk, c, chunk_idx):
                        S.op("tensor", k_mm(pS[d][:, 0:128], k2T[d][c * 64:(c + 1) * 64, :], vh[c * 64:(c + 1) * 64, vblk, :], True, True),
                             reads=[f"k2T{d}", "vh"], writes=[f"pS{d}"])
                        S.op("vector", k_stt(Sbf[d][:], S32[d][:], elast[d][:, chunk_idx:chunk_idx + 1], pS[d][:, 0:128], ALU.mult, ALU.add),
                             reads=[f"S32{d}", f"elast{d}", f"pS{d}"], writes=[f"Sbf{d}"])
                        S.op("vector", k_stt(S32[d][:], S32[d][:], elast[d][:, chunk_idx:chunk_idx + 1], pS[d][:, 0:128], ALU.mult, ALU.add),
                             reads=[f"S32{d}", f"elast{d}", f"pS{d}"], writes=[f"S32{d}"])

                    def k2_transpose(d, col0):
                        S.op("tensor", k_tr(pAt[d][:, 0:128], kd2[d][:, col0:col0 + 128], identb[:]),
                             reads=[f"kd2{d}", "identb"], writes=[f"pAt{d}"])
                        S.op("scalar", k_act(k2T[d][:], pAt[d][:, 0:128], AF.Copy), reads=[f"pAt{d}"], writes=[f"k2T{d}"])

                    for d in range(2):
                        S.op("gpsimd", k_memset(S32[d][:], 0.0), writes=[f"S32{d}"])
                        S.op("gpsimd", k_memset(Sbf[d][:], 0.0), writes=[f"Sbf{d}"])
                    S.op("gpsimd", k_memset(oloc[:], 0.0), writes=["oloc"])
                    for step in range(NCB):
                        for d in range(2):
                            cb = step if d == 0 else NCB - 1 - step
                            k2_transpose(d, NT + cb * 128)
                            for c in ([0, 1] if d == 0 else [1, 0]):
                                state_step(d, NB + cb, c, NCH + cb * 2 + c)
                    for d in range(2):
                        S.op("vector", k_copy(cstate[d][:, h, :], S32[d][:]), reads=[f"S32{d}"], writes=[f"cstate{d}"])
                        S.op("gpsimd", k_memset(S32[d][:], 0.0), reads=[f"cstate{d}"], writes=[f"S32{d}"])
                        S.op("gpsimd", k_memset(Sbf[d][:], 0.0), writes=[f"Sbf{d}"])
                    for step in range(NB):
                        for d in range(2):
                            jb = step if d == 0 else NB - 1 - step
                            corder = [0, 1] if d == 0 else [1, 0]
                            mask = C("maskf") if d == 0 else C("maskb")
                            col0 = jb * 128
                            S.op("tensor", k_mm(pA[d][:, 0:128], kd[d][:, col0:col0 + 128], qd[d][:, col0:col0 + 128], True, True),
                                 reads=[f"kd{d}", f"qd{d}"], writes=[f"pA{d}"])
                            S.op("vector", k_tt(ATm[d][:], pA[d][:, 0:128], mask, ALU.mult), reads=[f"pA{d}", "cst"], writes=[f"ATm{d}"])
                            k2_transpose(d, col0)
                            S.op("tensor", k_mm(pO[d][:, 0:128], ATm[d][:], vh[:, jb, :], True, False),
                                 reads=[f"ATm{d}", "vh"], writes=[f"pO{d}"])
                            for ci, c in enumerate(corder):
                                S.op("tensor", k_mm(pO[d][c * 64:(c + 1) * 64, 0:128], qd[d][:, col0 + c * 64:col0 + (c + 1) * 64],
                                                    Sbf[d][:], False, ci == 1),
                                     reads=[f"qd{d}", f"Sbf{d}"], writes=[f"pO{d}"])
                                state_step(d, jb, c, jb * 2 + c)
                            S.op("vector", k_tt(oloc[:, jb, :], oloc[:, jb, :], pO[d][:, 0:128], ALU.add),
                                 reads=[f"pO{d}", "oloc"], writes=["oloc"])
                    for d in range(2):
                        S.op("vector", k_copy(stt_t[:, h * 258 + d * 128:h * 258 + (d + 1) * 128], S32[d][:]),
                             reads=[f"S32{d}"], writes=["stt_t"])
                    S.dma("sync", k_dma(o_scr[h], oloc[:].rearrange("p b v -> p (b v)")), reads=["oloc"], writes=[f"o_scr{h}"])
                S.dma("sync", k_dma(st_loc, stt_t[:]), reads=["stt_t"], writes=["st_loc"])
                S.coll(k_ag(st_loc, st_all), reads=["st_loc"], writes=["st_all"])
                S.barrier()
                S.emit()
                if cfg.stop == 2:
                    finish()
                    return nc

            with ExitStack() as phB:
                BC = sb(phB, "BC", [128, 1, D], F32)
                ptr = ps(phB, "ptr", [128, 1024], BF16)
                pO = ps(phB, "pOb", [128, 512], F32)
                pG = ps(phB, "pGb", [128, 512], F32)
                go = sb(phB, "go", [128, NB, D], BF16)
                stA = [sb(phB, f"stA{i}", [128, 8, 258], F32) for i in range(2)]
                Sin = [sb(phB, f"Sin{d}", [128, 128], F32) for d in range(2)]
                Sinb = [sb(phB, f"Sinb{d}", [128, 128], BF16) for d in range(2)]
                acol = sb(phB, "acol", [128, 1], F32)
                qsgL = [[sb(phB, f"qsgL{i}{d}", [128, NT], BF16) for d in range(2)] for i in range(2)]
                olocL = [sb(phB, f"olocL{i}", [128, NB * 128], F32) for i in range(2)]
                wgh = [sb(phB, f"wgh{i}", [128, 8, 128], BF16) for i in range(2)]
                ot = sb(phB, "ot", [128, 128], F32)
                oj = sb(phB, "oj", [128, 128], F32)
                sg = sb(phB, "sg", [128, 128], F32)
                ss = sb(phB, "ssb", [128, 1], F32)
                rs = sb(phB, "rsb", [128, 2], F32)
                wo = sb(phB, "wo", [128, 8, D], BF16)
                xt = [sb(phB, f"xtb{i}", [128, D], F32) for i in range(2)]
                tmpy = sb(phB, "tmpy", [128, 512], F32)
                bcast_rows(BC, pb, ada(0, 2), 0, "ADAo")
                S.dma("gpsimd", k_dma(wo[:], hgwout_in.rearrange("(kc p) n -> p kc n", p=128)), writes=["wo"])
                for h in range(NH):
                    i = h % 2
                    S.dma("sync", k_dma(stA[i][:], st_all[:, h * 258:(h + 1) * 258].rearrange("(r p) n -> p r n", p=128)),
                          reads=["st_all"], writes=[f"stA{i}"])
                    S.dma("gpsimd", k_dma(wgh[i][:], hgwin_in[:, 4 * D + h * 128:4 * D + (h + 1) * 128]
                                          .rearrange("(kc p) n -> p kc n", p=128)), writes=[f"wgh{i}"])
                    for d in range(2):
                        S.dma("sync", k_dma(qsgL[i][d][:], qseg_scr[h, d]), reads=["qseg_scr"], writes=[f"qsgL{i}{d}"])
                    S.dma("sync", k_dma(olocL[i][:], o_scr[h]), reads=["o_scr"], writes=[f"olocL{i}"])
                    for d in range(2):
                        mname = "mf" if d == 0 else "mb"
                        S.op("vector", k_copy(Sin[d][:], cstate[d][:, h, :]), reads=[f"cstate{d}"], writes=[f"Sin{d}"])
                        for r in (range(8) if d == 0 else range(7, -1, -1)):
                            mcol = C(mname, r, 1)
                            S.op("vector", k_ts(acol[:], stA[i][:, r, 256 + d:257 + d], -1.0, mcol, ALU.add, ALU.mult),
                                 reads=[f"stA{i}", "cst"], writes=["acol"])
                            S.op("vector", k_ts(acol[:], acol[:], 1.0, None, ALU.add), reads=["acol"], writes=["acol"])
                            S.op("vector", k_ts(Sin[d][:], Sin[d][:], acol[:, 0:1], None, ALU.mult), reads=["acol", f"Sin{d}"], writes=[f"Sin{d}"])
                            S.op("vector", k_stt(Sin[d][:], stA[i][:, r, d * 128:(d + 1) * 128], mcol, Sin[d][:], ALU.mult, ALU.add),
                                 reads=[f"stA{i}", "cst", f"Sin{d}"], writes=[f"Sin{d}"])
                        S.op("scalar", k_act(Sinb[d][:], Sin[d][:], AF.Copy), reads=[f"Sin{d}"], writes=[f"Sinb{d}"])
                    for jb in range(NB):
                        blk = slice(jb * 128, (jb + 1) * 128)
                        S.op("tensor", k_mm(pO[:, 0:128], qsgL[i][0][:, blk], Sinb[0][:], True, False),
                             reads=[f"qsgL{i}0", "Sinb0"], writes=["pOb"])
                        S.op("tensor", k_mm(pO[:, 0:128], qsgL[i][1][:, blk], Sinb[1][:], False, True),
                             reads=[f"qsgL{i}1", "Sinb1"], writes=["pOb"])
                        S.op("vector", k_tt(ot[:], olocL[i][:, blk], pO[:, 0:128], ALU.add), reads=[f"olocL{i}", "pOb"], writes=["ot"])
                        if cfg.debug:
                            S.dma("sync", k_dma(dbg["d_o"][blk, h * 128:(h + 1) * 128], ot[:]), reads=["ot"], writes=["d_o"])
                        S.op("scalar", k_act(oj[:], ot[:], AF.Square, accum=ss[:, 0:1]), reads=["ot"], writes=["oj", "ssb"])
                        S.op("scalar", k_act(rs[:, 0:1], ss[:, 0:1], AF.Sqrt, scale=1.0 / 128, bias=epsb[:, 0:1]),
                             reads=["ssb", "epsb"], writes=["rsb"])
                        S.op("vector", k_recip(rs[:, 1:2], rs[:, 0:1]), reads=["rsb"], writes=["rsb2"])
                        S.op("vector", k_stt(oj[:], ot[:], rs[:, 1:2], C("hgn"), ALU.mult, ALU.mult), reads=["ot", "rsb2", "cst"], writes=["oj"])
                        for kc in range(8):
                            S.op("tensor", k_mm(pG[:, 0:128], hT[:, kc, blk], wgh[i][:, kc, :], kc == 0, kc == 7),
                                 reads=["hT", f"wgh{i}"], writes=["pGb"])
                        S.op("scalar", k_act(sg[:], pG[:, 0:128], AF.Silu), reads=["pGb"], writes=["sg"])
                        S.op("vector", k_tt(go[:, jb, h * 128:(h + 1) * 128], oj[:], sg[:], ALU.mult), reads=["oj", "sg"], writes=["go"])
                for jb in range(NB):
                    transpose_to(go[:, jb, :], "go", hT, "hT", jb * 128, ptr, "ptr")
                for jb in range(NB):
                    i = jb % 2
                    blk = slice(jb * 128, (jb + 1) * 128)
                    S.dma("sync", k_dma(xt[i][:], xres[blk, :]), reads=[f"xres{jb}"], writes=[f"xtb{i}"])
                    for hf in range(2):
                        for kc in range(8):
                            S.op("tensor", k_mm(pb[hf][:], hT[:, kc, blk], wo[:, kc, hf * 512:(hf + 1) * 512], kc == 0, kc == 7),
                                 reads=["hT", "wo"], writes=[f"pb{hf}"])
                        S.op("vector", k_tt(tmpy[:], pb[hf][:], BC[:, 0, hf * 512:(hf + 1) * 512], ALU.mult),
                             reads=[f"pb{hf}", "BC0"], writes=["tmpy"])
                        S.op("vector", k_tt(xt[i][:, hf * 512:(hf + 1) * 512], xt[i][:, hf * 512:(hf + 1) * 512], tmpy[:], ALU.add),
                             reads=["tmpy", f"xtb{i}"], writes=[f"xtb{i}"])
                    S.dma("sync", k_dma(xres[blk, :], xt[i][:]), reads=[f"xtb{i}"], writes=[f"xres{jb}"])
                    if cfg.debug:
                        S.dma("sync", k_dma(dbg["d_xmix0"][blk, :], xt[i][:]), reads=[f"xtb{i}"], writes=["d_xmix0"])
                S.barrier()
                S.emit()
                if cfg.stop == 3:
                    finish()
                    return nc
        def moe_layer(l, stop_base, dbg_name):
            with ExitStack() as ph:
                BC = sb(ph, "BCm", [128, 2, D], F32)
                pbq = [[ps(ph, f"pbm{i}{k}", [128, 512], F32) for k in range(2)] for i in range(2)]
                pb = pbq[0]
                pLq = [ps(ph, f"pL{i}", [128, 512], F32) for i in range(2)]
                xt = [sb(ph, f"xtm{i}", [128, D], F32) for i in range(2)]
                h2fq = [sb(ph, f"h2f{i}", [128, D], F32) for i in range(2)]
                rowt = [sb(ph, f"rowt{i}", [128, ROWW], BF16) for i in range(2)]
                h2Tq = [sb(ph, f"h2T{i}", [128, 8, 128], F32) for i in range(2)]
                wr = sb(ph, "wr", [128, 8, NE], F32)
                sqjq = [sb(ph, f"sqjm{i}", [128, D], F32) for i in range(2)]
                ssq = [sb(ph, f"ssm{i}", [128, 1], F32) for i in range(2)]
                rsq = [sb(ph, f"rsm{i}", [128, 2], F32) for i in range(2)]
                mxq = [sb(ph, f"mx{i}", [128, 2], F32) for i in range(2)]
                smq = [sb(ph, f"sm{i}", [128, 2], F32) for i in range(2)]
                exq = [sb(ph, f"ex{i}", [128, NE], F32) for i in range(2)]
                affq = [sb(ph, f"aff{i}", [128, NE], F32) for i in range(2)]
                affT = sb(ph, "affT", [NE, NT], F32)
                G2 = make_gain(ph, f"G2c{l}", ada(l, 4), C("nffn", l * 8, 8), ["ADAo", "cst"])
                bcast_rows(BC, pb, G2, 0, f"G2c{l}", pk=("pbm00", "pbm01"))
                bcast_rows(BC, pb, ada(l, 3), 1, "ADAo", pk=("pbm00", "pbm01"))
                S.dma("sync", k_dma(wr[:], router_in[l].rearrange("(kc p) e -> p kc e", p=128)), writes=["wr"])
                for j in range(NB):
                    i = j % 2
                    blk = slice(j * 128, (j + 1) * 128)
                    h2f, h2T, pL, pbj = h2fq[i], h2Tq[i], pLq[i], pbq[i]
                    mx, sm, ex, aff = mxq[i], smq[i], exq[i], affq[i]
                    S.dma("sync", k_dma(xt[i][:], xres[blk, :]), reads=[f"xres{j}"], writes=[f"xtm{i}"])
                    prenorm(BC, xt[i][:], f"xtm{i}", 0, 1, h2f[:], f"h2f{i}", ssq[i], rsq[i], sqjq[i], sfx=f"m{i}")
                    S.op("scalar", k_act(rowt[i][:, 0:D], h2f[:], AF.Copy), reads=[f"h2f{i}"], writes=[f"rowt{i}"])
                    for kc in range(8):
                        S.op("tensor", k_tr(pbj[kc // 4][:, (kc % 4) * 128:(kc % 4 + 1) * 128], h2f[:, kc * 128:(kc + 1) * 128], ident),
                             reads=[f"h2f{i}", "cst"], writes=[f"pbm{i}{kc // 4}"])
                    for hf in range(2):
                        eng_ = "scalar" if hf == 0 else "vector"
                        fn_ = (k_act(h2T[:, hf * 4:(hf + 1) * 4, :], pbj[hf][:].rearrange("p (k n) -> p k n", k=4), AF.Copy) if hf == 0
                               else k_copy(h2T[:, hf * 4:(hf + 1) * 4, :], pbj[hf][:].rearrange("p (k n) -> p k n", k=4)))
                        S.op(eng_, fn_, reads=[f"pbm{i}{hf}"], writes=[f"h2T{i}"])
                    for kc in range(8):
                        S.op("tensor", k_mm(pL[:, 0:NE], h2T[:, kc, :], wr[:, kc, :], kc == 0, kc == 7),
                             reads=[f"h2T{i}", "wr"], writes=[f"pL{i}"])
                    S.op("vector", lambda e, o=mx[:, 0:1], a=pL[:, 0:NE]: e.reduce_max(out=o, in_=a, axis=AX.X), reads=[f"pL{i}"], writes=[f"mx{i}"])
                    S.op("vector", k_ts(mx[:, 1:2], mx[:, 0:1], -1.0, None, ALU.mult), reads=[f"mx{i}"], writes=[f"mx2{i}"])
                    S.op("scalar", k_act(ex[:], pL[:, 0:NE], AF.Exp, bias=mx[:, 1:2], accum=sm[:, 0:1]), reads=[f"pL{i}", f"mx2{i}"], writes=[f"ex{i}", f"sm{i}"])
                    S.op("vector", k_recip(sm[:, 1:2], sm[:, 0:1]), reads=[f"sm{i}"], writes=[f"sm2{i}"])
                    S.op("vector", k_ts(aff[:], ex[:], sm[:, 1:2], None, ALU.mult), reads=[f"ex{i}", f"sm2{i}"], writes=[f"aff{i}"])
                    S.op("vector", k_copy(rowt[i][:, D:ROWW].bitcast(F32), aff[:]), reads=[f"aff{i}"], writes=[f"rowt{i}"])
                    S.dma("sync", k_dma(h2_loc[l][blk, :], rowt[i][:]), reads=[f"rowt{i}"], writes=[f"h2_loc{j}"])
                    S.op("tensor", k_tr(pL[0:NE, 256:384], aff[:], ident), reads=[f"aff{i}", "cst"], writes=[f"pLt{i}"])
                    S.op("scalar", k_act(affT[:, blk], pL[0:NE, 256:384], AF.Copy), reads=[f"pLt{i}"], writes=[f"affT{j}"])
                S.dma("sync", k_dma(aff_loc[l], affT[:]), reads=[f"affT{j_}" for j_ in range(NB)], writes=["aff_loc"])
                if cfg.debug and l == 0:
                    S.dma("sync", k_dma(dbg["d_aff"], affT[:]), reads=[f"affT{j_}" for j_ in range(NB)], writes=["d_aff"])
                S.coll(k_ag(aff_loc[l], aff_all[l]), reads=["aff_loc"], writes=["aff_all"])
                S.barrier()
                S.emit()
                if cfg.stop == stop_base:
                    finish()
                    return True

            with ExitStack() as phm:
                destSel = sb(phm, "destSel", [128, NB, 36], I32)
                selm = sb(phm, "selm", [128, NB, 16], F32)
                with ExitStack() as ph:
                    A = sb(ph, "A", [128, NT], F32)
                    junk = sb(ph, "junk", [128, NT], BF16)
                    mk = sb(ph, "mk", [128, NT], F32)
                    inc = sb(ph, "inc", [128, NT], F32)
                    bs_ = sb(ph, "bs_", [128, 8], F32)
                    cnt = sb(ph, "cnt", [128, 2], F32)
                    dsf = sb(ph, "dsf", [128, 32], F32)
                    pt_ = ps(ph, "pt_", [128, 512], F32)
                    pD = ps(ph, "pD", [128, 512], F32)
                    lo, hi, mid, ge, d1, offm = (bs_[:, k:k + 1] for k in range(6))
                    S.dma("sync", k_dma(A[:], aff_all[l]), reads=["aff_all"], writes=["A"])
                    S.coll(k_ag(h2_loc[l], h2_all[l]), reads=[], writes=["h2_all"])
                    S.op("vector", k_memset(cnt[:], 0.0), writes=["cnt"])
                    S.op("vector", k_memset(bs_[:, 0:1], 0.0), writes=["bs"])
                    S.op("vector", k_memset(bs_[:, 1:2], 1.0), writes=["bs"])
                    S.op("vector", k_memset(bs_[:, 2:3], 0.5), writes=["bs"])
                    for it in range(32):
                        S.op("vector", k_ts(junk[:], A[:], mid, None, ALU.is_ge, ALU.add, accum=cnt[:, 0:1]), reads=["A", "bs"], writes=["junk", "cnt"])
                        S.op("tensor", k_mm(pt_[:, 0:2], C("Bm"), cnt[:, 0:2], True, True), reads=["cnt", "cst"], writes=["pt_"])
                        S.op("vector", k_ts(ge, pt_[:, 0:1], float(CAP), None, ALU.is_ge), reads=["pt_"], writes=["bs"])
                        S.op("vector", k_tt(d1, mid, lo, ALU.subtract), reads=["bs"], writes=["bs"])
                        S.op("vector", k_stt(lo, d1, ge, lo, ALU.mult, ALU.add), reads=["bs"], writes=["bs"])
                        S.op("vector", k_tt(d1, hi, mid, ALU.subtract), reads=["bs"], writes=["bs"])
                        S.op("vector", k_stt(hi, d1, ge, mid, ALU.mult, ALU.add), reads=["bs"], writes=["bs"])
                        S.op("vector", k_tt(mid, lo, hi, ALU.add), reads=["bs"], writes=["bs"])
                        S.op("vector", k_ts(mid, mid, 0.5, None, ALU.mult), reads=["bs"], writes=["bs"])
                    S.op("vector", k_ts(mk[:], A[:], lo, None, ALU.is_ge), reads=["A", "bs"], writes=["mk"])
                    S.op("vector", k_scan(inc[:], C("ones", 0, 1).to_broadcast([128, NT]), mk[:], 0.0, ALU.mult, ALU.add),
                         reads=["mk", "cst"], writes=["inc"])
                    S.op("vector", k_copy(cnt[:, 0:1], inc[:, NT - 1:NT]), reads=["inc"], writes=["cnt"])
                    S.op("tensor", k_mm(pt_[:, 0:2], C("Tm"), cnt[:, 0:2], True, True), reads=["cnt", "cst"], writes=["pt_"])
                    S.op("vector", k_ts(offm, pt_[:, 0:1], -(1.0 + BIG), None, ALU.add), reads=["pt_"], writes=["bs"])
                    S.op("vector", k_ts(inc[:], inc[:], offm, None, ALU.add), reads=["inc", "bs"], writes=["inc"])
                    S.op("vector", k_tt(inc[:], inc[:], mk[:], ALU.mult), reads=["inc", "mk"], writes=["inc"])
                    S.op("vector", k_ts(inc[:], inc[:], BIG, None, ALU.add), reads=["inc"], writes=["inc"])
                    for j in range(NB):
                        S.op("tensor", k_mm(pD[:, 0:32], inc[:, j * 128:(j + 1) * 128], C("sel"), True, True),
                             reads=["inc", "cst"], writes=["pD"])
                        S.op("vector", k_ts(selm[:, j, :], pD[:, 16:32], BIG - 0.5, None, ALU.is_lt), reads=["pD"], writes=["selm"])
                        S.op("vector", k_tt(dsf[:], pD[:, 0:32], C("retbase"), ALU.add), reads=["pD", "cst"], writes=["dsf"])
                        S.op("vector", k_copy(destSel[:, j, 0:32], dsf[:]), reads=["dsf"], writes=["destSel"])
                        if cfg.debug and l == 0:
                            S.dma("sync", k_dma(dbg["d_dest"][:, j * 32:(j + 1) * 32], dsf[:]), reads=["dsf"], writes=["d_dest"])
                    S.barrier()
                    S.emit()
                    if cfg.stop == stop_base + 1:
                        finish()
                        return True

                with ExitStack() as ph:
                    ptr = ps(ph, "ptre", [128, 1024], BF16)
                    pa = [ps(ph, f"pa{i}", [128, 512], F32) for i in range(2)]
                    pu = [ps(ph, f"pu{i}", [128, 512], F32) for i in range(2)]
                    py = [ps(ph, f"py{i}", [128, 512], F32) for i in range(2)]
                    rw = [sb(ph, f"rw{i}", [128, ROWW], BF16) for i in range(6)]
                    xr = [sb(ph, f"xr{i}", [128, ROWW], BF16) for i in range(2)]
                    xgT = [sb(ph, f"xgT{bb}", [128, 8, CAP], BF16) for bb in range(2)]
                    actT = [sb(ph, f"actT{bb}", [128, NFC, CAP], BF16) for bb in range(2)]
                    wdt = sb(ph, "wdt", [128, NFC, D], BF16)
                    wgf = [sb(ph, f"wgf{i}", [128, 8, 128], BF16) for i in range(3)]
                    wuf = [sb(ph, f"wuf{i}", [128, 8, 128], BF16) for i in range(3)]
                    gates = sb(ph, "gates", [128, 2, NSB], F32)
                    t16 = sb(ph, "t16", [128, NE], F32)
                    sa = [sb(ph, f"sa{i}", [128, 512], F32) for i in range(2)]
                    yrow = [sb(ph, f"yrow{i}", [128, D], BF16) for i in range(2)]
                    n_rw = 0
                    for bb in range(2):
                        for g in range(4):
                            for j in range(NB):
                                i = n_rw % 6
                                n_rw += 1
                                r0 = (bb * 4 + g) * NT + j * 128
                                S.dma("sync", k_dma(rw[i][:], h2_all[l][r0:r0 + 128, :]), reads=["h2_all"], writes=[f"rw{i}"])
                                for el in range(2):
                                    col = bb * 8 + g * 2 + el
                                    S.dma("gpsimd", k_scatter(xg[l][bb * 2 + el], destSel[:, j, col:col + 1], rw[i][:], CAPP - 1),
                                          reads=[f"rw{i}", "destSel"], writes=[f"xg{bb * 2 + el}_{g}_{j}"])
                    NST = max(1, CAP // 512)
                    NN = min(512, CAP)
                    for el in range(2):
                        S.dma("gpsimd", k_dma(wdt[:], wd_in[l, el].rearrange("(fc p) d -> p fc d", p=128)), writes=["wdt"])
                        for bb in range(2):
                            for sbk in range(NSB):
                                i = sbk % 2
                                S.dma("sync", k_dma(xr[i][:], xg[l][bb * 2 + el][sbk * 128:(sbk + 1) * 128, :]),
                                      reads=[f"xg{bb * 2 + el}_{g_}_{j_}" for g_ in range(4) for j_ in range(NB)], writes=[f"xr{i}"])
                                S.op("vector", k_tt(t16[:], xr[i][:, D:ROWW].bitcast(F32), C("gsel", el * 16, 16), ALU.mult),
                                     reads=[f"xr{i}", "cst"], writes=["t16"])
                                S.op("vector", lambda e, o=gates[:, bb, sbk:sbk + 1], a=t16[:]: e.reduce_sum(out=o, in_=a, axis=AX.X),
                                     reads=["t16"], writes=["gates"])
                                transpose_to(xr[i], f"xr{i}", xgT[bb], f"xgT{bb}", sbk * 128, ptr, "ptre", eng="vector")
                        for fc in range(NFC):
                            wi = fc % 3
                            S.dma("gpsimd", k_dma(wgf[wi][:], wg_in[l, el][:, fc * 128:(fc + 1) * 128].rearrange("(kc p) f -> p kc f", p=128)),
                                  writes=[f"wgf{wi}"])
                            S.dma("gpsimd", k_dma(wuf[wi][:], wu_in[l, el][:, fc * 128:(fc + 1) * 128].rearrange("(kc p) f -> p kc f", p=128)),
                                  writes=[f"wuf{wi}"])
                            for bb in range(2):
                                for st in range(NST):
                                    pi = (bb * NST + st) % 2
                                    cols = slice(st * NN, (st + 1) * NN)
                                    for kc in range(8):
                                        S.op("tensor", k_mm(pa[pi][:, 0:NN], wgf[wi][:, kc, :], xgT[bb][:, kc, cols], kc == 0, kc == 7),
                                             reads=[f"wgf{wi}", f"xgT{bb}"], writes=[f"pa{pi}"])
                                    for kc in range(8):
                                        S.op("tensor", k_mm(pu[pi][:, 0:NN], wuf[wi][:, kc, :], xgT[bb][:, kc, cols], kc == 0, kc == 7),
                                             reads=[f"wuf{wi}", f"xgT{bb}"], writes=[f"pu{pi}"])
                                    S.op("scalar", k_act(sa[pi][:, 0:NN], pa[pi][:, 0:NN], AF.Silu), reads=[f"pa{pi}"], writes=[f"sa{pi}"])
                                    S.op("vector", k_tt(actT[bb][:, fc, cols], sa[pi][:, 0:NN], pu[pi][:, 0:NN], ALU.mult),
                                         reads=[f"sa{pi}", f"pu{pi}"], writes=[f"actT{bb}"])
                        for bb in range(2):
                            for sbk in range(NSB):
                                yi = sbk % 2
                                for hf in range(2):
                                    for fc in range(NFC):
                                        S.op("tensor", k_mm(py[hf][:], actT[bb][:, fc, sbk * 128:(sbk + 1) * 128], wdt[:, fc, hf * 512:(hf + 1) * 512],
                                                            fc == 0, fc == NFC - 1), reads=[f"actT{bb}", "wdt"], writes=[f"py{hf}"])
                                    S.op("scalar", k_act(yrow[yi][:, hf * 512:(hf + 1) * 512], py[hf][:], AF.Copy, scale=gates[:, bb, sbk:sbk + 1]),
                                         reads=[f"py{hf}", "gates"], writes=[f"yrow{yi}"])
                                r0 = (bb * 2 + el) * CAPP + sbk * 128
                                S.dma("sync", k_dma(y_loc[l][r0:r0 + 128, :], yrow[yi][:]), reads=[f"yrow{yi}"], writes=[f"y_loc{bb}_{el}_{sbk}"])
                    S.barrier()
                    S.emit()
                    if cfg.stop == stop_base + 2:
                        finish()
                        return True

                with ExitStack() as ph:
                    BC = sb(ph, "BCr", [128, 1, D], F32)
                    pb = [ps(ph, f"pbr{i}", [128, 512], F32) for i in range(2)]
                    Yt = [sb(ph, f"Yt{e}", [128, D], BF16) for e in range(NE)]
                    dg = [sb(ph, f"dg{i}", [128, 128], BF16) for i in range(4)]
                    xt = [sb(ph, f"xtr{i}", [128, D], F32) for i in range(2)]
                    tmpy = sb(ph, "tmpyr", [128, 512], F32)
                    S.coll(k_ag(y_loc[l], y_all[l]), reads=[], writes=["y_all"])
                    bcast_rows(BC, pb, ada(l, 5), 0, "ADAo", pk=("pbr0", "pbr1"))
                    for e in range(NE):
                        S.op("gpsimd", k_memset(Yt[e][:], 0.0), writes=[f"Yt{e}"])
                    nd = 0
                    for j in range(NB):
                        i = j % 2
                        blk = slice(j * 128, (j + 1) * 128)
                        S.dma("sync", k_dma(xt[i][:], xres[blk, :]), reads=[f"xres{j}"], writes=[f"xtr{i}"])
                        for e in range(NE):
                            S.dma("gpsimd", k_gather(Yt[e][:], y_all[l], destSel[:, j, 16 + e:17 + e], 8 * 4 * CAPP - 1),
                                  reads=["destSel", "y_all"], writes=[f"Yt{e}"])
                            di = nd % 4
                            nd += 1
                            S.op("vector", k_ts(dg[di][:], identb[:], selm[:, j, e:e + 1], None, ALU.mult),
                                 reads=["identb", "selm"], writes=[f"dg{di}"])
                            for hf in range(2):
                                S.op("tensor", k_mm(pb[hf][:], dg[di][:], Yt[e][:, hf * 512:(hf + 1) * 512], e == 0, e == NE - 1),
                                     reads=[f"dg{di}", f"Yt{e}"], writes=[f"pbr{hf}"])
                        for hf in range(2):
                            S.op("vector", k_tt(tmpy[:], pb[hf][:], BC[:, 0, hf * 512:(hf + 1) * 512], ALU.mult),
                                 reads=[f"pbr{hf}", "BC0"], writes=["tmpyr"])
                            S.op("vector", k_tt(xt[i][:, hf * 512:(hf + 1) * 512], xt[i][:, hf * 512:(hf + 1) * 512], tmpy[:], ALU.add),
                                 reads=["tmpyr", f"xtr{i}"], writes=[f"xtr{i}"])
                        S.dma("sync", k_dma(xres[blk, :], xt[i][:]), reads=[f"xtr{i}"], writes=[f"xres{j}"])
                        if cfg.debug:
                            S.dma("sync", k_dma(dbg[dbg_name][blk, :], xt[i][:]), reads=[f"xtr{i}"], writes=[dbg_name])
                    S.barrier()
                    S.emit()
                    if cfg.stop == stop_base + 3:
                        finish()
                        return True
            return False

        if moe_layer(0, 4, "d_xmoe0"):
            return nc
        with ExitStack() as ph:
            hT = sb(ph, "hTc", [128, 8, NT], BF16)
            zT = sb(ph, "zT", [128, 8, NT], BF16)
            BC = sb(ph, "BCc", [128, 3, D], F32)
            pb = [ps(ph, f"pbc{i}", [128, 512], F32) for i in range(2)]
            ptr = ps(ph, "ptrc", [128, 1024], BF16)
            pc_ = [ps(ph, f"pcc{i}", [128, 512], F32) for i in range(2)]
            pu_ = [ps(ph, f"puc{i}", [128, 512], F32) for i in range(2)]
            xt = [sb(ph, f"xtc{i}", [128, D], F32) for i in range(2)]
            hb = [sb(ph, f"hbc{i}", [128, D], BF16) for i in range(2)]
            sqj = [sb(ph, f"sqjc{i}", [128, D], F32) for i in range(2)]
            ss = [sb(ph, f"ssc{i}", [128, 1], F32) for i in range(2)]
            rs = [sb(ph, f"rsc{i}", [128, 2], F32) for i in range(2)]
            w3 = [[sb(ph, f"w3{n}{i}", [128, 8, 128], BF16) for n in range(3)] for i in range(2)]
            wo = sb(ph, "woc", [128, 8, D], BF16)
            cu2 = sb(ph, "cu2", [128, 16], F32)
            c2s = sb(ph, "c2s", [128, 2], F32)
            hl = sb(ph, "hl", [128, 8, 16], F32)
            t88 = sb(ph, "t88", [128, 8, 8], F32)
            halo = sb(ph, "halo", [128, 2, 8], F32)
            cup = sb(ph, "cup", [128, NT + 2], F32)
            ycv = sb(ph, "ycv", [128, NT], F32)
            cs_ = sb(ph, "cs_", [128, 512], F32)
            tmpy = sb(ph, "tmpyc", [128, 512], F32)
            G1 = make_gain(ph, "G1c1", ada(1, 1), C("nmix", 8, 8), ["ADAo", "cst"])
            bcast_rows(BC, pb, G1, 0, "G1c1", pk=("pbc0", "pbc1"))
            bcast_rows(BC, pb, ada(1, 0), 1, "ADAo", pk=("pbc0", "pbc1"))
            bcast_rows(BC, pb, ada(1, 2), 2, "ADAo", pk=("pbc0", "pbc1"))
            S.dma("gpsimd", k_dma(wo[:], scwout_in.rearrange("(kc p) n -> p kc n", p=128)), writes=["woc"])
            for j in range(NB):
                i = j % 2
                blk = slice(j * 128, (j + 1) * 128)
                S.dma("sync", k_dma(xt[i][:], xres[blk, :]), reads=[f"xres{j}"], writes=[f"xtc{i}"])
                prenorm(BC, xt[i][:], f"xtc{i}", 0, 1, hb[i][:], f"hbc{i}", ss[i], rs[i], sqj[i], sfx=f"c{i}")
                transpose_to(hb[i], f"hbc{i}", hT, "hTc", j * 128, ptr, "ptrc")

            def load_w3(dc, i):
                for n in range(3):
                    S.dma("gpsimd", k_dma(w3[i][n][:], scwin_in[:, n * D + dc * 128:n * D + (dc + 1) * 128]
                                          .rearrange("(kc p) n -> p kc n", p=128)), writes=[f"w3{n}{i}"])

            for dc in range(8):
                i = dc % 2
                load_w3(dc, i)
                for kc in range(8):
                    S.op("tensor", k_mm(pc_[0][:, 0:2], w3[i][1][:, kc, :], hT[:, kc, 0:NT:NT - 1], kc == 0, kc == 7),
                         reads=[f"w31{i}", "hTc"], writes=["pcc0"])
                for kc in range(8):
                    S.op("tensor", k_mm(pu_[0][:, 0:2], w3[i][2][:, kc, :], hT[:, kc, 0:NT:NT - 1], kc == 0, kc == 7),
                         reads=[f"w32{i}", "hTc"], writes=["puc0"])
                S.op("scalar", k_act(c2s[:], pc_[0][:, 0:2], AF.Copy), reads=["pcc0"], writes=["c2s"])
                S.op("vector", k_tt(cu2[:, dc * 2:dc * 2 + 2], c2s[:], pu_[0][:, 0:2], ALU.mult), reads=["c2s", "puc0"], writes=["cu2"])
            S.dma("sync", k_dma(halo_loc, cu2[:]), reads=["cu2"], writes=["halo_loc"])
            S.coll(k_ag(halo_loc, halo_all), reads=["halo_loc"], writes=["halo_all"])
            S.dma("sync", k_dma(hl[:], halo_all.rearrange("(r p) n -> p r n", p=128)), reads=["halo_all"], writes=["hl"])
            for side, (sname, off) in enumerate((("selL", 1), ("selR", 0))):
                S.op("vector", k_tt(t88[:], hl[:, :, off:16:2].rearrange("p r c -> p c r"),
                                    C(sname).unsqueeze(1).to_broadcast([128, 8, 8]), ALU.mult), reads=["hl", "cst"], writes=["t88"])
                S.op("vector", lambda e, o=halo[:, side, :], a=t88[:]: e.reduce_sum(out=o, in_=a, axis=AX.X),
                     reads=["t88"], writes=["halo"])
            NN = min(512, NT)
            NST = NT // NN
            for dc in range(8):
                i = dc % 2
                load_w3(dc, i)
                for st in range(NST):
                    cols = slice(st * NN, (st + 1) * NN)
                    pi = st % 2
                    for kc in range(8):
                        S.op("tensor", k_mm(pc_[pi][:, 0:NN], w3[i][1][:, kc, :], hT[:, kc, cols], kc == 0, kc == 7),
                             reads=[f"w31{i}", "hTc"], writes=[f"pcc{pi}"])
                    for kc in range(8):
                        S.op("tensor", k_mm(pu_[pi][:, 0:NN], w3[i][2][:, kc, :], hT[:, kc, cols], kc == 0, kc == 7),
                             reads=[f"w32{i}", "hTc"], writes=[f"puc{pi}"])
                    S.op("scalar", k_act(cs_[:, 0:NN], pc_[pi][:, 0:NN], AF.Copy), reads=[f"pcc{pi}"], writes=["cs_"])
                    S.op("vector", k_tt(cup[:, 1 + st * NN:1 + (st + 1) * NN], cs_[:, 0:NN], pu_[pi][:, 0:NN], ALU.mult),
                         reads=["cs_", f"puc{pi}"], writes=["cup"])
                S.op("vector", k_copy(cup[:, 0:1], halo[:, 0, dc:dc + 1]), reads=["halo"], writes=["cup"])
                S.op("vector", k_copy(cup[:, NT + 1:NT + 2], halo[:, 1, dc:dc + 1]), reads=["halo"], writes=["cup"])
                S.op("vector", k_ts(ycv[:], cup[:, 0:NT], C("scw", 0 * 8 + dc, 1), None, ALU.mult), reads=["cup", "cst"], writes=["ycv"])
                S.op("vector", k_stt(ycv[:], cup[:, 1:NT + 1], C("scw", 1 * 8 + dc, 1), ycv[:], ALU.mult, ALU.add),
                     reads=["cup", "cst", "ycv"], writes=["ycv"])
                S.op("vector", k_stt(ycv[:], cup[:, 2:NT + 2], C("scw", 2 * 8 + dc, 1), ycv[:], ALU.mult, ALU.add),
                     reads=["cup", "cst", "ycv"], writes=["ycv"])
                for st in range(NST):
                    cols = slice(st * NN, (st + 1) * NN)
                    pi = st % 2
                    for kc in range(8):
                        S.op("tensor", k_mm(pb[pi][:, 0:NN], w3[i][0][:, kc, :], hT[:, kc, cols], kc == 0, kc == 7),
                             reads=[f"w30{i}", "hTc"], writes=[f"pbc{pi}"])
                    S.op("vector", k_tt(zT[:, dc, cols], ycv[:, cols], pb[pi][:, 0:NN], ALU.mult), reads=["ycv", f"pbc{pi}"], writes=["zT"])
            for jb in range(NB):
                i = jb % 2
                blk = slice(jb * 128, (jb + 1) * 128)
                S.dma("sync", k_dma(xt[i][:], xres[blk, :]), reads=[f"xres{jb}"], writes=[f"xtc{i}"])
                for hf in range(2):
                    for kc in range(8):
                        S.op("tensor", k_mm(pb[hf][:], zT[:, kc, blk], wo[:, kc, hf * 512:(hf + 1) * 512], kc == 0, kc == 7),
                             reads=["zT", "woc"], writes=[f"pbc{hf}"])
                    S.op("vector", k_tt(tmpy[:], pb[hf][:], BC[:, 2, hf * 512:(hf + 1) * 512], ALU.mult),
                         reads=[f"pbc{hf}", "BC2"], writes=["tmpyc"])
                    S.op("vector", k_tt(xt[i][:, hf * 512:(hf + 1) * 512], xt[i][:, hf * 512:(hf + 1) * 512], tmpy[:], ALU.add),
                         reads=["tmpyc", f"xtc{i}"], writes=[f"xtc{i}"])
                S.dma("sync", k_dma(xres[blk, :], xt[i][:]), reads=[f"xtc{i}"], writes=[f"xres{jb}"])
                if cfg.debug:
                    S.dma("sync", k_dma(dbg["d_xmix1"][blk, :], xt[i][:]), reads=[f"xtc{i}"], writes=["d_xmix1"])
            S.barrier()
            S.emit()
            if cfg.stop == 8:
                finish()
                return nc

        if moe_layer(1, 9, "d_xmoe1"):
            return nc

        with ExitStack() as ph:
            BC = sb(ph, "BCf", [128, 1, D], F32)
            pb = [ps(ph, f"pbf{i}", [128, 512], F32) for i in range(2)]
            xt = [sb(ph, f"xtf{i}", [128, D], F32) for i in range(2)]
            ot = [sb(ph, f"otf{i}", [128, D], F32) for i in range(2)]
            sqj = [sb(ph, f"sqjf{i}", [128, D], F32) for i in range(2)]
            ss = [sb(ph, f"ssf{i}", [128, 1], F32) for i in range(2)]
            rs = [sb(ph, f"rsf{i}", [128, 2], F32) for i in range(2)]
            bcast_rows(BC, pb, C("nfin"), 0, "cst")
            for j in range(NB):
                i = j % 2
                blk = slice(j * 128, (j + 1) * 128)
                S.dma("sync", k_dma(xt[i][:], xres[blk, :]), reads=[f"xres{j}"], writes=[f"xtf{i}"])
                S.op("scalar", k_act(sqj[i][:], xt[i][:], AF.Square, accum=ss[i][:, 0:1]), reads=[f"xtf{i}"], writes=[f"sqjf{i}", f"ssf{i}"])
                S.op("scalar", k_act(rs[i][:, 0:1], ss[i][:, 0:1], AF.Sqrt, scale=1.0 / D, bias=epsb[:, 0:1]), reads=[f"ssf{i}", "epsb"], writes=[f"rsf{i}"])
                S.op("vector", k_recip(rs[i][:, 1:2], rs[i][:, 0:1]), reads=[f"rsf{i}"], writes=[f"rsf2{i}"])
                S.op("vector", k_stt(ot[i][:], xt[i][:], rs[i][:, 1:2], BC[:, 0, :], ALU.mult, ALU.mult),
                     reads=[f"xtf{i}", f"rsf2{i}", "BC0"], writes=[f"otf{i}"])
                S.dma("sync", k_dma(out_ap[blk, :], ot[i][:]), reads=[f"otf{i}"], writes=[f"out{j}"])
        S.barrier()
        S.emit()
        finish()
    return nc


def _pos_table(T):
    rows = T // 64
    row = np.repeat(np.arange(rows), 64).astype(np.float32)
    col = np.tile(np.arange(64), rows).astype(np.float32)
    n_freq = D // 4
    omega = (np.float32(10000.0) ** (-np.arange(n_freq, dtype=np.float32) / np.float32(n_freq))).astype(np.float32)

    def emb(p):
        a = (p[:, None] * omega[None, :]).astype(np.float32)
        return np.concatenate([np.sin(a), np.cos(a)], axis=-1)
    return np.concatenate([emb(row), emb(col)], axis=-1).astype(np.float32)


def _consts(core, cfg, inp):
    b, q = core // 4, core % 4
    CAPP = cfg.CAPP
    cs = {}
    cs["ident"] = np.eye(128, dtype=np.float32)
    s = np.arange(128)[:, None]
    t = np.arange(128)[None, :]
    same = (s // 64) == (t // 64)
    cs["maskf"] = (same & (s <= t)).astype(np.float32)
    cs["maskb"] = (same & (s >= t)).astype(np.float32)
    pb_, pg_, pe_ = s // 64, (s % 64) // 16, s % 16
    qb_, qg_, qe_ = t // 64, (t % 64) // 16, t % 16
    samepair = (pb_ == qb_) & (pe_ == qe_)
    cs["Bm"] = samepair.astype(np.float32)
    cs["Tm"] = (samepair & (pg_ < qg_)).astype(np.float32)
    cs["hgn"] = np.broadcast_to(np.asarray(inp["hg_norm"][0], np.float32)[None, :], (128, 128)).copy()
    sel = np.zeros((128, 32), np.float32)
    for bb in range(2):
        for g in range(4):
            for el in range(2):
                sel[bb * 64 + g * 16 + 2 * core + el, bb * 8 + g * 2 + el] = 1.0
    rb = np.zeros((128, 32), np.float32)
    for e in range(16):
        sel[b * 64 + q * 16 + e, 16 + e] = 1.0
        rb[:, 16 + e] = (e // 2) * 4 * CAPP + (b * 2 + e % 2) * CAPP
    cs["sel"] = sel
    cs["retbase"] = rb
    gs = np.zeros((128, 32), np.float32)
    for el in range(2):
        gs[:, el * 16 + 2 * core + el] = 1.0
    cs["gsel"] = gs
    bs = np.zeros((128, 2), np.float32)
    bs[:, b] = 1.0
    cs["bsel"] = bs
    mf = np.zeros((128, 8), np.float32)
    mb = np.zeros((128, 8), np.float32)
    sl = np.zeros((128, 8), np.float32)
    sr = np.zeros((128, 8), np.float32)
    for r in range(8):
        if r // 4 == b and r % 4 < q:
            mf[:, r] = 1.0
        if r // 4 == b and r % 4 > q:
            mb[:, r] = 1.0
    if q > 0:
        sl[:, core - 1] = 1.0
    if q < 3:
        sr[:, core + 1] = 1.0
    cs["mf"], cs["mb"], cs["selL"], cs["selR"] = mf, mb, sl, sr

    def fm(v):
        return np.asarray(v, np.float32).reshape(8, 128).T

    cs["nmix"] = np.concatenate([fm(inp["norm_mix"][l]) for l in range(2)], axis=1)
    cs["nffn"] = np.concatenate([fm(inp["norm_ffn"][l]) for l in range(2)], axis=1)
    cs["nfin"] = fm(inp["norm_final"])
    cs["lbl"] = np.concatenate([fm(inp["hg_lb_logits"][i]) for i in range(3)], axis=1)
    cs["scw"] = np.concatenate([fm(inp["sc_conv"][0][j]) for j in range(3)], axis=1)
    ab = np.zeros((128, 12), np.float32)
    for l in range(2):
        ab[:, l * 6:(l + 1) * 6] = np.asarray(inp["ada_b"][l][core * 768:(core + 1) * 768], np.float32).reshape(6, 128).T
    cs["adab"] = ab
    cv = np.zeros((128, 8, 4), np.float32)
    cv[:, :, 0] = fm(inp["c"][0])
    cv[:, :, 1] = fm(inp["c"][1])
    cv[:, :, 2] = fm(inp["c_ctx"])
    cs["cvT"] = cv.reshape(128, 32)
    cs["ones"] = np.ones((128, 128), np.float32)
    out = np.zeros((128, NCONST), np.float32)
    for n, (o, w) in CO.items():
        out[:, o:o + w] = cs[n]
    return out


def make_in_maps(inp, cfg):
    NT = cfg.NT
    T = 4 * NT
    pos = _pos_table(T)
    f32 = lambda a: np.ascontiguousarray(np.asarray(a, np.float32))
    shared = {
        "hg_w_in": f32(inp["hg_w_in"][0]), "hg_w_out": f32(inp["hg_w_out"][0]),
        "sc_w_in": f32(inp["sc_w_in"][0]), "sc_w_out": f32(inp["sc_w_out"][0]),
        "router": f32(inp["moe_router"]),
    }
    maps = []
    for core in range(8):
        b, q = core // 4, core % 4
        m = dict(shared)
        m["x"] = f32(inp["x"][b, q * NT:(q + 1) * NT])
        m["pos"] = f32(pos[q * NT:(q + 1) * NT])
        m["ctx"] = f32(inp["ctx"][b])
        m["consts"] = _consts(core, cfg, inp)
        m["adaw"] = f32(np.asarray(inp["ada_w"])[:, :, core * 768:(core + 1) * 768])
        m["wg"] = f32(np.asarray(inp["moe_w_gate"])[:, 2 * core:2 * core + 2])
        m["wu"] = f32(np.asarray(inp["moe_w_up"])[:, 2 * core:2 * core + 2])
        m["wd"] = f32(np.asarray(inp["moe_w_down"])[:, 2 * core:2 * core + 2])
        maps.append(m)
    return maps


_NC_CACHE = {}


def run(inp, cfg, trace=False):
    key = (cfg.NT, cfg.DEXP, cfg.stop, cfg.debug)
    if key not in _NC_CACHE:
        _NC_CACHE[key] = build(cfg)
    nc = _NC_CACHE[key]
    maps = make_in_maps(inp, cfg)
    res = run_bass_kernel_spmd(nc, maps, core_ids=list(range(8)))
    return res


def kernel(**inputs):
    cfg = Cfg(NT=np.asarray(inputs["x"]).shape[1] // 4, DEXP=np.asarray(inputs["moe_w_gate"]).shape[-1])
    res = run(inputs, cfg)
    NT = cfg.NT
    out = np.zeros((2, 4 * NT, D), np.float32)
    for core in range(8):
        b, q = core // 4, core % 4
        out[b, q * NT:(q + 1) * NT] = res.results[core]["out"]
    return out
```

```python
import numpy as np
from contextlib import ExitStack
import concourse.bass as bass
import concourse.mybir as mybir
from concourse.bass_utils import run_bass_kernel_spmd

F32 = mybir.dt.float32
BF16 = mybir.dt.bfloat16
I32 = mybir.dt.int32
AF = mybir.ActivationFunctionType
ALU = mybir.AluOpType
AX = mybir.AxisListType

D = 1024
NH = 8
NE = 16
CTXL = 256
EPS = 1e-6
BIG = 16384.0
ENGS = ["sync", "scalar", "vector", "gpsimd", "tensor"]
NDMA = 32
SAME_ENGINE_SYNC = True


class Cfg:
    def __init__(self, NT=2048, DEXP=2048, stop=99, debug=False):
        self.NT = NT
        self.DEXP = DEXP
        self.stop = stop
        self.debug = debug
        self.NB = NT // 128
        self.NCH = NT // 64
        self.CAP = NT // 2
        self.CAPP = self.CAP + 128
        self.NSB = self.CAP // 128
        self.NFC = DEXP // 128


class Sched:
    def __init__(self, nc, sems, n_dma):
        self.nc = nc
        self.ops = {e: [] for e in ENGS}
        self.cnt = {e: 0 for e in ENGS}
        self.waited = {e: {} for e in ENGS}
        self.last_w = {}
        self.readers = {}
        self.esem = {"scalar": sems[0], "vector": sems[1], "gpsimd": sems[2], "tensor": sems[3]}
        self.dsem = list(sems[4:4 + n_dma])
        self.csem = sems[4 + n_dma]
        self.cval = 0
        self.dval = [0] * n_dma
        self.drr = 0
        self.grr = 0
        self.n_g = 12

    def _semh(self, key):
        if isinstance(key, str):
            return self.esem[key]
        return self.csem if key[0] == "c" else self.dsem[key[1]]

    def _need(self, eng, ev, waits):
        if ev is None:
            return
        key, val = ev
        if key == eng and (eng == "tensor" or not SAME_ENGINE_SYNC):
            return
        if self.waited[eng].get(key, 0) >= val:
            return
        self.waited[eng][key] = val
        waits.append((self._semh(key), val))

    def _hazards(self, eng, reads, writes, waits):
        for r in reads:
            self._need(eng, self.last_w.get(r), waits)
        for w in writes:
            self._need(eng, self.last_w.get(w), waits)
            for ev in self.readers.get(w, ()):
                self._need(eng, ev, waits)

    def _commit(self, ev, reads, writes):
        for r in reads:
            self.readers.setdefault(r, []).append(ev)
        for w in writes:
            self.last_w[w] = ev
            self.readers[w] = []

    def op(self, eng, fn, reads=(), writes=()):
        waits = []
        self._hazards(eng, reads, writes, waits)
        self.cnt[eng] += 1
        ev = (eng, self.cnt[eng])
        self.ops[eng].append((waits, fn, self.esem[eng], 1))
        self._commit(ev, reads, writes)
        return ev

    def dma(self, eng, fn, reads=(), writes=(), inc=16):
        waits = []
        self._hazards(eng, reads, writes, waits)
        if eng == "gpsimd":
            i = self.grr
            self.grr = (self.grr + 1) % self.n_g
        else:
            i = self.n_g + self.drr
            self.drr = (self.drr + 1) % (len(self.dsem) - self.n_g)
        key = ("d", i)
        if self.dval[i] > 0:
            self._need(eng, (key, self.dval[i]), waits)
        self.dval[i] += inc
        ev = (key, self.dval[i])
        self.ops[eng].append((waits, fn, self.dsem[i], inc))
        self._commit(ev, reads, writes)
        return ev

    def coll(self, fn, reads=(), writes=()):
        waits = []
        self._hazards("gpsimd", reads, writes, waits)
        self.cval += 1
        ev = (("c", 0), self.cval)
        self.ops["gpsimd"].append((waits, fn, self.csem, 1))
        self._commit(ev, reads, writes)
        return ev

    def barrier(self):
        for eng in ENGS:
            waits = []
            for k in self.esem:
                if self.cnt[k] > 0:
                    self._need(eng, (k, self.cnt[k]), waits)
            for i in range(len(self.dsem)):
                if self.dval[i] > 0:
                    self._need(eng, (("d", i), self.dval[i]), waits)
            if self.cval > 0:
                self._need(eng, (("c", 0), self.cval), waits)
            self.ops[eng].append((waits, None, None, 0))
        self.last_w = {}
        self.readers = {}

    def emit(self):
        nc = self.nc
        ops = self.ops
        self.ops = {e: [] for e in ENGS}
        with nc.Block() as block:
            def run(e, lst):
                for waits, fn, sem, inc in lst:
                    for s, v in waits:
                        e.wait_ge(s, v)
                    if fn is not None:
                        fn(e).then_inc(sem, inc)

            @block.sync
            def _(e):
                run(e, ops["sync"])

            @block.scalar
            def _(e):
                run(e, ops["scalar"])

            @block.vector
            def _(e):
                run(e, ops["vector"])

            @block.gpsimd
            def _(e):
                run(e, ops["gpsimd"])

            @block.tensor
            def _(e):
                run(e, ops["tensor"])


CONST_SPEC = [("ident", 128), ("maskf", 128), ("maskb", 128), ("Bm", 128), ("Tm", 128), ("hgn", 128),
              ("sel", 32), ("retbase", 32), ("gsel", 32), ("bsel", 2), ("mf", 8), ("mb", 8),
              ("selL", 8), ("selR", 8), ("nmix", 16), ("nffn", 16), ("nfin", 8), ("lbl", 24),
              ("scw", 24), ("adab", 12), ("cvT", 32), ("ones", 128)]
CO = {}
_o = 0
for _n, _w in CONST_SPEC:
    CO[_n] = (_o, _w)
    _o += _w
NCONST = _o


def k_dma(out, in_):
    return lambda e: e.dma_start(out=out, in_=in_)


def k_mm(out, lhsT, rhs, start, stop):
    return lambda e: e.matmul(out, lhsT=lhsT, rhs=rhs, start=start, stop=stop)


def k_tr(out, in_, ident):
    return lambda e: e.transpose(out, in_, ident)


def k_act(out, in_, func, scale=1.0, bias=None, accum=None):
    def f(e):
        kw = {}
        if bias is not None:
            kw["bias"] = bias
        if accum is not None:
            kw["accum_out"] = accum
        return e.activation(out=out, in_=in_, func=func, scale=scale, **kw)
    return f


def k_ts(out, in0, s1, s2, op0, op1=None, accum=None):
    def f(e):
        kw = {}
        if op1 is not None:
            kw["op1"] = op1
        if accum is not None:
            kw["accum_out"] = accum
        return e.tensor_scalar(out=out, in0=in0, scalar1=s1, scalar2=s2, op0=op0, **kw)
    return f


def k_tt(out, in0, in1, op):
    return lambda e: e.tensor_tensor(out=out, in0=in0, in1=in1, op=op)


def k_stt(out, in0, scalar, in1, op0, op1):
    return lambda e: e.scalar_tensor_tensor(out=out, in0=in0, scalar=scalar, in1=in1, op0=op0, op1=op1)


def k_copy(out, in_):
    return lambda e: e.tensor_copy(out=out, in_=in_)


def k_memset(ap, v):
    return lambda e: e.memset(ap, v)


def k_scan(out, d0, d1, init, op0, op1):
    return lambda e: e.tensor_tensor_scan(out=out, data0=d0, data1=d1, initial=init, op0=op0, op1=op1)


def k_recip(out, in_):
    return lambda e: e.reciprocal(out=out, in_=in_)


def k_ag(in_, out):
    return lambda e: e.collective_compute("AllGather", ALU.bypass, replica_groups=[list(range(8))],
                                          ins=[in_], outs=[out])


BREG = {}


def _breg(e, bound):
    if bound not in BREG:
        BREG[bound] = e.to_reg(bound)
    return BREG[bound]


def k_scatter(out, idx, in_, bound):
    return lambda e: e.indirect_dma_start(out=out, out_offset=bass.IndirectOffsetOnAxis(ap=idx, axis=0),
                                          in_=in_, in_offset=None, bounds_check=_breg(e, bound), oob_is_err=False)


def k_gather(out, in_, idx, bound):
    return lambda e: e.indirect_dma_start(out=out, out_offset=None, in_=in_,
                                          in_offset=bass.IndirectOffsetOnAxis(ap=idx, axis=0),
                                          bounds_check=_breg(e, bound), oob_is_err=False)


def build(cfg):
    NT, NB, NCH, CAP, CAPP, NSB, DEXP, NFC = cfg.NT, cfg.NB, cfg.NCH, cfg.CAP, cfg.CAPP, cfg.NSB, cfg.DEXP, cfg.NFC
    nc = bass.Bass("TRN2", target_bir_lowering=False)
    BREG.clear()

    def din(name, shape, dt=F32):
        return nc.dram_tensor(name, list(shape), dt, kind="ExternalInput").ap()

    def dscr(name, shape, dt=F32):
        return nc.dram_tensor(name, list(shape), dt).ap()

    x_in = din("x", [NT, D])
    pos_in = din("pos", [NT, D])
    ctx_in = din("ctx", [CTXL, D])
    consts_in = din("consts", [128, NCONST])
    adaw_in = din("adaw", [2, D, 768])
    hgwin_in = din("hg_w_in", [D, 5 * D])
    hgwout_in = din("hg_w_out", [D, D])
    scwin_in = din("sc_w_in", [D, 3 * D])
    scwout_in = din("sc_w_out", [D, D])
    router_in = din("router", [2, D, NE])
    wg_in = din("wg", [2, 2, D, DEXP])
    wu_in = din("wu", [2, 2, D, DEXP])
    wd_in = din("wd", [2, 2, DEXP, D])
    out_ap = nc.dram_tensor("out", [NT, D], F32, kind="ExternalOutput").ap()
    dbg = {}
    if cfg.debug:
        for nm, shp in (("d_xmix0", [NT, D]), ("d_xmoe0", [NT, D]), ("d_xmix1", [NT, D]), ("d_xmoe1", [NT, D]), ("d_h1", [NT, D]),
                        ("d_o", [NT, D]), ("d_aff", [NE, NT]), ("d_dest", [128, NB * 32])):
            dbg[nm] = nc.dram_tensor(nm, shp, F32, kind="ExternalOutput").ap()

    xres = dscr("xres", [NT, D])
    ada_loc = dscr("ada_loc", [128, 48])
    ada_all = dscr("ada_all", [8 * 128, 48])
    qseg_scr = dscr("qseg_scr", [NH, 2, 128, NT], BF16)
    o_scr = dscr("o_scr", [NH, 128, NB * 128])
    st_loc = dscr("st_loc", [128, NH * 258])
    st_all = dscr("st_all", [8 * 128, NH * 258])
    aff_loc = [dscr(f"aff_loc{l}", [NE, NT]) for l in range(2)]
    aff_all = [dscr(f"aff_all{l}", [8 * NE, NT]) for l in range(2)]
    ROWW = D + 32
    h2_loc = [dscr(f"h2_loc{l}", [NT, ROWW], BF16) for l in range(2)]
    h2_all = [dscr(f"h2_all{l}", [8 * NT, ROWW], BF16) for l in range(2)]
    xg = [[dscr(f"xg{l}_{i}", [CAPP, ROWW], BF16) for i in range(4)] for l in range(2)]
    y_loc = [[dscr(f"y_loc{l}_{el}", [2 * CAPP, D], BF16) for el in range(2)] for l in range(2)]
    y_all = [[dscr(f"y_all{l}_{el}", [8 * 2 * CAPP, D], BF16) for el in range(2)] for l in range(2)]
    halo_loc = dscr("halo_loc", [128, 16])
    halo_all = dscr("halo_all", [8 * 128, 16])

    with ExitStack() as top:
        uid = [0]

        def sb(es, name, shape, dt):
            uid[0] += 1
            return es.enter_context(nc.sbuf_tensor(f"{name}_u{uid[0]}", list(shape), dt))

        def ps(es, name, shape, dt):
            uid[0] += 1
            return es.enter_context(nc.psum_tensor(f"{name}_u{uid[0]}", list(shape), dt))

        sems = [top.enter_context(nc.semaphore(f"s{i}")) for i in range(5 + NDMA)]
        S = Sched(nc, sems, NDMA)
        for s_ in sems:
            nc.gpsimd.sem_clear(s_)
        nc.all_engine_barrier()

        def finish():
            nc.all_engine_barrier()
            for s_ in sems:
                nc.gpsimd.sem_clear(s_)
            nc.all_engine_barrier()
        cst = sb(top, "cst", [128, NCONST], F32)

        def C(name, lo=0, n=None):
            o, w = CO[name]
            n = w - lo if n is None else n
            return cst[:, o + lo:o + lo + n]

        identb = sb(top, "identb", [128, 128], BF16)
        ADAo = sb(top, "ADAo", [128, 2 * 48], F32)
        ADAx = sb(top, "ADAx", [128, 48], F32)
        lbt = sb(top, "lbt", [128, 16], F32)
        S.dma("sync", k_dma(cst[:], consts_in), writes=["cst"])
        S.op("vector", k_copy(identb[:], C("ident")), reads=["cst"], writes=["identb"])
        ident = C("ident")

        def bcast_rows(BC, es_ps, col, slot, tag, pk=("pb0", "pb1")):
            pb = es_ps
            for dc in range(8):
                cb = colbc[dc % 2]
                S.op("vector", k_ts(cb[:], C("ones"), col[:, dc:dc + 1], None, ALU.mult),
                     reads=["cst", tag], writes=[f"colbc{dc % 2}"])
                S.op("tensor", k_mm(pb[dc // 4][:, (dc % 4) * 128:(dc % 4 + 1) * 128], cb[:], ident, True, True),
                     reads=[f"colbc{dc % 2}", "cst"], writes=[pk[dc // 4]])
            for hlf in range(2):
                S.op("scalar", k_act(BC[:, slot, hlf * 512:(hlf + 1) * 512], pb[hlf][:], AF.Copy),
                     reads=[pk[hlf]], writes=[f"BC{slot}"])

        def prenorm(BC, xt, xkey, gslot, sslot, hb, hbkey, ss, rs, sqj, sfx=""):
            S.op("scalar", k_act(sqj[:], xt, AF.Square, accum=ss[:, 0:1]), reads=[xkey], writes=["sqj" + sfx, "ss" + sfx])
            S.op("scalar", k_act(rs[:, 0:1], ss[:, 0:1], AF.Sqrt, scale=1.0 / D, bias=epsb[:, 0:1]),
                 reads=["ss" + sfx, "epsb"], writes=["rs" + sfx])
            S.op("vector", k_recip(rs[:, 1:2], rs[:, 0:1]), reads=["rs" + sfx], writes=["rs2" + sfx])
            S.op("vector", k_stt(sqj[:], xt, rs[:, 1:2], BC[:, gslot, :], ALU.mult, ALU.mult),
                 reads=[xkey, "rs2" + sfx, f"BC{gslot}"], writes=["sqj" + sfx])
            S.op("vector", k_tt(hb, sqj[:], BC[:, sslot, :], ALU.add), reads=["sqj" + sfx, f"BC{sslot}"], writes=[hbkey])

        def transpose_to(hb, hbkey, dstT, dkey, col0, ptile, pkey, eng="scalar"):
            for kc in range(8):
                S.op("tensor", k_tr(ptile[:, kc * 128:(kc + 1) * 128], hb[:, kc * 128:(kc + 1) * 128], identb[:]),
                     reads=[hbkey, "identb"], writes=[pkey])
            src = ptile[:].rearrange("p (k n) -> p k n", k=8)
            if eng == "scalar":
                S.op("scalar", k_act(dstT[:, :, col0:col0 + 128], src, AF.Copy), reads=[pkey], writes=[dkey])
            else:
                S.op("vector", k_copy(dstT[:, :, col0:col0 + 128], src), reads=[pkey], writes=[dkey])

        colbc = [sb(top, f"colbc{i}", [128, 128], F32) for i in range(2)]
        epsb = sb(top, "epsb", [128, 1], F32)
        S.op("gpsimd", k_memset(epsb[:], EPS), writes=["epsb"])

        with ExitStack() as ph:
            W = sb(ph, "adaW", [128, 8, 768], F32)
            scv = sb(ph, "scv", [128, 32], F32)
            adaloc = sb(ph, "adaloc", [128, 48], F32)
            adaA = sb(ph, "adaA", [128, 8, 48], F32)
            ADAc = sb(ph, "ADAc", [128, 6 * 48], F32)
            lbe = sb(ph, "lbe", [128, 32], F32)
            pa = ps(ph, "pa", [128, 8], F32)
            S.op("scalar", k_act(scv[:], C("cvT"), AF.Silu), reads=["cst"], writes=["scv"])
            for l in range(2):
                S.dma("sync", k_dma(W[:], adaw_in[l].rearrange("(kc p) n -> p kc n", p=128)), writes=["adaW"])
                for fc in range(6):
                    for kc in range(8):
                        S.op("tensor", k_mm(pa[:, 0:4], W[:, kc, fc * 128:(fc + 1) * 128], scv[:, kc * 4:(kc + 1) * 4],
                                            kc == 0, kc == 7), reads=["adaW", "scv"], writes=["pa"])
                    c0 = (l * 6 + fc) * 4
                    S.op("vector", k_ts(adaloc[:, c0:c0 + 4], pa[:, 0:4], C("adab", l * 6 + fc, 1), None, ALU.add),
                         reads=["pa", "cst"], writes=["adaloc"])
            S.dma("sync", k_dma(ada_loc, adaloc[:]), reads=["adaloc"], writes=["ada_loc"])
            S.coll(k_ag(ada_loc, ada_all), reads=["ada_loc"], writes=["ada_all"])
            S.dma("sync", k_dma(adaA[:], ada_all.rearrange("(r p) n -> p r n", p=128)), reads=["ada_all"], writes=["adaA"])
            for l in range(2):
                for v in range(3):
                    i6 = l * 3 + v
                    S.op("vector", k_copy(ADAc[:, i6 * 48:(i6 + 1) * 48].rearrange("p (r f) -> p r f", f=6),
                                          adaA[:, :, l * 24 + v:l * 24 + 24:4]), reads=["adaA"], writes=["ADAc"])
            for l in range(2):
                S.op("vector", k_ts(ADAo[:, l * 48:(l + 1) * 48], ADAc[:, (l * 3) * 48:(l * 3 + 1) * 48],
                                    C("bsel", 0, 1), None, ALU.mult), reads=["ADAc", "cst"], writes=["ADAo"])
                S.op("vector", k_stt(ADAo[:, l * 48:(l + 1) * 48], ADAc[:, (l * 3 + 1) * 48:(l * 3 + 2) * 48],
                                     C("bsel", 1, 1), ADAo[:, l * 48:(l + 1) * 48], ALU.mult, ALU.add),
                     reads=["ADAc", "cst", "ADAo"], writes=["ADAo"])
            S.op("vector", k_copy(ADAx[:], ADAc[:, 2 * 48:3 * 48]), reads=["ADAc"], writes=["ADAx"])
            S.op("scalar", k_act(lbe[:, 0:24], C("lbl"), AF.Exp), reads=["cst"], writes=["lbe"])
            S.op("vector", k_tt(lbe[:, 24:32], lbe[:, 0:8], lbe[:, 8:16], ALU.add), reads=["lbe"], writes=["lbe"])
            S.op("vector", k_tt(lbe[:, 24:32], lbe[:, 24:32], lbe[:, 16:24], ALU.add), reads=["lbe"], writes=["lbe"])
            S.op("vector", k_recip(lbe[:, 24:32], lbe[:, 24:32]), reads=["lbe"], writes=["lbe"])
            S.op("vector", k_tt(lbt[:, 0:8], lbe[:, 0:8], lbe[:, 24:32], ALU.mult), reads=["lbe"], writes=["lbt"])
            S.op("vector", k_ts(lbt[:, 8:16], lbt[:, 0:8], -1.0, 1.0, ALU.mult, ALU.add), reads=["lbt"], writes=["lbt"])
            S.barrier()
            S.emit()
            if cfg.stop == 0:
                finish()
                return nc

        def ada(l, j):
            return ADAo[:, l * 48 + j * 8:l * 48 + (j + 1) * 8]

        def make_gain(es, name, scale_col, norm_col, tagr):
            g = sb(es, name, [128, 8], F32)
            S.op("vector", k_stt(g[:], scale_col, 1.0, norm_col, ALU.add, ALU.mult), reads=tagr, writes=[name])
            return g

        with ExitStack() as ph:
            hT = sb(ph, "hT", [128, 8, NT], BF16)
            hcT = sb(ph, "hcT", [128, 8, CTXL], BF16)
            cstate = [sb(ph, f"cstate{d}", [128, NH, 128], F32) for d in range(2)]
            pb = [ps(ph, f"pb{i}", [128, 512], F32) for i in range(2)]
            with ExitStack() as ph1:
                BC = sb(ph1, "BC", [128, 4, D], F32)
                ptr = ps(ph1, "ptr", [128, 1024], BF16)
                xt = [sb(ph1, f"xt{i}", [128, D], F32) for i in range(2)]
                pt = [sb(ph1, f"pt{i}", [128, D], F32) for i in range(2)]
                hb = [sb(ph1, f"hb{i}", [128, D], BF16) for i in range(2)]
                sqj = [sb(ph1, f"sqj{i}", [128, D], F32) for i in range(2)]
                ss = [sb(ph1, f"ss{i}", [128, 1], F32) for i in range(2)]
                rs = [sb(ph1, f"rs{i}", [128, 2], F32) for i in range(2)]
                G1 = make_gain(ph1, "G1c", ada(0, 1), C("nmix", 0, 8), ["ADAo", "cst"])
                GC = make_gain(ph1, "GCc", ADAx[:, 8:16], C("nmix", 0, 8), ["ADAx", "cst"])
                bcast_rows(BC, pb, G1, 0, "G1c")
                bcast_rows(BC, pb, ada(0, 0), 1, "ADAo")
                bcast_rows(BC, pb, GC, 2, "GCc")
                bcast_rows(BC, pb, ADAx[:, 0:8], 3, "ADAx")
                for j in range(NB):
                    i = j % 2
                    S.dma("sync", k_dma(xt[i][:], x_in[j * 128:(j + 1) * 128, :]), writes=[f"xt{i}"])
                    S.dma("sync", k_dma(pt[i][:], pos_in[j * 128:(j + 1) * 128, :]), writes=[f"pt{i}"])
                    S.op("gpsimd", k_tt(xt[i][:], xt[i][:], pt[i][:], ALU.add), reads=[f"xt{i}", f"pt{i}"], writes=[f"xt{i}"])
                    S.dma("sync", k_dma(xres[j * 128:(j + 1) * 128, :], xt[i][:]), reads=[f"xt{i}"], writes=[f"xres{j}"])
                    prenorm(BC, xt[i][:], f"xt{i}", 0, 1, hb[i][:], f"hb{i}", ss[i], rs[i], sqj[i], sfx=str(i))
                    if cfg.debug:
                        S.op("vector", k_copy(sqj[i][:], hb[i][:]), reads=[f"hb{i}"], writes=[f"sqj{i}"])
                        S.dma("sync", k_dma(dbg["d_h1"][j * 128:(j + 1) * 128, :], sqj[i][:]), reads=[f"sqj{i}"], writes=["d_h1"])
                    transpose_to(hb[i], f"hb{i}", hT, "hT", j * 128, ptr, "ptr")
                for j in range(CTXL // 128):
                    i = j % 2
                    S.dma("sync", k_dma(xt[i][:], ctx_in[j * 128:(j + 1) * 128, :]), writes=[f"xt{i}"])
                    prenorm(BC, xt[i][:], f"xt{i}", 2, 3, hb[i][:], f"hb{i}", ss[i], rs[i], sqj[i], sfx=str(i))
                    transpose_to(hb[i], f"hb{i}", hcT, "hcT", j * 128, ptr, "ptr")
                S.barrier()
                S.emit()
                if cfg.stop == 1:
                    finish()
                    return nc

            NTC = NT + CTXL
            NCHC = NTC // 64
            NCB = CTXL // 128
            with ExitStack() as phA:
                wts = [[sb(phA, f"w{n}{i}", [128, 8, 128], BF16) for n in range(4)] for i in range(2)]
                qs = sb(phA, "qs", [128, NT], F32)
                A1 = sb(phA, "A1", [128, NTC], F32)
                A2 = sb(phA, "A2", [128, NTC], F32)
                A3 = sb(phA, "A3", [128, NTC], F32)
                A4 = sb(phA, "A4", [128, NTC], F32)
                A5 = sb(phA, "A5", [128, NTC], F32)
                qd = [sb(phA, f"qd{d}", [128, NT], BF16) for d in range(2)]
                kd = [sb(phA, f"kd{d}", [128, NT], BF16) for d in range(2)]
                kd2 = [sb(phA, f"kd2{d}", [128, NTC], BF16) for d in range(2)]
                qsg = [sb(phA, f"qsg{d}", [128, NT], BF16) for d in range(2)]
                bse = sb(phA, "bse", [128, NCHC], F32)
                lastc = [sb(phA, f"lastc{d}", [128, NCHC], F32) for d in range(2)]
                elast = [sb(phA, f"elast{d}", [128, NCHC], F32) for d in range(2)]
                vh = sb(phA, "vh", [128, NB + NCB, 128], BF16)
                oloc = sb(phA, "oloc", [128, NB, 128], F32)
                S32 = [sb(phA, f"S32{d}", [128, 128], F32) for d in range(2)]
                Sbf = [sb(phA, f"Sbf{d}", [128, 128], BF16) for d in range(2)]
                ATm = [sb(phA, f"ATm{d}", [128, 128], BF16) for d in range(2)]
                k2T = [sb(phA, f"k2T{d}", [128, 128], BF16) for d in range(2)]
                stt_t = sb(phA, "stt_t", [128, NH * 258], F32)
                pA = [ps(phA, f"pA{d}", [128, 512], F32) for d in range(2)]
                pO = [ps(phA, f"pO{d}", [128, 512], F32) for d in range(2)]
                pS = [ps(phA, f"pS{d}", [128, 512], F32) for d in range(2)]
                pAt = [pA[d][:, 256:384].bitcast(BF16) for d in range(2)]
                ones_b = C("ones", 0, 1)

                def featproj(w, wkey, dst_fn):
                    tiles = [(hT, "hT", t0, min(512, NT - t0), t0) for t0 in range(0, NT, 512)]
                    tiles += [(hcT, "hcT", 0, CTXL, NT)]
                    for ti, (src, skey, t0, n, o0) in enumerate(tiles):
                        pp = pb[ti % 2]
                        for kc in range(8):
                            S.op("tensor", k_mm(pp[:, 0:n], w[:, kc, :], src[:, kc, t0:t0 + n], kc == 0, kc == 7),
                                 reads=[wkey, skey], writes=[f"pb{ti % 2}"])
                        dst_fn(pp[:, 0:n], f"pb{ti % 2}", o0, n)

                for h in range(NH):
                    wi = h % 2
                    w5 = wts[wi]
                    for n in range(4):
                        S.dma("gpsimd", k_dma(w5[n][:], hgwin_in[:, n * D + h * 128:n * D + (h + 1) * 128]
                                              .rearrange("(kc p) n -> p kc n", p=128)), writes=[f"w{n}{wi}"])
                    lb_h = lbt[:, h:h + 1]
                    oml_h = lbt[:, 8 + h:9 + h]

                    def q_dst(pp, pkey, o0, n):
                        if o0 < NT:
                            S.op("scalar", k_act(qs[:, o0:o0 + n], pp, AF.Copy, scale=128.0 ** -0.5), reads=[pkey], writes=["qs"])
                    featproj(w5[0], f"w0{wi}", q_dst)
                    for jb in range(NB + NCB):
                        src, skey, c0 = (hT, "hT", jb * 128) if jb < NB else (hcT, "hcT", (jb - NB) * 128)
                        pp = pb[jb % 2]
                        for kc in range(8):
                            S.op("tensor", k_mm(pp[:, 0:128], src[:, kc, c0:c0 + 128], w5[1][:, kc, :], kc == 0, kc == 7),
                                 reads=[skey, f"w1{wi}"], writes=[f"pb{jb % 2}"])
                        S.op("vector", k_copy(vh[:, jb, :], pp[:, 0:128]), reads=[f"pb{jb % 2}"], writes=["vh"])
                    P3 = A4[:].rearrange("p (c j) -> p c j", j=64)
                    C3 = A5[:].rearrange("p (c j) -> p c j", j=64)
                    E3 = A1[:].rearrange("p (c j) -> p c j", j=64)
                    for d in range(2):
                        def z_dst(pp, pkey, o0, n):
                            S.op("scalar", k_act(A1[:, o0:o0 + n], pp, AF.Sigmoid), reads=[pkey], writes=["A1"])
                        featproj(w5[2 + d], f"w{2 + d}{wi}", z_dst)
                        S.op("vector", k_ts(A1[:], A1[:], oml_h, lb_h, ALU.mult, ALU.add), reads=["A1", "lbt"], writes=["A1"])
                        S.op("scalar", k_act(A2[:], A1[:], AF.Ln), reads=["A1"], writes=["A2"])
                        S.op("gpsimd", k_ts(A3[:], A1[:], -1.0, 1.0, ALU.mult, ALU.add), reads=["A1"], writes=["A3"])
                        S.op("vector", k_scan(A4[:, 0:NT], ones_b.to_broadcast([128, NT]), A2[:, 0:NT], 0.0, ALU.mult, ALU.add),
                             reads=["A2", "cst"], writes=["A4"])
                        S.op("vector", k_scan(A4[:, NT:NTC], ones_b.to_broadcast([128, CTXL]), A2[:, NT:NTC], 0.0, ALU.mult, ALU.add),
                             reads=["A2", "cst"], writes=["A4"])
                        if d == 0:
                            S.op("vector", k_copy(bse[:, 1:NCHC], A4[:, 63:NTC - 1:64]), reads=["A4"], writes=["bse"])
                            S.op("vector", k_memset(bse[:, 0:1], 0.0), writes=["bse"])
                            S.op("vector", k_memset(bse[:, NCH:NCH + 1], 0.0), writes=["bse"])
                            S.op("vector", k_tt(C3, P3, bse[:].unsqueeze(2).to_broadcast([128, NCHC, 64]), ALU.subtract),
                                 reads=["A4", "bse"], writes=["A5"])
                            S.op("vector", k_copy(lastc[d][:], A5[:, 63:NTC:64]), reads=["A5"], writes=[f"lastc{d}"])
                            S.op("scalar", k_act(A1[:, 0:NT], A4[:, 0:NT], AF.Exp), reads=["A4", "A3", "A2"], writes=["A1"])
                        else:
                            S.op("vector", k_copy(bse[:], A4[:, 63:NTC:64]), reads=["A4"], writes=["bse"])
                            S.op("vector", k_tt(A1[:], A2[:], A4[:], ALU.subtract), reads=["A2", "A4", "A3"], writes=["A1"])
                            S.op("vector", k_tt(C3, E3, bse[:].unsqueeze(2).to_broadcast([128, NCHC, 64]), ALU.add),
                                 reads=["A1", "bse"], writes=["A5"])
                            S.op("vector", k_copy(lastc[d][:], A5[:, 0:NTC:64]), reads=["A5"], writes=[f"lastc{d}"])
                            S.op("scalar", k_act(A1[:, 0:NT], A1[:, 0:NT], AF.Exp, bias=A4[:, NT - 1:NT]), reads=["A1", "A4"], writes=["A1"])
                        S.op("scalar", k_act(elast[d][:], lastc[d][:], AF.Exp), reads=[f"lastc{d}"], writes=[f"elast{d}"])
                        dcol = h * 258 + 256 + d
                        S.op("vector", k_tt(qsg[d][:], qs[:], A1[:, 0:NT], ALU.mult), reads=["qs", "A1"], writes=[f"qsg{d}"])
                        S.dma("sync", k_dma(qseg_scr[h, d], qsg[d][:]), reads=[f"qsg{d}"], writes=[f"qseg_scr{h}_{d}"])
                        S.op("scalar", k_act(stt_t[:, dcol:dcol + 1], A4[:, NT - 1:NT], AF.Exp), reads=["A4"], writes=["stt_t"])
                        S.op("scalar", k_act(A1[:, 0:NT], A5[:, 0:NT], AF.Exp), reads=["A5", f"qsg{d}"], writes=["A1"])
                        S.op("vector", k_tt(qd[d][:], qs[:], A1[:, 0:NT], ALU.mult), reads=["qs", "A1"], writes=[f"qd{d}"])
                        S.op("scalar", k_act(A1[:, 0:NT], A5[:, 0:NT], AF.Exp, scale=-1.0), reads=["A5", f"qd{d}"], writes=["A1"])
                        S.op("gpsimd", k_tt(kd[d][:], A3[:, 0:NT], A1[:, 0:NT], ALU.mult), reads=["A3", "A1"], writes=[f"kd{d}"])
                        S.op("vector", k_tt(E3, lastc[d][:].unsqueeze(2).to_broadcast([128, NCHC, 64]), C3, ALU.subtract),
                             reads=[f"lastc{d}", "A5", f"kd{d}"], writes=["A1"])
                        S.op("scalar", k_act(A1[:], A1[:], AF.Exp), reads=["A1"], writes=["A1"])
                        S.op("vector", k_tt(kd2[d][:], A3[:], A1[:], ALU.mult), reads=["A3", "A1"], writes=[f"kd2{d}"])

                    def state_step(d, vblk, c, chunk_idx):
                        S.op("tensor", k_mm(pS[d][:, 0:128], k2T[d][c * 64:(c + 1) * 64, :], vh[c * 64:(c + 1) * 64, vblk, :], True, True),
                             reads=[f"k2T{d}", "vh"], writes=[f"pS{d}"])
                        S.op("vector", k_stt(Sbf[d][:], S32[d][:], elast[d][:, chunk_idx:chunk_idx + 1], pS[d][:, 0:128], ALU.mult, ALU.add),
                             reads=[f"S32{d}", f"elast{d}", f"pS{d}"], writes=[f"Sbf{d}"])
                        S.op("vector", k_stt(S32[d][:], S32[d][:], elast[d][:, chunk_idx:chunk_idx + 1], pS[d][:, 0:128], ALU.mult, ALU.add),
                             reads=[f"S32{d}", f"elast{d}", f"pS{d}"], writes=[f"S32{d}"])

                    def k2_transpose(d, col0):
                        S.op("tensor", k_tr(pAt[d][:, 0:128], kd2[d][:, col0:col0 + 128], identb[:]),
                             reads=[f"kd2{d}", "identb"], writes=[f"pAt{d}"])
                        S.op("scalar", k_act(k2T[d][:], pAt[d][:, 0:128], AF.Copy), reads=[f"pAt{d}"], writes=[f"k2T{d}"])

                    for d in range(2):
                        S.op("gpsimd", k_memset(S32[d][:], 0.0), writes=[f"S32{d}"])
                        S.op("gpsimd", k_memset(Sbf[d][:], 0.0), writes=[f"Sbf{d}"])
                    S.op("gpsimd", k_memset(oloc[:], 0.0), writes=["oloc"])
                    for step in range(NCB):
                        for d in range(2):
                            cb = step if d == 0 else NCB - 1 - step
                            k2_transpose(d, NT + cb * 128)
                            for c in ([0, 1] if d == 0 else [1, 0]):
                                state_step(d, NB + cb, c, NCH + cb * 2 + c)
                    for d in range(2):
                        S.op("vector", k_copy(cstate[d][:, h, :], S32[d][:]), reads=[f"S32{d}"], writes=[f"cstate{d}"])
                        S.op("gpsimd", k_memset(S32[d][:], 0.0), reads=[f"cstate{d}"], writes=[f"S32{d}"])
                        S.op("gpsimd", k_memset(Sbf[d][:], 0.0), writes=[f"Sbf{d}"])
                    for step in range(NB):
                        for d in range(2):
                            jb = step if d == 0 else NB - 1 - step
                            corder = [0, 1] if d == 0 else [1, 0]
                            mask = C("maskf") if d == 0 else C("maskb")
                            col0 = jb * 128
                            S.op("tensor", k_mm(pA[d][:, 0:128], kd[d][:, col0:col0 + 128], qd[d][:, col0:col0 + 128], True, True),
                                 reads=[f"kd{d}", f"qd{d}"], writes=[f"pA{d}"])
                            S.op("vector", k_tt(ATm[d][:], pA[d][:, 0:128], mask, ALU.mult), reads=[f"pA{d}", "cst"], writes=[f"ATm{d}"])
                            k2_transpose(d, col0)
                            S.op("tensor", k_mm(pO[d][:, 0:128], ATm[d][:], vh[:, jb, :], True, False),
                                 reads=[f"ATm{d}", "vh"], writes=[f"pO{d}"])
                            for ci, c in enumerate(corder):
                                S.op("tensor", k_mm(pO[d][c * 64:(c + 1) * 64, 0:128], qd[d][:, col0 + c * 64:col0 + (c + 1) * 64],
                                                    Sbf[d][:], False, ci == 1),
                                     reads=[f"qd{d}", f"Sbf{d}"], writes=[f"pO{d}"])
                                state_step(d, jb, c, jb * 2 + c)
                            S.op("vector", k_tt(oloc[:, jb, :], oloc[:, jb, :], pO[d][:, 0:128], ALU.add),
                                 reads=[f"pO{d}", "oloc"], writes=["oloc"])
                    for d in range(2):
                        S.op("vector", k_copy(stt_t[:, h * 258 + d * 128:h * 258 + (d + 1) * 128], S32[d][:]),
                             reads=[f"S32{d}"], writes=["stt_t"])
                    S.dma("sync", k_dma(o_scr[h], oloc[:].rearrange("p b v -> p (b v)")), reads=["oloc"], writes=[f"o_scr{h}"])
                S.dma("sync", k_dma(st_loc, stt_t[:]), reads=["stt_t"], writes=["st_loc"])
                S.coll(k_ag(st_loc, st_all), reads=["st_loc"], writes=["st_all"])
                S.barrier()
                S.emit()
                if cfg.stop == 2:
                    finish()
                    return nc

            with ExitStack() as phB:
                BC = sb(phB, "BC", [128, 1, D], F32)
                ptr = ps(phB, "ptr", [128, 1024], BF16)
                pOq = [ps(phB, f"pOb{k}", [128, 512], F32) for k in range(2)]
                pGq = [ps(phB, f"pGb{k}", [128, 512], F32) for k in range(2)]
                go = sb(phB, "go", [128, NB, D], BF16)
                stA = [sb(phB, f"stA{i}", [128, 8, 258], F32) for i in range(2)]
                Sin = [sb(phB, f"Sin{d}", [128, 128], F32) for d in range(2)]
                Sinb = [sb(phB, f"Sinb{d}", [128, 128], BF16) for d in range(2)]
                acol = sb(phB, "acol", [128, 1], F32)
                qsgL = [[sb(phB, f"qsgL{i}{d}", [128, NT], BF16) for d in range(2)] for i in range(2)]
                olocL = [sb(phB, f"olocL{i}", [128, NB * 128], F32) for i in range(2)]
                wgh = [sb(phB, f"wgh{i}", [128, 8, 128], BF16) for i in range(2)]
                otq = [sb(phB, f"ot{k}", [128, 128], F32) for k in range(2)]
                ojq = [sb(phB, f"oj{k}", [128, 128], F32) for k in range(2)]
                sgq = [sb(phB, f"sg{k}", [128, 128], F32) for k in range(2)]
                ssq = [sb(phB, f"ssb{k}", [128, 1], F32) for k in range(2)]
                rsq = [sb(phB, f"rsb{k}", [128, 2], F32) for k in range(2)]
                wo = sb(phB, "wo", [128, 8, D], BF16)
                xt = [sb(phB, f"xtb{i}", [128, D], F32) for i in range(2)]
                tmpy = sb(phB, "tmpy", [128, 512], F32)
                bcast_rows(BC, pb, ada(0, 2), 0, "ADAo")
                S.dma("gpsimd", k_dma(wo[:], hgwout_in.rearrange("(kc p) n -> p kc n", p=128)), writes=["wo"])
                for h in range(NH):
                    i = h % 2
                    S.dma("sync", k_dma(stA[i][:], st_all[:, h * 258:(h + 1) * 258].rearrange("(r p) n -> p r n", p=128)),
                          reads=["st_all"], writes=[f"stA{i}"])
                    S.dma("gpsimd", k_dma(wgh[i][:], hgwin_in[:, 4 * D + h * 128:4 * D + (h + 1) * 128]
                                          .rearrange("(kc p) n -> p kc n", p=128)), writes=[f"wgh{i}"])
                    for d in range(2):
                        S.dma("sync", k_dma(qsgL[i][d][:], qseg_scr[h, d]), reads=["qseg_scr"], writes=[f"qsgL{i}{d}"])
                    S.dma("sync", k_dma(olocL[i][:], o_scr[h]), reads=["o_scr"], writes=[f"olocL{i}"])
                    for d in range(2):
                        mname = "mf" if d == 0 else "mb"
                        S.op("vector", k_copy(Sin[d][:], cstate[d][:, h, :]), reads=[f"cstate{d}"], writes=[f"Sin{d}"])
                        for r in (range(8) if d == 0 else range(7, -1, -1)):
                            mcol = C(mname, r, 1)
                            S.op("vector", k_ts(acol[:], stA[i][:, r, 256 + d:257 + d], -1.0, mcol, ALU.add, ALU.mult),
                                 reads=[f"stA{i}", "cst"], writes=["acol"])
                            S.op("vector", k_ts(acol[:], acol[:], 1.0, None, ALU.add), reads=["acol"], writes=["acol"])
                            S.op("vector", k_ts(Sin[d][:], Sin[d][:], acol[:, 0:1], None, ALU.mult), reads=["acol", f"Sin{d}"], writes=[f"Sin{d}"])
                            S.op("vector", k_stt(Sin[d][:], stA[i][:, r, d * 128:(d + 1) * 128], mcol, Sin[d][:], ALU.mult, ALU.add),
                                 reads=[f"stA{i}", "cst", f"Sin{d}"], writes=[f"Sin{d}"])
                        S.op("scalar", k_act(Sinb[d][:], Sin[d][:], AF.Copy), reads=[f"Sin{d}"], writes=[f"Sinb{d}"])
                    for jb in range(NB):
                        blk = slice(jb * 128, (jb + 1) * 128)
                        k = jb % 2
                        pO_, pG_, ot_, oj_, sg_, ss_, rs_ = pOq[k], pGq[k], otq[k], ojq[k], sgq[k], ssq[k], rsq[k]
                        S.op("tensor", k_mm(pO_[:, 0:128], qsgL[i][0][:, blk], Sinb[0][:], True, False),
                             reads=[f"qsgL{i}0", "Sinb0"], writes=[f"pOb{k}"])
                        S.op("tensor", k_mm(pO_[:, 0:128], qsgL[i][1][:, blk], Sinb[1][:], False, True),
                             reads=[f"qsgL{i}1", "Sinb1"], writes=[f"pOb{k}"])
                        S.op("vector", k_tt(ot_[:], olocL[i][:, blk], pO_[:, 0:128], ALU.add), reads=[f"olocL{i}", f"pOb{k}"], writes=[f"ot{k}"])
                        if cfg.debug:
                            S.dma("sync", k_dma(dbg["d_o"][blk, h * 128:(h + 1) * 128], ot_[:]), reads=[f"ot{k}"], writes=["d_o"])
                        S.op("scalar", k_act(oj_[:], ot_[:], AF.Square, accum=ss_[:, 0:1]), reads=[f"ot{k}"], writes=[f"oj{k}", f"ssb{k}"])
                        S.op("scalar", k_act(rs_[:, 0:1], ss_[:, 0:1], AF.Sqrt, scale=1.0 / 128, bias=epsb[:, 0:1]),
                             reads=[f"ssb{k}", "epsb"], writes=[f"rsb{k}"])
                        S.op("vector", k_recip(rs_[:, 1:2], rs_[:, 0:1]), reads=[f"rsb{k}"], writes=[f"rsb2{k}"])
                        S.op("vector", k_stt(oj_[:], ot_[:], rs_[:, 1:2], C("hgn"), ALU.mult, ALU.mult), reads=[f"ot{k}", f"rsb2{k}", "cst"], writes=[f"oj{k}"])
                        for kc in range(8):
                            S.op("tensor", k_mm(pG_[:, 0:128], hT[:, kc, blk], wgh[i][:, kc, :], kc == 0, kc == 7),
                                 reads=["hT", f"wgh{i}"], writes=[f"pGb{k}"])
                        S.op("scalar", k_act(sg_[:], pG_[:, 0:128], AF.Silu), reads=[f"pGb{k}"], writes=[f"sg{k}"])
                        S.op("gpsimd", k_tt(go[:, jb, h * 128:(h + 1) * 128], oj_[:], sg_[:], ALU.mult), reads=[f"oj{k}", f"sg{k}"], writes=["go"])
                for jb in range(NB):
                    transpose_to(go[:, jb, :], "go", hT, "hT", jb * 128, ptr, "ptr")
                for jb in range(NB):
                    i = jb % 2
                    blk = slice(jb * 128, (jb + 1) * 128)
                    S.dma("sync", k_dma(xt[i][:], xres[blk, :]), reads=[f"xres{jb}"], writes=[f"xtb{i}"])
                    for hf in range(2):
                        for kc in range(8):
                            S.op("tensor", k_mm(pb[hf][:], hT[:, kc, blk], wo[:, kc, hf * 512:(hf + 1) * 512], kc == 0, kc == 7),
                                 reads=["hT", "wo"], writes=[f"pb{hf}"])
                        S.op("vector", k_tt(tmpy[:], pb[hf][:], BC[:, 0, hf * 512:(hf + 1) * 512], ALU.mult),
                             reads=[f"pb{hf}", "BC0"], writes=["tmpy"])
                        S.op("vector", k_tt(xt[i][:, hf * 512:(hf + 1) * 512], xt[i][:, hf * 512:(hf + 1) * 512], tmpy[:], ALU.add),
                             reads=["tmpy", f"xtb{i}"], writes=[f"xtb{i}"])
                    S.dma("sync", k_dma(xres[blk, :], xt[i][:]), reads=[f"xtb{i}"], writes=[f"xres{jb}"])
                    if cfg.debug:
                        S.dma("sync", k_dma(dbg["d_xmix0"][blk, :], xt[i][:]), reads=[f"xtb{i}"], writes=["d_xmix0"])
                S.barrier()
                S.emit()
                if cfg.stop == 3:
                    finish()
                    return nc
        def moe_layer(l, stop_base, dbg_name):
            with ExitStack() as ph:
                BC = sb(ph, "BCm", [128, 2, D], F32)
                pbq = [[ps(ph, f"pbm{i}{k}", [128, 512], F32) for k in range(2)] for i in range(2)]
                pb = pbq[0]
                pLq = [ps(ph, f"pL{i}", [128, 512], F32) for i in range(2)]
                xt = [sb(ph, f"xtm{i}", [128, D], F32) for i in range(2)]
                h2fq = [sb(ph, f"h2f{i}", [128, D], F32) for i in range(2)]
                rowt = [sb(ph, f"rowt{i}", [128, ROWW], BF16) for i in range(2)]
                h2Tq = [sb(ph, f"h2T{i}", [128, 8, 128], F32) for i in range(2)]
                wr = sb(ph, "wr", [128, 8, NE], F32)
                sqjq = [sb(ph, f"sqjm{i}", [128, D], F32) for i in range(2)]
                ssq = [sb(ph, f"ssm{i}", [128, 1], F32) for i in range(2)]
                rsq = [sb(ph, f"rsm{i}", [128, 2], F32) for i in range(2)]
                mxq = [sb(ph, f"mx{i}", [128, 2], F32) for i in range(2)]
                smq = [sb(ph, f"sm{i}", [128, 2], F32) for i in range(2)]
                exq = [sb(ph, f"ex{i}", [128, NE], F32) for i in range(2)]
                affq = [sb(ph, f"aff{i}", [128, NE], F32) for i in range(2)]
                affT = sb(ph, "affT", [NE, NT], F32)
                G2 = make_gain(ph, f"G2c{l}", ada(l, 4), C("nffn", l * 8, 8), ["ADAo", "cst"])
                bcast_rows(BC, pb, G2, 0, f"G2c{l}", pk=("pbm00", "pbm01"))
                bcast_rows(BC, pb, ada(l, 3), 1, "ADAo", pk=("pbm00", "pbm01"))
                S.dma("sync", k_dma(wr[:], router_in[l].rearrange("(kc p) e -> p kc e", p=128)), writes=["wr"])
                for j in range(NB):
                    i = j % 2
                    blk = slice(j * 128, (j + 1) * 128)
                    h2f, h2T, pL, pbj = h2fq[i], h2Tq[i], pLq[i], pbq[i]
                    mx, sm, ex, aff = mxq[i], smq[i], exq[i], affq[i]
                    S.dma("sync", k_dma(xt[i][:], xres[blk, :]), reads=[f"xres{j}"], writes=[f"xtm{i}"])
                    prenorm(BC, xt[i][:], f"xtm{i}", 0, 1, h2f[:], f"h2f{i}", ssq[i], rsq[i], sqjq[i], sfx=f"m{i}")
                    S.op("scalar", k_act(rowt[i][:, 0:D], h2f[:], AF.Copy), reads=[f"h2f{i}"], writes=[f"rowt{i}"])
                    for kc in range(8):
                        S.op("tensor", k_tr(pbj[kc // 4][:, (kc % 4) * 128:(kc % 4 + 1) * 128], h2f[:, kc * 128:(kc + 1) * 128], ident),
                             reads=[f"h2f{i}", "cst"], writes=[f"pbm{i}{kc // 4}"])
                    for hf in range(2):
                        eng_ = "scalar" if hf == 0 else "vector"
                        fn_ = (k_act(h2T[:, hf * 4:(hf + 1) * 4, :], pbj[hf][:].rearrange("p (k n) -> p k n", k=4), AF.Copy) if hf == 0
                               else k_copy(h2T[:, hf * 4:(hf + 1) * 4, :], pbj[hf][:].rearrange("p (k n) -> p k n", k=4)))
                        S.op(eng_, fn_, reads=[f"pbm{i}{hf}"], writes=[f"h2T{i}"])
                    for kc in range(8):
                        S.op("tensor", k_mm(pL[:, 0:NE], h2T[:, kc, :], wr[:, kc, :], kc == 0, kc == 7),
                             reads=[f"h2T{i}", "wr"], writes=[f"pL{i}"])
                    S.op("vector", lambda e, o=mx[:, 0:1], a=pL[:, 0:NE]: e.reduce_max(out=o, in_=a, axis=AX.X), reads=[f"pL{i}"], writes=[f"mx{i}"])
                    S.op("vector", k_ts(mx[:, 1:2], mx[:, 0:1], -1.0, None, ALU.mult), reads=[f"mx{i}"], writes=[f"mx2{i}"])
                    S.op("scalar", k_act(ex[:], pL[:, 0:NE], AF.Exp, bias=mx[:, 1:2], accum=sm[:, 0:1]), reads=[f"pL{i}", f"mx2{i}"], writes=[f"ex{i}", f"sm{i}"])
                    S.op("vector", k_recip(sm[:, 1:2], sm[:, 0:1]), reads=[f"sm{i}"], writes=[f"sm2{i}"])
                    S.op("vector", k_ts(aff[:], ex[:], sm[:, 1:2], None, ALU.mult), reads=[f"ex{i}", f"sm2{i}"], writes=[f"aff{i}"])
                    S.op("vector", k_copy(rowt[i][:, D:ROWW].bitcast(F32), aff[:]), reads=[f"aff{i}"], writes=[f"rowt{i}"])
                    S.dma("sync", k_dma(h2_loc[l][blk, :], rowt[i][:]), reads=[f"rowt{i}"], writes=[f"h2_loc{j}"])
                    S.op("tensor", k_tr(pL[0:NE, 256:384], aff[:], ident), reads=[f"aff{i}", "cst"], writes=[f"pLt{i}"])
                    S.op("scalar", k_act(affT[:, blk], pL[0:NE, 256:384], AF.Copy), reads=[f"pLt{i}"], writes=[f"affT{j}"])
                S.dma("sync", k_dma(aff_loc[l], affT[:]), reads=[f"affT{j_}" for j_ in range(NB)], writes=["aff_loc"])
                if cfg.debug and l == 0:
                    S.dma("sync", k_dma(dbg["d_aff"], affT[:]), reads=[f"affT{j_}" for j_ in range(NB)], writes=["d_aff"])
                S.coll(k_ag(aff_loc[l], aff_all[l]), reads=["aff_loc"], writes=["aff_all"])
                S.barrier()
                S.emit()
                if cfg.stop == stop_base:
                    finish()
                    return True

            with ExitStack() as phm:
                destSel = sb(phm, "destSel", [128, NB, 36], I32)
                selm = sb(phm, "selm", [128, NB, 16], F32)
                with ExitStack() as ph:
                    A = sb(ph, "A", [128, NT], F32)
                    junk = sb(ph, "junk", [128, NT], BF16)
                    mk = sb(ph, "mk", [128, NT], F32)
                    inc = sb(ph, "inc", [128, NT], F32)
                    bs_ = sb(ph, "bs_", [128, 8], F32)
                    cnt = sb(ph, "cnt", [128, 2], F32)
                    dsf = sb(ph, "dsf", [128, 32], F32)
                    pt_ = ps(ph, "pt_", [128, 512], F32)
                    pD = ps(ph, "pD", [128, 512], F32)
                    lo, hi, mid, ge, d1, offm = (bs_[:, k:k + 1] for k in range(6))
                    S.dma("sync", k_dma(A[:], aff_all[l]), reads=["aff_all"], writes=["A"])
                    S.coll(k_ag(h2_loc[l], h2_all[l]), reads=[], writes=["h2_all"])
                    S.op("vector", k_memset(cnt[:], 0.0), writes=["cnt"])
                    S.op("vector", k_memset(bs_[:, 0:1], 0.0), writes=["bs"])
                    S.op("vector", k_memset(bs_[:, 1:2], 1.0), writes=["bs"])
                    S.op("vector", k_memset(bs_[:, 2:3], 0.5), writes=["bs"])
                    for it in range(32):
                        S.op("vector", k_ts(junk[:], A[:], mid, None, ALU.is_ge, ALU.add, accum=cnt[:, 0:1]), reads=["A", "bs"], writes=["junk", "cnt"])
                        S.op("tensor", k_mm(pt_[:, 0:2], C("Bm"), cnt[:, 0:2], True, True), reads=["cnt", "cst"], writes=["pt_"])
                        S.op("vector", k_ts(ge, pt_[:, 0:1], float(CAP), None, ALU.is_ge), reads=["pt_"], writes=["bs"])
                        S.op("vector", k_tt(d1, mid, lo, ALU.subtract), reads=["bs"], writes=["bs"])
                        S.op("vector", k_stt(lo, d1, ge, lo, ALU.mult, ALU.add), reads=["bs"], writes=["bs"])
                        S.op("vector", k_tt(d1, hi, mid, ALU.subtract), reads=["bs"], writes=["bs"])
                        S.op("vector", k_stt(hi, d1, ge, mid, ALU.mult, ALU.add), reads=["bs"], writes=["bs"])
                        S.op("vector", k_tt(mid, lo, hi, ALU.add), reads=["bs"], writes=["bs"])
                        S.op("vector", k_ts(mid, mid, 0.5, None, ALU.mult), reads=["bs"], writes=["bs"])
                    S.op("vector", k_ts(mk[:], A[:], lo, None, ALU.is_ge), reads=["A", "bs"], writes=["mk"])
                    S.op("vector", k_scan(inc[:], C("ones", 0, 1).to_broadcast([128, NT]), mk[:], 0.0, ALU.mult, ALU.add),
                         reads=["mk", "cst"], writes=["inc"])
                    S.op("vector", k_copy(cnt[:, 0:1], inc[:, NT - 1:NT]), reads=["inc"], writes=["cnt"])
                    S.op("tensor", k_mm(pt_[:, 0:2], C("Tm"), cnt[:, 0:2], True, True), reads=["cnt", "cst"], writes=["pt_"])
                    S.op("vector", k_ts(offm, pt_[:, 0:1], -(1.0 + BIG), None, ALU.add), reads=["pt_"], writes=["bs"])
                    S.op("vector", k_ts(inc[:], inc[:], offm, None, ALU.add), reads=["inc", "bs"], writes=["inc"])
                    S.op("vector", k_tt(inc[:], inc[:], mk[:], ALU.mult), reads=["inc", "mk"], writes=["inc"])
                    S.op("vector", k_ts(inc[:], inc[:], BIG, None, ALU.add), reads=["inc"], writes=["inc"])
                    for j in range(NB):
                        S.op("tensor", k_mm(pD[:, 0:32], inc[:, j * 128:(j + 1) * 128], C("sel"), True, True),
                             reads=["inc", "cst"], writes=["pD"])
                        S.op("vector", k_ts(selm[:, j, :], pD[:, 16:32], BIG - 0.5, None, ALU.is_lt), reads=["pD"], writes=["selm"])
                        S.op("vector", k_tt(dsf[:], pD[:, 0:32], C("retbase"), ALU.add), reads=["pD", "cst"], writes=["dsf"])
                        S.op("vector", k_copy(destSel[:, j, 0:32], dsf[:]), reads=["dsf"], writes=["destSel"])
                        if cfg.debug and l == 0:
                            S.dma("sync", k_dma(dbg["d_dest"][:, j * 32:(j + 1) * 32], dsf[:]), reads=["dsf"], writes=["d_dest"])
                    S.barrier()
                    S.emit()
                    if cfg.stop == stop_base + 1:
                        finish()
                        return True

                with ExitStack() as ph:
                    ptr = ps(ph, "ptre", [128, 1024], BF16)
                    pa = [ps(ph, f"pa{i}", [128, 512], F32) for i in range(2)]
                    pu = [ps(ph, f"pu{i}", [128, 512], F32) for i in range(2)]
                    py = [ps(ph, f"py{i}", [128, 512], F32) for i in range(2)]
                    rw = [sb(ph, f"rw{i}", [128, ROWW], BF16) for i in range(6)]
                    xr = [sb(ph, f"xr{i}", [128, ROWW], BF16) for i in range(2)]
                    xgT = [sb(ph, f"xgT{bb}", [128, 8, CAP], BF16) for bb in range(2)]
                    actT = [sb(ph, f"actT{bb}", [128, NFC, CAP], BF16) for bb in range(2)]
                    wdt = sb(ph, "wdt", [128, NFC, D], BF16)
                    wgf = [sb(ph, f"wgf{i}", [128, 8, 128], BF16) for i in range(3)]
                    wuf = [sb(ph, f"wuf{i}", [128, 8, 128], BF16) for i in range(3)]
                    gates = sb(ph, "gates", [128, 2, NSB], F32)
                    t16 = sb(ph, "t16", [128, NE], F32)
                    sa = [sb(ph, f"sa{i}", [128, 512], F32) for i in range(2)]
                    yrow = [sb(ph, f"yrow{i}", [128, D], BF16) for i in range(2)]
                    n_rw = 0
                    for bb in range(2):
                        for g in range(4):
                            for j in range(NB):
                                i = n_rw % 6
                                n_rw += 1
                                r0 = (bb * 4 + g) * NT + j * 128
                                S.dma("sync", k_dma(rw[i][:], h2_all[l][r0:r0 + 128, :]), reads=["h2_all"], writes=[f"rw{i}"])
                                for el in range(2):
                                    col = bb * 8 + g * 2 + el
                                    S.dma("gpsimd", k_scatter(xg[l][bb * 2 + el], destSel[:, j, col:col + 1], rw[i][:], CAPP - 1),
                                          reads=[f"rw{i}", "destSel"], writes=[f"xg{bb * 2 + el}_{g}_{j}"])
                    NST = max(1, CAP // 512)
                    NN = min(512, CAP)
                    for el in range(2):
                        S.dma("gpsimd", k_dma(wdt[:], wd_in[l, el].rearrange("(fc p) d -> p fc d", p=128)), writes=["wdt"])
                        for bb in range(2):
                            for sbk in range(NSB):
                                i = sbk % 2
                                S.dma("sync", k_dma(xr[i][:], xg[l][bb * 2 + el][sbk * 128:(sbk + 1) * 128, :]),
                                      reads=[f"xg{bb * 2 + el}_{g_}_{j_}" for g_ in range(4) for j_ in range(NB)], writes=[f"xr{i}"])
                                S.op("vector", k_tt(t16[:], xr[i][:, D:ROWW].bitcast(F32), C("gsel", el * 16, 16), ALU.mult),
                                     reads=[f"xr{i}", "cst"], writes=["t16"])
                                S.op("vector", lambda e, o=gates[:, bb, sbk:sbk + 1], a=t16[:]: e.reduce_sum(out=o, in_=a, axis=AX.X),
                                     reads=["t16"], writes=["gates"])
                                transpose_to(xr[i], f"xr{i}", xgT[bb], f"xgT{bb}", sbk * 128, ptr, "ptre", eng="vector")
                        for fc in range(NFC):
                            wi = fc % 3
                            S.dma("gpsimd", k_dma(wgf[wi][:], wg_in[l, el][:, fc * 128:(fc + 1) * 128].rearrange("(kc p) f -> p kc f", p=128)),
                                  writes=[f"wgf{wi}"])
                            S.dma("gpsimd", k_dma(wuf[wi][:], wu_in[l, el][:, fc * 128:(fc + 1) * 128].rearrange("(kc p) f -> p kc f", p=128)),
                                  writes=[f"wuf{wi}"])
                            if el == 1 and fc == min(3, NFC - 1):
                                S.coll(k_ag(y_loc[l][0], y_all[l][0]),
                                       reads=[f"y_loc{b_}_0_{s_}" for b_ in range(2) for s_ in range(NSB)], writes=["y_all0"])
                            for bb in range(2):
                                for st in range(NST):
                                    pi = (bb * NST + st) % 2
                                    cols = slice(st * NN, (st + 1) * NN)
                                    for kc in range(8):
                                        S.op("tensor", k_mm(pa[pi][:, 0:NN], wgf[wi][:, kc, :], xgT[bb][:, kc, cols], kc == 0, kc == 7),
                                             reads=[f"wgf{wi}", f"xgT{bb}"], writes=[f"pa{pi}"])
                                    for kc in range(8):
                                        S.op("tensor", k_mm(pu[pi][:, 0:NN], wuf[wi][:, kc, :], xgT[bb][:, kc, cols], kc == 0, kc == 7),
                                             reads=[f"wuf{wi}", f"xgT{bb}"], writes=[f"pu{pi}"])
                                    S.op("scalar", k_act(sa[pi][:, 0:NN], pa[pi][:, 0:NN], AF.Silu), reads=[f"pa{pi}"], writes=[f"sa{pi}"])
                                    S.op("vector", k_tt(actT[bb][:, fc, cols], sa[pi][:, 0:NN], pu[pi][:, 0:NN], ALU.mult),
                                         reads=[f"sa{pi}", f"pu{pi}"], writes=[f"actT{bb}"])
                        for bb in range(2):
                            for sbk in range(NSB):
                                yi = sbk % 2
                                for hf in range(2):
                                    for fc in range(NFC):
                                        S.op("tensor", k_mm(py[hf][:], actT[bb][:, fc, sbk * 128:(sbk + 1) * 128], wdt[:, fc, hf * 512:(hf + 1) * 512],
                                                            fc == 0, fc == NFC - 1), reads=[f"actT{bb}", "wdt"], writes=[f"py{hf}"])
                                    S.op("scalar", k_act(yrow[yi][:, hf * 512:(hf + 1) * 512], py[hf][:], AF.Copy, scale=gates[:, bb, sbk:sbk + 1]),
                                         reads=[f"py{hf}", "gates"], writes=[f"yrow{yi}"])
                                r0 = bb * CAPP + sbk * 128
                                S.dma("sync", k_dma(y_loc[l][el][r0:r0 + 128, :], yrow[yi][:]), reads=[f"yrow{yi}"], writes=[f"y_loc{bb}_{el}_{sbk}"])
                    S.barrier()
                    S.emit()
                    if cfg.stop == stop_base + 2:
                        finish()
                        return True

                with ExitStack() as ph:
                    BC = sb(ph, "BCr", [128, 1, D], F32)
                    pb = [ps(ph, f"pbr{i}", [128, 512], F32) for i in range(2)]
                    Yt = [sb(ph, f"Yt{e}", [128, D], BF16) for e in range(NE)]
                    dg = [sb(ph, f"dg{i}", [128, 128], BF16) for i in range(4)]
                    xt = [sb(ph, f"xtr{i}", [128, D], F32) for i in range(2)]
                    tmpy = sb(ph, "tmpyr", [128, 512], F32)
                    S.coll(k_ag(y_loc[l][1], y_all[l][1]), reads=[], writes=["y_all1"])
                    bcast_rows(BC, pb, ada(l, 5), 0, "ADAo", pk=("pbr0", "pbr1"))
                    for e in range(NE):
                        S.op("gpsimd", k_memset(Yt[e][:], 0.0), writes=[f"Yt{e}"])
                    nd = 0
                    for j in range(NB):
                        i = j % 2
                        blk = slice(j * 128, (j + 1) * 128)
                        S.dma("sync", k_dma(xt[i][:], xres[blk, :]), reads=[f"xres{j}"], writes=[f"xtr{i}"])
                        for e in range(NE):
                            S.dma("gpsimd", k_gather(Yt[e][:], y_all[l][e % 2], destSel[:, j, 16 + e:17 + e], 8 * 2 * CAPP - 1),
                                  reads=["destSel", f"y_all{e % 2}"], writes=[f"Yt{e}"])
                            di = nd % 4
                            nd += 1
                            S.op("vector", k_ts(dg[di][:], identb[:], selm[:, j, e:e + 1], None, ALU.mult),
                                 reads=["identb", "selm"], writes=[f"dg{di}"])
                            for hf in range(2):
                                S.op("tensor", k_mm(pb[hf][:], dg[di][:], Yt[e][:, hf * 512:(hf + 1) * 512], e == 0, e == NE - 1),
                                     reads=[f"dg{di}", f"Yt{e}"], writes=[f"pbr{hf}"])
                        for hf in range(2):
                            S.op("vector", k_tt(tmpy[:], pb[hf][:], BC[:, 0, hf * 512:(hf + 1) * 512], ALU.mult),
                                 reads=[f"pbr{hf}", "BC0"], writes=["tmpyr"])
                            S.op("vector", k_tt(xt[i][:, hf * 512:(hf + 1) * 512], xt[i][:, hf * 512:(hf + 1) * 512], tmpy[:], ALU.add),
                                 reads=["tmpyr", f"xtr{i}"], writes=[f"xtr{i}"])
                        S.dma("sync", k_dma(xres[blk, :], xt[i][:]), reads=[f"xtr{i}"], writes=[f"xres{j}"])
                        if cfg.debug:
                            S.dma("sync", k_dma(dbg[dbg_name][blk, :], xt[i][:]), reads=[f"xtr{i}"], writes=[dbg_name])
                    S.barrier()
                    S.emit()
                    if cfg.stop == stop_base + 3:
                        finish()
                        return True
            return False

        if moe_layer(0, 4, "d_xmoe0"):
            return nc
        with ExitStack() as ph:
            hT = sb(ph, "hTc", [128, 8, NT], BF16)
            zT = sb(ph, "zT", [128, 8, NT], BF16)
            BC = sb(ph, "BCc", [128, 3, D], F32)
            pb = [ps(ph, f"pbc{i}", [128, 512], F32) for i in range(2)]
            ptr = ps(ph, "ptrc", [128, 1024], BF16)
            pc_ = [ps(ph, f"pcc{i}", [128, 512], F32) for i in range(2)]
            pu_ = [ps(ph, f"puc{i}", [128, 512], F32) for i in range(2)]
            xt = [sb(ph, f"xtc{i}", [128, D], F32) for i in range(2)]
            hb = [sb(ph, f"hbc{i}", [128, D], BF16) for i in range(2)]
            sqj = [sb(ph, f"sqjc{i}", [128, D], F32) for i in range(2)]
            ss = [sb(ph, f"ssc{i}", [128, 1], F32) for i in range(2)]
            rs = [sb(ph, f"rsc{i}", [128, 2], F32) for i in range(2)]
            w3 = [[sb(ph, f"w3{n}{i}", [128, 8, 128], BF16) for n in range(3)] for i in range(2)]
            wo = sb(ph, "woc", [128, 8, D], BF16)
            cu2 = sb(ph, "cu2", [128, 16], F32)
            c2s = sb(ph, "c2s", [128, 2], F32)
            hl = sb(ph, "hl", [128, 8, 16], F32)
            t88 = sb(ph, "t88", [128, 8, 8], F32)
            halo = sb(ph, "halo", [128, 2, 8], F32)
            cup = sb(ph, "cup", [128, NT + 2], F32)
            ycv = sb(ph, "ycv", [128, NT], F32)
            cs_ = sb(ph, "cs_", [128, 512], F32)
            tmpy = sb(ph, "tmpyc", [128, 512], F32)
            G1 = make_gain(ph, "G1c1", ada(1, 1), C("nmix", 8, 8), ["ADAo", "cst"])
            bcast_rows(BC, pb, G1, 0, "G1c1", pk=("pbc0", "pbc1"))
            bcast_rows(BC, pb, ada(1, 0), 1, "ADAo", pk=("pbc0", "pbc1"))
            bcast_rows(BC, pb, ada(1, 2), 2, "ADAo", pk=("pbc0", "pbc1"))
            S.dma("gpsimd", k_dma(wo[:], scwout_in.rearrange("(kc p) n -> p kc n", p=128)), writes=["woc"])
            for j in range(NB):
                i = j % 2
                blk = slice(j * 128, (j + 1) * 128)
                S.dma("sync", k_dma(xt[i][:], xres[blk, :]), reads=[f"xres{j}"], writes=[f"xtc{i}"])
                prenorm(BC, xt[i][:], f"xtc{i}", 0, 1, hb[i][:], f"hbc{i}", ss[i], rs[i], sqj[i], sfx=f"c{i}")
                transpose_to(hb[i], f"hbc{i}", hT, "hTc", j * 128, ptr, "ptrc")

            def load_w3(dc, i):
                for n in range(3):
                    S.dma("gpsimd", k_dma(w3[i][n][:], scwin_in[:, n * D + dc * 128:n * D + (dc + 1) * 128]
                                          .rearrange("(kc p) n -> p kc n", p=128)), writes=[f"w3{n}{i}"])

            for dc in range(8):
                i = dc % 2
                load_w3(dc, i)
                for kc in range(8):
                    S.op("tensor", k_mm(pc_[0][:, 0:2], w3[i][1][:, kc, :], hT[:, kc, 0:NT:NT - 1], kc == 0, kc == 7),
                         reads=[f"w31{i}", "hTc"], writes=["pcc0"])
                for kc in range(8):
                    S.op("tensor", k_mm(pu_[0][:, 0:2], w3[i][2][:, kc, :], hT[:, kc, 0:NT:NT - 1], kc == 0, kc == 7),
                         reads=[f"w32{i}", "hTc"], writes=["puc0"])
                S.op("scalar", k_act(c2s[:], pc_[0][:, 0:2], AF.Copy), reads=["pcc0"], writes=["c2s"])
                S.op("vector", k_tt(cu2[:, dc * 2:dc * 2 + 2], c2s[:], pu_[0][:, 0:2], ALU.mult), reads=["c2s", "puc0"], writes=["cu2"])
            S.dma("sync", k_dma(halo_loc, cu2[:]), reads=["cu2"], writes=["halo_loc"])
            S.coll(k_ag(halo_loc, halo_all), reads=["halo_loc"], writes=["halo_all"])
            S.dma("sync", k_dma(hl[:], halo_all.rearrange("(r p) n -> p r n", p=128)), reads=["halo_all"], writes=["hl"])
            for side, (sname, off) in enumerate((("selL", 1), ("selR", 0))):
                S.op("vector", k_tt(t88[:], hl[:, :, off:16:2].rearrange("p r c -> p c r"),
                                    C(sname).unsqueeze(1).to_broadcast([128, 8, 8]), ALU.mult), reads=["hl", "cst"], writes=["t88"])
                S.op("vector", lambda e, o=halo[:, side, :], a=t88[:]: e.reduce_sum(out=o, in_=a, axis=AX.X),
                     reads=["t88"], writes=["halo"])
            NN = min(512, NT)
            NST = NT // NN
            for dc in range(8):
                i = dc % 2
                load_w3(dc, i)
                for st in range(NST):
                    cols = slice(st * NN, (st + 1) * NN)
                    pi = st % 2
                    for kc in range(8):
                        S.op("tensor", k_mm(pc_[pi][:, 0:NN], w3[i][1][:, kc, :], hT[:, kc, cols], kc == 0, kc == 7),
                             reads=[f"w31{i}", "hTc"], writes=[f"pcc{pi}"])
                    for kc in range(8):
                        S.op("tensor", k_mm(pu_[pi][:, 0:NN], w3[i][2][:, kc, :], hT[:, kc, cols], kc == 0, kc == 7),
                             reads=[f"w32{i}", "hTc"], writes=[f"puc{pi}"])
                    S.op("scalar", k_act(cs_[:, 0:NN], pc_[pi][:, 0:NN], AF.Copy), reads=[f"pcc{pi}"], writes=["cs_"])
                    S.op("vector", k_tt(cup[:, 1 + st * NN:1 + (st + 1) * NN], cs_[:, 0:NN], pu_[pi][:, 0:NN], ALU.mult),
                         reads=["cs_", f"puc{pi}"], writes=["cup"])
                S.op("vector", k_copy(cup[:, 0:1], halo[:, 0, dc:dc + 1]), reads=["halo"], writes=["cup"])
                S.op("vector", k_copy(cup[:, NT + 1:NT + 2], halo[:, 1, dc:dc + 1]), reads=["halo"], writes=["cup"])
                S.op("vector", k_ts(ycv[:], cup[:, 0:NT], C("scw", 0 * 8 + dc, 1), None, ALU.mult), reads=["cup", "cst"], writes=["ycv"])
                S.op("vector", k_stt(ycv[:], cup[:, 1:NT + 1], C("scw", 1 * 8 + dc, 1), ycv[:], ALU.mult, ALU.add),
                     reads=["cup", "cst", "ycv"], writes=["ycv"])
                S.op("vector", k_stt(ycv[:], cup[:, 2:NT + 2], C("scw", 2 * 8 + dc, 1), ycv[:], ALU.mult, ALU.add),
                     reads=["cup", "cst", "ycv"], writes=["ycv"])
                for st in range(NST):
                    cols = slice(st * NN, (st + 1) * NN)
                    pi = st % 2
                    for kc in range(8):
                        S.op("tensor", k_mm(pb[pi][:, 0:NN], w3[i][0][:, kc, :], hT[:, kc, cols], kc == 0, kc == 7),
                             reads=[f"w30{i}", "hTc"], writes=[f"pbc{pi}"])
                    S.op("vector", k_tt(zT[:, dc, cols], ycv[:, cols], pb[pi][:, 0:NN], ALU.mult), reads=["ycv", f"pbc{pi}"], writes=["zT"])
            for jb in range(NB):
                i = jb % 2
                blk = slice(jb * 128, (jb + 1) * 128)
                S.dma("sync", k_dma(xt[i][:], xres[blk, :]), reads=[f"xres{jb}"], writes=[f"xtc{i}"])
                for hf in range(2):
                    for kc in range(8):
                        S.op("tensor", k_mm(pb[hf][:], zT[:, kc, blk], wo[:, kc, hf * 512:(hf + 1) * 512], kc == 0, kc == 7),
                             reads=["zT", "woc"], writes=[f"pbc{hf}"])
                    S.op("vector", k_tt(tmpy[:], pb[hf][:], BC[:, 2, hf * 512:(hf + 1) * 512], ALU.mult),
                         reads=[f"pbc{hf}", "BC2"], writes=["tmpyc"])
                    S.op("vector", k_tt(xt[i][:, hf * 512:(hf + 1) * 512], xt[i][:, hf * 512:(hf + 1) * 512], tmpy[:], ALU.add),
                         reads=["tmpyc", f"xtc{i}"], writes=[f"xtc{i}"])
                S.dma("sync", k_dma(xres[blk, :], xt[i][:]), reads=[f"xtc{i}"], writes=[f"xres{jb}"])
                if cfg.debug:
                    S.dma("sync", k_dma(dbg["d_xmix1"][blk, :], xt[i][:]), reads=[f"xtc{i}"], writes=["d_xmix1"])
            S.barrier()
            S.emit()
            if cfg.stop == 8:
                finish()
                return nc

        if moe_layer(1, 9, "d_xmoe1"):
            return nc

        with ExitStack() as ph:
            BC = sb(ph, "BCf", [128, 1, D], F32)
            pb = [ps(ph, f"pbf{i}", [128, 512], F32) for i in range(2)]
            xt = [sb(ph, f"xtf{i}", [128, D], F32) for i in range(2)]
            ot = [sb(ph, f"otf{i}", [128, D], F32) for i in range(2)]
            sqj = [sb(ph, f"sqjf{i}", [128, D], F32) for i in range(2)]
            ss = [sb(ph, f"ssf{i}", [128, 1], F32) for i in range(2)]
            rs = [sb(ph, f"rsf{i}", [128, 2], F32) for i in range(2)]
            bcast_rows(BC, pb, C("nfin"), 0, "cst")
            for j in range(NB):
                i = j % 2
                blk = slice(j * 128, (j + 1) * 128)
                S.dma("sync", k_dma(xt[i][:], xres[blk, :]), reads=[f"xres{j}"], writes=[f"xtf{i}"])
                S.op("scalar", k_act(sqj[i][:], xt[i][:], AF.Square, accum=ss[i][:, 0:1]), reads=[f"xtf{i}"], writes=[f"sqjf{i}", f"ssf{i}"])
                S.op("scalar", k_act(rs[i][:, 0:1], ss[i][:, 0:1], AF.Sqrt, scale=1.0 / D, bias=epsb[:, 0:1]), reads=[f"ssf{i}", "epsb"], writes=[f"rsf{i}"])
                S.op("vector", k_recip(rs[i][:, 1:2], rs[i][:, 0:1]), reads=[f"rsf{i}"], writes=[f"rsf2{i}"])
                S.op("vector", k_stt(ot[i][:], xt[i][:], rs[i][:, 1:2], BC[:, 0, :], ALU.mult, ALU.mult),
                     reads=[f"xtf{i}", f"rsf2{i}", "BC0"], writes=[f"otf{i}"])
                S.dma("sync", k_dma(out_ap[blk, :], ot[i][:]), reads=[f"otf{i}"], writes=[f"out{j}"])
        S.barrier()
        S.emit()
        finish()
    return nc


def _pos_table(T):
    rows = T // 64
    row = np.repeat(np.arange(rows), 64).astype(np.float32)
    col = np.tile(np.arange(64), rows).astype(np.float32)
    n_freq = D // 4
    omega = (np.float32(10000.0) ** (-np.arange(n_freq, dtype=np.float32) / np.float32(n_freq))).astype(np.float32)

    def emb(p):
        a = (p[:, None] * omega[None, :]).astype(np.float32)
        return np.concatenate([np.sin(a), np.cos(a)], axis=-1)
    return np.concatenate([emb(row), emb(col)], axis=-1).astype(np.float32)


def _consts(core, cfg, inp):
    b, q = core // 4, core % 4
    CAPP = cfg.CAPP
    cs = {}
    cs["ident"] = np.eye(128, dtype=np.float32)
    s = np.arange(128)[:, None]
    t = np.arange(128)[None, :]
    same = (s // 64) == (t // 64)
    cs["maskf"] = (same & (s <= t)).astype(np.float32)
    cs["maskb"] = (same & (s >= t)).astype(np.float32)
    pb_, pg_, pe_ = s // 64, (s % 64) // 16, s % 16
    qb_, qg_, qe_ = t // 64, (t % 64) // 16, t % 16
    samepair = (pb_ == qb_) & (pe_ == qe_)
    cs["Bm"] = samepair.astype(np.float32)
    cs["Tm"] = (samepair & (pg_ < qg_)).astype(np.float32)
    cs["hgn"] = np.broadcast_to(np.asarray(inp["hg_norm"][0], np.float32)[None, :], (128, 128)).copy()
    sel = np.zeros((128, 32), np.float32)
    for bb in range(2):
        for g in range(4):
            for el in range(2):
                sel[bb * 64 + g * 16 + 2 * core + el, bb * 8 + g * 2 + el] = 1.0
    rb = np.zeros((128, 32), np.float32)
    for e in range(16):
        sel[b * 64 + q * 16 + e, 16 + e] = 1.0
        rb[:, 16 + e] = (e // 2) * 2 * CAPP + b * CAPP
    cs["sel"] = sel
    cs["retbase"] = rb
    gs = np.zeros((128, 32), np.float32)
    for el in range(2):
        gs[:, el * 16 + 2 * core + el] = 1.0
    cs["gsel"] = gs
    bs = np.zeros((128, 2), np.float32)
    bs[:, b] = 1.0
    cs["bsel"] = bs
    mf = np.zeros((128, 8), np.float32)
    mb = np.zeros((128, 8), np.float32)
    sl = np.zeros((128, 8), np.float32)
    sr = np.zeros((128, 8), np.float32)
    for r in range(8):
        if r // 4 == b and r % 4 < q:
            mf[:, r] = 1.0
        if r // 4 == b and r % 4 > q:
            mb[:, r] = 1.0
    if q > 0:
        sl[:, core - 1] = 1.0
    if q < 3:
        sr[:, core + 1] = 1.0
    cs["mf"], cs["mb"], cs["selL"], cs["selR"] = mf, mb, sl, sr

    def fm(v):
        return np.asarray(v, np.float32).reshape(8, 128).T

    cs["nmix"] = np.concatenate([fm(inp["norm_mix"][l]) for l in range(2)], axis=1)
    cs["nffn"] = np.concatenate([fm(inp["norm_ffn"][l]) for l in range(2)], axis=1)
    cs["nfin"] = fm(inp["norm_final"])
    cs["lbl"] = np.concatenate([fm(inp["hg_lb_logits"][i]) for i in range(3)], axis=1)
    cs["scw"] = np.concatenate([fm(inp["sc_conv"][0][j]) for j in range(3)], axis=1)
    ab = np.zeros((128, 12), np.float32)
    for l in range(2):
        ab[:, l * 6:(l + 1) * 6] = np.asarray(inp["ada_b"][l][core * 768:(core + 1) * 768], np.float32).reshape(6, 128).T
    cs["adab"] = ab
    cv = np.zeros((128, 8, 4), np.float32)
    cv[:, :, 0] = fm(inp["c"][0])
    cv[:, :, 1] = fm(inp["c"][1])
    cv[:, :, 2] = fm(inp["c_ctx"])
    cs["cvT"] = cv.reshape(128, 32)
    cs["ones"] = np.ones((128, 128), np.float32)
    out = np.zeros((128, NCONST), np.float32)
    for n, (o, w) in CO.items():
        out[:, o:o + w] = cs[n]
    return out


def make_in_maps(inp, cfg):
    NT = cfg.NT
    T = 4 * NT
    pos = _pos_table(T)
    f32 = lambda a: np.ascontiguousarray(np.asarray(a, np.float32))
    shared = {
        "hg_w_in": f32(inp["hg_w_in"][0]), "hg_w_out": f32(inp["hg_w_out"][0]),
        "sc_w_in": f32(inp["sc_w_in"][0]), "sc_w_out": f32(inp["sc_w_out"][0]),
        "router": f32(inp["moe_router"]),
    }
    maps = []
    for core in range(8):
        b, q = core // 4, core % 4
        m = dict(shared)
        m["x"] = f32(inp["x"][b, q * NT:(q + 1) * NT])
        m["pos"] = f32(pos[q * NT:(q + 1) * NT])
        m["ctx"] = f32(inp["ctx"][b])
        m["consts"] = _consts(core, cfg, inp)
        m["adaw"] = f32(np.asarray(inp["ada_w"])[:, :, core * 768:(core + 1) * 768])
        m["wg"] = f32(np.asarray(inp["moe_w_gate"])[:, 2 * core:2 * core + 2])
        m["wu"] = f32(np.asarray(inp["moe_w_up"])[:, 2 * core:2 * core + 2])
        m["wd"] = f32(np.asarray(inp["moe_w_down"])[:, 2 * core:2 * core + 2])
        maps.append(m)
    return maps


_NC_CACHE = {}


def run(inp, cfg, trace=False):
    key = (cfg.NT, cfg.DEXP, cfg.stop, cfg.debug)
    if key not in _NC_CACHE:
        _NC_CACHE[key] = build(cfg)
    nc = _NC_CACHE[key]
    maps = make_in_maps(inp, cfg)
    res = run_bass_kernel_spmd(nc, maps, core_ids=list(range(8)))
    return res


def kernel(**inputs):
    cfg = Cfg(NT=np.asarray(inputs["x"]).shape[1] // 4, DEXP=np.asarray(inputs["moe_w_gate"]).shape[-1])
    res = run(inputs, cfg)
    NT = cfg.NT
    out = np.zeros((2, 4 * NT, D), np.float32)
    for core in range(8):
        b, q = core // 4, core % 4
        out[b, q * NT:(q + 1) * NT] = res.results[core]["out"]
    return out
```
